# Optimizing a Trainium2 kernel written in Bass

```python
import jax
import jax.numpy as jnp
from jax import lax
import numpy as np

D_MODEL = 4096
BATCH = 1
SEQ = 8192
DEPTH = 2

CHUNK = 64
N_MEM = 256
ML_HEADS = 8
ML_DQK = 128
ML_DV = 256
ML_WIDTH = ML_HEADS * ML_DV
GATE_SOFTCAP = 15.0
MLA_HEADS = 16
MLA_NOPE = 128
MLA_ROPE = 64
MLA_DV = 128
MLA_QK = MLA_NOPE + MLA_ROPE
MLA_WIDTH = MLA_HEADS * MLA_DV
Q_LORA = 1024
KV_LORA = 512
ROPE_THETA = 10000.0
Q_BLOCK = 128
MIX_WIDTH = ML_WIDTH + MLA_WIDTH
X_HEADS = 4
X_HEAD_DIM = 256
X_WIDTH = X_HEADS * X_HEAD_DIM
N_EXPERTS = 32
TOP_K = 4
D_EXPERT = 768
SWIGLU_LIMIT = 7.0
SWIGLU_ALPHA = 1.702
EXPERT_BLOCK = 128
DN_ALPHA = (2.0 * DEPTH) ** 0.25
DN_BETA = (8.0 * DEPTH) ** -0.25
LN_EPS = 1e-5
RMS_EPS = 1e-6
IN_SIZES = (ML_HEADS * ML_DQK, ML_HEADS * ML_DQK, ML_WIDTH, ML_WIDTH, ML_HEADS, ML_HEADS, Q_LORA, KV_LORA, MLA_ROPE)
IN_WIDTH = sum(IN_SIZES)
IN_SPLITS = tuple(sum(IN_SIZES[:i + 1]) for i in range(len(IN_SIZES) - 1))

kernel_name = 'hybrid_mlstm_mla_moe_deepnorm'


def layer_norm(x, g, b):
    xf = x.astype(jnp.float32)
    mu = jnp.mean(xf, axis=-1, keepdims=True)
    var = jnp.mean(jnp.square(xf - mu), axis=-1, keepdims=True)
    return ((xf - mu) * lax.rsqrt(var + LN_EPS) * g + b).astype(x.dtype)


def rms_norm(x, g):
    xf = x.astype(jnp.float32)
    y = xf * lax.rsqrt(jnp.mean(jnp.square(xf), axis=-1, keepdims=True) + RMS_EPS)
    return (y * g).astype(x.dtype)


def softcap(x):
    return GATE_SOFTCAP * jnp.tanh(x / GATE_SOFTCAP)


def rope(x, pos):
    half = x.shape[-1] // 2
    inv = ROPE_THETA ** (-jnp.arange(half, dtype=jnp.float32) / half)
    ang = pos.astype(jnp.float32)[:, :, None, None] * inv
    cos, sin = jnp.cos(ang), jnp.sin(ang)
    xf = x.astype(jnp.float32)
    x1, x2 = xf[..., :half], xf[..., half:]
    return jnp.concatenate([x1 * cos - x2 * sin, x2 * cos + x1 * sin], axis=-1).astype(x.dtype)


def mlstm_chunkwise(q, k, v, ig, fg):
    B, S, H, dqk = q.shape
    dv = v.shape[-1]
    nc = S // CHUNK

    def to_chunks(t):
        t = t.astype(jnp.float32).reshape((B, nc, CHUNK) + t.shape[2:])
        return jnp.moveaxis(t, (1, 3), (0, 2))

    qc = to_chunks(q)
    kc = to_chunks(k) * (dqk ** -0.5)
    vc = to_chunks(v)
    lic = to_chunks(ig)
    lfc = jax.nn.log_sigmoid(to_chunks(fg))
    causal = jnp.tril(jnp.ones((CHUNK, CHUNK), dtype=bool))

    def step(carry, inp):
        c_prev, n_prev, m_prev = carry
        qt, kt, vt, li, lf = inp
        b = jnp.cumsum(lf, axis=-1)
        d_mat = jnp.where(causal, b[..., :, None] - b[..., None, :] + li[..., None, :], -jnp.inf)
        a_inter = b + m_prev[..., None]
        m_t = jnp.maximum(a_inter, jnp.max(d_mat, axis=-1))
        w_inter = jnp.exp(a_inter - m_t)
        s_qk = jnp.einsum('bhtd,bhsd->bhts', qt, kt) * jnp.exp(d_mat - m_t[..., None])
        num = w_inter[..., None] * jnp.einsum('bhvd,bhtd->bhtv', c_prev, qt) + jnp.einsum('bhts,bhsv->bhtv', s_qk, vt)
        den = w_inter * jnp.einsum('bhd,bhtd->bht', n_prev, qt) + jnp.sum(s_qk, axis=-1)
        h = num / jnp.maximum(jnp.abs(den), jnp.exp(-m_t))[..., None]
        m_new = m_t[..., -1]
        w_prev = jnp.exp(b[..., -1] + m_prev - m_new)
        w_s = jnp.exp(b[..., -1:] - b + li - m_new[..., None])
        c_new = w_prev[..., None, None] * c_prev + jnp.einsum('bhs,bhsv,bhsd->bhvd', w_s, vt, kt)
        n_new = w_prev[..., None] * n_prev + jnp.einsum('bhs,bhsd->bhd', w_s, kt)
        return (c_new, n_new, m_new), h

    init = (jnp.zeros((B, H, dv, dqk), jnp.float32), jnp.zeros((B, H, dqk), jnp.float32), jnp.zeros((B, H), jnp.float32))
    _, h = lax.scan(step, init, (qc, kc, vc, lic, lfc))
    return jnp.moveaxis(h, (0, 2), (1, 3)).reshape(B, S, H, dv)


def chunk_causal_attention(q, k, v):
    B, S, H, dk = q.shape
    dv = v.shape[-1]
    nqb = S // Q_BLOCK
    scale = dk ** -0.5
    qb = q.reshape(B, nqb, Q_BLOCK, H, dk).transpose(1, 0, 2, 3, 4)
    k_chunk = jnp.arange(S) // CHUNK

    def attend(args):
        q_blk, blk = args
        s = jnp.einsum('bqhd,bkhd->bhqk', q_blk, k, preferred_element_type=jnp.float32) * scale
        q_chunk = (blk * Q_BLOCK + jnp.arange(Q_BLOCK)) // CHUNK
        s = jnp.where(k_chunk[None, :] <= q_chunk[:, None], s, -jnp.inf)
        p = jax.nn.softmax(s, axis=-1)
        return jnp.einsum('bhqk,bkhd->bqhd', p.astype(v.dtype), v)

    out = lax.map(attend, (qb, jnp.arange(nqb)))
    return out.transpose(1, 0, 2, 3, 4).reshape(B, S, H, dv)


def mla_group(cq, ckv, kr, pos, g_q, w_uq, g_kv, w_ukv):
    B, S, _ = cq.shape
    q = (rms_norm(cq, g_q) @ w_uq).reshape(B, S, MLA_HEADS, MLA_QK)
    q = jnp.concatenate([q[..., :MLA_NOPE], rope(q[..., MLA_NOPE:], pos)], axis=-1)
    kv = (rms_norm(ckv, g_kv) @ w_ukv).reshape(B, S, MLA_HEADS, MLA_NOPE + MLA_DV)
    k_rope = jnp.broadcast_to(rope(kr[:, :, None, :], pos), (B, S, MLA_HEADS, MLA_ROPE))
    k = jnp.concatenate([kv[..., :MLA_NOPE], k_rope], axis=-1)
    v = kv[..., MLA_NOPE:]
    return chunk_causal_attention(q, k, v)


def hybrid_mixer(x, pos, w_in, ml_b_i, ml_b_f, ml_norm_g, mla_g_q, mla_w_uq, mla_g_kv, mla_w_ukv, w_out):
    B, S, _ = x.shape
    q, k, v, o, ig, fg, cq, ckv, kr = jnp.split(x @ w_in, IN_SPLITS, axis=-1)
    ig = softcap(ig.astype(jnp.float32) + ml_b_i)
    fg = softcap(fg.astype(jnp.float32) + ml_b_f)
    h = mlstm_chunkwise(q.reshape(B, S, ML_HEADS, ML_DQK), k.reshape(B, S, ML_HEADS, ML_DQK),
                        v.reshape(B, S, ML_HEADS, ML_DV), ig, fg)
    h = rms_norm(h, ml_norm_g.reshape(ML_HEADS, ML_DV)) * jax.nn.sigmoid(o.astype(jnp.float32)).reshape(B, S, ML_HEADS, ML_DV)
    y_ml = h.reshape(B, S, ML_WIDTH).astype(x.dtype)
    y_mla = mla_group(cq, ckv, kr, pos, mla_g_q, mla_w_uq, mla_g_kv, mla_w_ukv).reshape(B, S, MLA_WIDTH).astype(x.dtype)
    return jnp.concatenate([y_ml, y_mla], axis=-1) @ w_out


def memory_cross_attention(x, mem, g_m, b_m, w_q, w_kv, w_o):
    B, S, _ = x.shape
    M = mem.shape[1]
    mem_n = layer_norm(mem, g_m, b_m)
    q = (x @ w_q).reshape(B, S, X_HEADS, X_HEAD_DIM)
    kv = mem_n @ w_kv
    k = kv[..., :X_WIDTH].reshape(B, M, X_HEADS, X_HEAD_DIM)
    v = kv[..., X_WIDTH:].reshape(B, M, X_HEADS, X_HEAD_DIM)
    s = jnp.einsum('bqhd,bmhd->bhqm', q, k, preferred_element_type=jnp.float32) * (X_HEAD_DIM ** -0.5)
    p = jax.nn.softmax(s, axis=-1)
    o = jnp.einsum('bhqm,bmhd->bqhd', p.astype(v.dtype), v).reshape(B, S, X_WIDTH)
    return o @ w_o


def moe_ffn(x, w_router, b_router, w_gu, b_gu, w_down, b_down):
    B, S, D = x.shape
    T = B * S
    xt = x.reshape(T, D)
    logits = (xt @ w_router).astype(jnp.float32) + b_router
    top_val, top_idx = lax.top_k(logits, TOP_K)
    gates = jax.nn.softmax(top_val, axis=-1)
    A = T * TOP_K
    e_flat = top_idx.reshape(A)
    tok_flat = jnp.arange(A, dtype=jnp.int32) // TOP_K
    g_flat = gates.reshape(A)
    order = jnp.argsort(e_flat)
    e_sorted, tok_sorted, g_sorted = e_flat[order], tok_flat[order], g_flat[order]
    counts = jnp.bincount(e_flat, length=N_EXPERTS)
    starts = jnp.cumsum(counts) - counts
    padded = (counts + EXPERT_BLOCK - 1) // EXPERT_BLOCK * EXPERT_BLOCK
    pad_ends = jnp.cumsum(padded)
    pad_starts = pad_ends - padded
    dest = pad_starts[e_sorted] + jnp.arange(A) - starts[e_sorted]
    nb = -(-A // EXPERT_BLOCK) + N_EXPERTS
    P = nb * EXPERT_BLOCK
    row_tok = jnp.full((P,), T, jnp.int32).at[dest].set(tok_sorted)
    row_gate = jnp.zeros((P,), jnp.float32).at[dest].set(g_sorted)
    block_expert = jnp.clip(jnp.searchsorted(pad_ends, jnp.arange(nb) * EXPERT_BLOCK, side='right'), 0, N_EXPERTS - 1)
    x_pad = jnp.concatenate([xt, jnp.zeros((1, D), xt.dtype)], axis=0)

    def expert_block(args):
        toks, e = args
        gu = x_pad[toks] @ w_gu[e] + b_gu[e]
        gate = jnp.minimum(gu[:, :D_EXPERT], SWIGLU_LIMIT)
        up = jnp.clip(gu[:, D_EXPERT:], -SWIGLU_LIMIT, SWIGLU_LIMIT)
        h = (up + 1.0) * gate * jax.nn.sigmoid(SWIGLU_ALPHA * gate)
        return h @ w_down[e] + b_down[e]

    y_rows = lax.map(expert_block, (row_tok.reshape(nb, EXPERT_BLOCK), block_expert)).reshape(P, D)
    y = jax.ops.segment_sum(y_rows * row_gate[:, None].astype(y_rows.dtype), row_tok, num_segments=T + 1)[:T]
    return y.reshape(B, S, D)


def setup_inputs(seed: int = 0) -> dict:
    key = jax.random.key(seed)
    ks = iter(jax.random.split(key, 64))
    f32 = jnp.float32
    L, D = DEPTH, D_MODEL

    def nrm(shape, scale):
        return jax.random.normal(next(ks), shape, f32) * scale

    def gain(shape):
        return 1.0 + nrm(shape, 0.02)

    x = nrm((BATCH, SEQ, D), 1.0)
    mem = nrm((BATCH, N_MEM, D), 1.0)
    positions = jnp.arange(SEQ, dtype=jnp.int32)[None, :] + jax.random.randint(next(ks), (BATCH, 1), 0, 4096, dtype=jnp.int32)
    in_scales = (1.0, 1.0, DN_BETA, 1.0, 1.0, 1.0, 1.0, 1.0, 1.0)
    w_in = jnp.concatenate([nrm((L, D, sz), D ** -0.5 * sc) for sz, sc in zip(IN_SIZES, in_scales)], axis=-1)
    ml_b_i = nrm((L, ML_HEADS), 0.1)
    ml_b_f = 3.0 + nrm((L, ML_HEADS), 0.5)
    ml_norm_g = gain((L, ML_WIDTH))
    mla_g_q = gain((L, Q_LORA))
    mla_w_uq = nrm((L, Q_LORA, MLA_HEADS * MLA_QK), Q_LORA ** -0.5)
    mla_g_kv = gain((L, KV_LORA))
    mla_w_ukv = jnp.concatenate([nrm((L, KV_LORA, MLA_HEADS, MLA_NOPE), KV_LORA ** -0.5),
                                 nrm((L, KV_LORA, MLA_HEADS, MLA_DV), KV_LORA ** -0.5 * DN_BETA)], axis=-1).reshape(L, KV_LORA, MLA_HEADS * (MLA_NOPE + MLA_DV))
    w_out = nrm((L, MIX_WIDTH, D), MIX_WIDTH ** -0.5 * DN_BETA)
    ln1_g, ln1_b = gain((L, D)), nrm((L, D), 0.02)
    x_g_mem, x_b_mem = gain((L, D)), nrm((L, D), 0.02)
    x_w_q = nrm((L, D, X_WIDTH), D ** -0.5)
    x_w_kv = jnp.concatenate([nrm((L, D, X_WIDTH), D ** -0.5), nrm((L, D, X_WIDTH), D ** -0.5 * DN_BETA)], axis=-1)
    x_w_o = nrm((L, X_WIDTH, D), X_WIDTH ** -0.5 * DN_BETA)
    ln2_g, ln2_b = gain((L, D)), nrm((L, D), 0.02)
    w_router = nrm((L, D, N_EXPERTS), D ** -0.5)
    b_router = nrm((L, N_EXPERTS), 0.01)
    w_gu = nrm((L, N_EXPERTS, D, 2 * D_EXPERT), D ** -0.5)
    b_gu = nrm((L, N_EXPERTS, 2 * D_EXPERT), 0.01)
    w_down = nrm((L, N_EXPERTS, D_EXPERT, D), D_EXPERT ** -0.5 * DN_BETA)
    b_down = nrm((L, N_EXPERTS, D), 0.01)
    ln3_g, ln3_b = gain((L, D)), nrm((L, D), 0.02)
    return {'x': x, 'mem': mem, 'positions': positions,
            'w_in': w_in, 'ml_b_i': ml_b_i, 'ml_b_f': ml_b_f, 'ml_norm_g': ml_norm_g,
            'mla_g_q': mla_g_q, 'mla_w_uq': mla_w_uq, 'mla_g_kv': mla_g_kv, 'mla_w_ukv': mla_w_ukv,
            'w_out': w_out, 'ln1_g': ln1_g, 'ln1_b': ln1_b,
            'x_g_mem': x_g_mem, 'x_b_mem': x_b_mem, 'x_w_q': x_w_q, 'x_w_kv': x_w_kv, 'x_w_o': x_w_o,
            'ln2_g': ln2_g, 'ln2_b': ln2_b,
            'w_router': w_router, 'b_router': b_router, 'w_gu': w_gu, 'b_gu': b_gu,
            'w_down': w_down, 'b_down': b_down, 'ln3_g': ln3_g, 'ln3_b': ln3_b}


def reference(x, mem, positions, w_in, ml_b_i, ml_b_f, ml_norm_g, mla_g_q, mla_w_uq, mla_g_kv, mla_w_ukv,
              w_out, ln1_g, ln1_b, x_g_mem, x_b_mem, x_w_q, x_w_kv, x_w_o, ln2_g, ln2_b,
              w_router, b_router, w_gu, b_gu, w_down, b_down, ln3_g, ln3_b):
    for l in range(DEPTH):
        h = hybrid_mixer(x, positions, w_in[l], ml_b_i[l], ml_b_f[l], ml_norm_g[l], mla_g_q[l], mla_w_uq[l],
                         mla_g_kv[l], mla_w_ukv[l], w_out[l])
        x = layer_norm(DN_ALPHA * x + h, ln1_g[l], ln1_b[l])
        h = memory_cross_attention(x, mem, x_g_mem[l], x_b_mem[l], x_w_q[l], x_w_kv[l], x_w_o[l])
        x = layer_norm(DN_ALPHA * x + h, ln2_g[l], ln2_b[l])
        h = moe_ffn(x, w_router[l], b_router[l], w_gu[l], b_gu[l], w_down[l], b_down[l])
        x = layer_norm(DN_ALPHA * x + h, ln3_g[l], ln3_b[l])
    return x
```

```python
import math, os
import os
import numpy as np
from contextlib import ExitStack
import concourse.bass as bass
import concourse.mybir as mybir
from concourse.bass_utils import run_bass_kernel_spmd

F32 = mybir.dt.float32
BF16 = mybir.dt.bfloat16
I32 = mybir.dt.int32
U32 = mybir.dt.uint32
AF = mybir.ActivationFunctionType
ALU = mybir.AluOpType
AX = mybir.AxisListType


class Res:
    __slots__ = ("name", "ws", "rs", "excl", "wdma")

    def __init__(self, name="", excl=False):
        self.name = name
        self.excl = excl
        self.ws = {}
        self.rs = {}
        self.wdma = False


class PB:
    NDMA = 16

    def __init__(self, nc, es):
        self.nc = nc
        self.es = es
        self.eng = {"pe": nc.tensor, "act": nc.scalar, "dve": nc.vector, "pool": nc.gpsimd, "sp": nc.sync}
        self.sem = {k: es.enter_context(nc.semaphore("prog_" + k)) for k in self.eng}
        self.cnt = {k: 0 for k in self.eng}
        self.seen = {k: {} for k in self.eng}
        self.dsem = {}
        self.dval = {}
        self.dnext = {}
        for q in ("sp", "act", "pool"):
            self.dsem[q] = [es.enter_context(nc.semaphore("dma_%s_%d" % (q, i))) for i in range(self.NDMA)]
            self.dval[q] = [0] * self.NDMA
            self.dnext[q] = 0
        self.n_inst = 0
        self.inorder = set(os.environ.get("PB_INORDER", "").split(",")) if "os" in globals() else set()

    def sb(self, name, shape, dt):
        return self.es.enter_context(self.nc.sbuf_tensor(name, shape, dt))

    def ps(self, name, shape, dt=F32):
        return self.es.enter_context(self.nc.psum_tensor(name, shape, dt))

    def _wait(self, en, tok):
        key, sem, val = tok
        if self.seen[en].get(key, 0) >= val:
            return
        self.eng[en].wait_ge(sem, val)
        if getattr(self, 'log', None) is not None: self.log.append((en, 'wait', key, val))
        self.seen[en][key] = val
        self.n_inst += 1

    def _deps(self, en, reads, writes, is_dma=False):
        toks = []
        for r in reads:
            toks.extend(r.ws.values())
        for w in writes:
            if is_dma and w.wdma and not w.rs:
                continue
            toks.extend(w.ws.values())
            toks.extend(w.rs.values())
        for t in toks:
            if t[0] == en and (en == "pe" or en in self.inorder):
                continue
            self._wait(en, t)

    def _commit(self, tok, reads, writes, is_dma=False):
        for r in reads:
            old = r.rs.get(tok[0])
            if old is None or old[2] < tok[2]:
                r.rs[tok[0]] = tok
        for w in writes:
            if is_dma and w.wdma and not w.rs:
                old = w.ws.get(tok[0])
                if old is None or old[2] < tok[2]:
                    w.ws[tok[0]] = tok
            else:
                w.ws = {tok[0]: tok}
                w.rs = {}
                w.wdma = is_dma

    def op(self, en, fn, reads=(), writes=()):
        ex = [r for r in reads if r.excl]
        if ex:
            reads = [r for r in reads if not r.excl]
            writes = list(writes) + ex
        self._deps(en, reads, writes)
        inst = fn(self.eng[en])
        self.cnt[en] += 1
        if getattr(self, 'log', None) is not None: self.log.append((en, 'op', self.cnt[en], [r.name for r in reads], [w.name for w in writes]))
        inst.then_inc(self.sem[en], 1)
        self.n_inst += 1
        self._commit((en, self.sem[en], self.cnt[en]), reads, writes)
        return inst

    def dma(self, q, out, in_, reads=(), writes=(), fn=None, **kw):
        self._deps(q, reads, writes, is_dma=True)
        i = self.dnext[q]
        self.dnext[q] = (i + 1) % self.NDMA
        sem = self.dsem[q][i]
        key = "d_%s_%d" % (q, i)
        if self.dval[q][i] > 0:
            self._wait(q, (key, sem, self.dval[q][i]))
        if fn is not None:
            inst = fn(self.eng[q])
        else:
            inst = self.eng[q].dma_start(out=out, in_=in_, **kw)
        self.dval[q][i] += 16
        inst.then_inc(sem, 16)
        self.n_inst += 1
        self._commit((key, sem, self.dval[q][i]), reads, writes, is_dma=True)
        return inst

    def barrier(self):
        toks = [(k, self.sem[k], self.cnt[k]) for k in self.eng if self.cnt[k] > 0]
        for q in self.dsem:
            for i in range(self.NDMA):
                if self.dval[q][i] > 0:
                    toks.append(("d_%s_%d" % (q, i), self.dsem[q][i], self.dval[q][i]))
        for en in self.eng:
            for t in toks:
                if t[0] == en:
                    continue
                self._wait(en, t)

D = 4096; TPC = 1024

KT = 32
NW = 256
OFF = {}
_o = 0
for _n, _sz in [("mlqT", 128 * 1024), ("mlkT", 128 * 1024), ("mlk", 1024 * 128), ("mlv", 1024 * 256), ("mlo", 1024 * 256),
                ("qT", 2 * 192 * 1024), ("kT", 2 * 128 * 1024), ("krT", 64 * 1024), ("v", 1024 * 256)]:
    OFF[_n] = _o; _o += _sz
P1N = _o
ROPE_INV = (10000.0 ** (-np.arange(32, dtype=np.float32) / 32)).astype(np.float32)


def p1_consts():
    inv = np.concatenate([ROPE_INV, ROPE_INV]).reshape(64, 1).astype(np.float32)
    sgn = np.concatenate([-np.ones(32), np.ones(32)]).reshape(64, 1).astype(np.float32)
    return {"c_inv": inv, "c_sgn": sgn}


def build_p1(x_is_bf16=False):
    nc = bass.Bass("TRN2", target_bir_lowering=False)
    dt_x = BF16 if x_is_bf16 else F32
    xT_d = nc.dram_tensor("xT", [D, TPC], dt_x, kind="ExternalInput").ap()
    w_in = nc.dram_tensor("w_in", [D, 7760], F32, kind="ExternalInput").ap()
    w_uq = nc.dram_tensor("w_uq", [1024, 3072], F32, kind="ExternalInput").ap()
    w_ukv = nc.dram_tensor("w_ukv", [512, 4096], F32, kind="ExternalInput").ap()
    g_q = nc.dram_tensor("g_q", [1024], F32, kind="ExternalInput").ap()
    g_kv = nc.dram_tensor("g_kv", [512], F32, kind="ExternalInput").ap()
    pos_d = nc.dram_tensor("pos", [1, TPC], I32, kind="ExternalInput").ap()
    c_inv = nc.dram_tensor("c_inv", [64, 1], F32, kind="ExternalInput").ap()
    c_sgn = nc.dram_tensor("c_sgn", [64, 1], F32, kind="ExternalInput").ap()
    out = nc.dram_tensor("p1out", [8, P1N], BF16, kind="ExternalOutput").ap()
    outg = nc.dram_tensor("p1g", [8, 2, TPC], F32, kind="ExternalOutput").ap()
    with ExitStack() as es:
        pb = PB(nc, es)
        emit_p1(pb, xT_d, x_is_bf16, w_in, w_uq, w_ukv, g_q, g_kv, pos_d, c_inv, c_sgn, out, outg)
        pb.barrier()
        print("p1 instructions", pb.n_inst)
    return nc


def emit_p1(pb, xT_d, x_is_bf16, w_in, w_uq, w_ukv, g_q, g_kv, pos_d, c_inv, c_sgn, out, outg):
    nc = pb.nc
    xT = pb.sb("xT_sb", [128, KT, TPC], BF16); r_xT = Res("xT")
    wbuf = [pb.sb("wb%d" % i, [128, KT, NW], BF16) for i in range(2)]
    r_w = [Res("w%d" % i) for i in range(2)]
    psum = [pb.ps("ps%d" % i, [128, 512]) for i in range(8)]
    r_ps = [Res("ps%d" % i, excl=True) for i in range(8)]
    stg = [pb.sb("stg%d" % i, [128, 512], BF16) for i in range(4)]
    r_stg = [Res("stg%d" % i) for i in range(4)]
    stgf = [pb.sb("stgf%d" % i, [128, 512], F32) for i in range(2)]
    r_stgf = [Res("stgf%d" % i) for i in range(2)]
    cqT = pb.sb("cqT", [128, 8, TPC], BF16); r_cq = Res("cq")
    ckvT = pb.sb("ckvT", [128, 4, TPC], BF16); r_ckv = Res("ckv")
    gq_sb = pb.sb("gq_sb", [128, 8], F32); gkv_sb = pb.sb("gkv_sb", [128, 4], F32); r_g = Res("g")
    ones = pb.sb("ones", [128, 128], BF16); r_ones = Res("ones")
    rstd = pb.sb("rstd", [128, TPC], F32); r_rstd = Res("rstd")
    krT = pb.sb("krT", [64, TPC], F32); krR = pb.sb("krR", [64, TPC], F32); r_kr = Res("kr"); r_krR = Res("krR")
    cos_t = pb.sb("cos_t", [64, TPC], F32); sin_t = pb.sb("sin_t", [64, TPC], F32); r_cs = Res("cs")
    tmpA = pb.sb("tmpA", [64, TPC], F32); tmpB = pb.sb("tmpB", [64, TPC], F32); r_tA = Res("tA"); r_tB = Res("tB")
    posi = pb.sb("posi", [64, TPC], I32); inv_sb = pb.sb("inv_sb", [64, 1], F32); sgn_sb = pb.sb("sgn_sb", [64, 1], F32); r_pos = Res("pos")
    wkr = pb.sb("wkr", [128, KT, 128], BF16); r_wkr = Res("wkr")
    st = {"w": 0, "ps": 0, "stg": 0, "ev": 0, "nps": 8}

    xT_v = xT_d.rearrange("(kt p) t -> p kt t", p=128)
    xq = "pool" if not x_is_bf16 else "sp"
    for g in range(8):
        pb.dma(xq, xT[:, g * 4:(g + 1) * 4, :], xT_v[:, g * 4:(g + 1) * 4, :], writes=[r_xT])
    w_v = w_in.rearrange("(kt p) n -> p kt n", p=128)
    pb.dma("sp", gq_sb[:, :], g_q.rearrange("(ft p) -> p ft", p=128), writes=[r_g], allow_slow_non_contiguous=True)
    pb.dma("sp", gkv_sb[:, :], g_kv.rearrange("(ft p) -> p ft", p=128), writes=[r_g], allow_slow_non_contiguous=True)
    pb.dma("sp", posi[:, :], pos_d[0:1, :].partition_broadcast(64), writes=[r_pos])
    pb.dma("sp", inv_sb[:, :], c_inv, writes=[r_pos])
    pb.dma("sp", sgn_sb[:, :], c_sgn, writes=[r_pos])
    pb.op("pool", lambda e: e.memset(ones[:, :], 1.0), writes=[r_ones])

    def sub(c, name, n):
        return out[c, OFF[name]:OFF[name] + n]

    def load_w(c0, ncols):
        i = st["w"] % 2; st["w"] += 1
        for g in range(4):
            pb.dma("pool", wbuf[i][:, g * 8:(g + 1) * 8, 0:ncols], w_v[:, g * 8:(g + 1) * 8, c0:c0 + ncols], writes=[r_w[i]])
        return i

    def next_ps():
        i = st["ps"] % st["nps"]; st["ps"] += 1
        return i

    def next_stg():
        i = st["stg"] % 4; st["stg"] += 1
        return i

    def evac_engine():
        st["ev"] += 1
        return "act" if st["ev"] % 2 else "dve"

    def copy_scaled(en, o, i, scale, reads, writes):
        if en == "act":
            pb.op("act", lambda e: e.activation(out=o, in_=i, func=AF.Copy, scale=float(scale)), reads, writes)
        else:
            pb.op("dve", lambda e: e.tensor_scalar(out=o, in0=i, scalar1=float(scale), scalar2=None, op0=ALU.mult), reads, writes)

    def fm_group(lhs_fn, kts, rhs_fn, m, pi, reads):
        n = len(kts)
        for j, kt in enumerate(kts):
            pb.op("pe", lambda e, kt=kt, j=j: e.matmul(psum[pi][0:m, :], lhsT=lhs_fn(kt), rhs=rhs_fn(kt), start=(j == 0), stop=(j == n - 1)),
                  reads=reads, writes=[r_ps[pi]])

    def fm_segment(c0, ncols_total, evac):
        for cb in range(0, ncols_total, NW):
            ncols = min(NW, ncols_total - cb)
            wi = load_w(c0 + cb, ncols)
            for m0 in range(0, ncols, 128):
                m = min(128, ncols - m0)
                for th in range(2):
                    pi = next_ps()
                    fm_group(lambda kt: wbuf[wi][:, kt, m0:m0 + m], range(KT), lambda kt: xT[:, kt, th * 512:(th + 1) * 512], m, pi, [r_w[wi], r_xT])
                    evac(cb + m0, m, th, pi)

    def ev_q(scale, name):
        def f(c, m, th, pi):
            si = next_stg()
            copy_scaled(evac_engine(), stg[si][0:m, :], psum[pi][0:m, :], scale, [r_ps[pi]], [r_stg[si]])
            h = c // 128
            pb.dma("sp", sub(h, name, 128 * 1024).rearrange("(d t) -> d t", t=1024)[:, th * 512:(th + 1) * 512], stg[si][0:m, :], reads=[r_stg[si]])
        return f
    fm_segment(0, 1024, ev_q(1.0, "mlqT"))
    fm_segment(1024, 1024, ev_q(128 ** -0.5, "mlkT"))

    def ev_g(c, m, th, pi):
        si = st["stg"] % 2; st["stg"] += 1
        pb.op("dve", lambda e: e.tensor_copy(out=stgf[si][0:16, :], in_=psum[pi][0:16, :]), [r_ps[pi]], [r_stgf[si]])
        pb.dma("sp", outg[:, 0, th * 512:(th + 1) * 512], stgf[si][0:8, :], reads=[r_stgf[si]])
        pb.dma("sp", outg[:, 1, th * 512:(th + 1) * 512], stgf[si][8:16, :], reads=[r_stgf[si]])
    fm_segment(6144, 16, ev_g)

    def tm_segment(c0, ncols_total, kind, name, hw):
        for cb in range(0, ncols_total, NW):
            wi = load_w(c0 + cb, NW)
            for tt in range(8):
                pi = next_ps()
                for kt in range(KT):
                    pb.op("pe", lambda e, kt=kt: e.matmul(psum[pi][:, 0:NW], lhsT=xT[:, kt, tt * 128:(tt + 1) * 128], rhs=wbuf[wi][:, kt, 0:NW], start=(kt == 0), stop=(kt == KT - 1)),
                          reads=[r_w[wi], r_xT], writes=[r_ps[pi]])
                si = next_stg()
                if kind == "sig":
                    pb.op("act", lambda e: e.activation(out=stg[si][:, 0:NW], in_=psum[pi][:, 0:NW], func=AF.Sigmoid), [r_ps[pi]], [r_stg[si]])
                else:
                    copy_scaled(evac_engine(), stg[si][:, 0:NW], psum[pi][:, 0:NW], kind, [r_ps[pi]], [r_stg[si]])
                nh = NW // hw
                h0 = cb // hw
                for hh in range(max(nh, 1)):
                    if hw >= NW:
                        h = cb // hw; coff = cb % hw; wd = NW
                    else:
                        h = h0 + hh; coff = 0; wd = hw
                    dst = sub(h, name, 1024 * hw).rearrange("(t d) -> t d", d=hw)[tt * 128:(tt + 1) * 128, coff:coff + wd]
                    src = stg[si][:, hh * wd:(hh + 1) * wd] if hw < NW else stg[si][:, 0:NW]
                    pb.dma("sp", dst, src, reads=[r_stg[si]])
    tm_segment(1024, 1024, 128 ** -0.5, "mlk", 128)
    tm_segment(2048, 2048, 1.0, "mlv", 256)
    tm_segment(4096, 2048, "sig", "mlo", 256)

    import os
    STAGE = int(os.environ.get('STAGE', '99'))
    if STAGE < 1: return
    st["nps"] = 6
    st["ps"] = 0

    def latent_segment(c0, nf, latT, r_lat, g_sb):
        nft = nf // 128

        def ev(c, m, th, pi):
            ft = c // 128
            pb.op("dve", lambda e: e.tensor_scalar(out=latT[:, ft, th * 512:(th + 1) * 512], in0=psum[pi][:, :], scalar1=g_sb[:, ft:ft + 1], scalar2=None, op0=ALU.mult),
                  [r_ps[pi], r_g], [r_lat])
            if os.environ.get('SUB') == 'b': return
            si = next_stg()
            pb.op("act", lambda e: e.activation(out=stg[si][:, :], in_=psum[pi][:, :], func=AF.Square), [r_ps[pi]], [r_stg[si]])
            if os.environ.get('SUB2') == 'nomm': return
            pb.op("pe", lambda e: e.matmul(psum[6 + th][:, :], lhsT=ones[:, :], rhs=stg[si][:, :], start=(ft == 0), stop=(ft == nft - 1)),
                  reads=[r_ones, r_stg[si]], writes=[r_ps[6 + th]])
        fm_segment(c0, nf, ev)
        if os.environ.get('SUB') in ('b', 'c'): return
        for th in range(2):
            pb.op("act", lambda e: e.activation(out=rstd[:, th * 512:(th + 1) * 512], in_=psum[6 + th][:, :], func=AF.Sqrt, scale=1.0 / nf, bias=1e-6),
                  [r_ps[6 + th]], [r_rstd])
        pb.op("dve", lambda e: e.reciprocal(out=rstd[:, :], in_=rstd[:, :]), [r_rstd], [r_rstd])
        for ft in range(nft):
            en = "pool" if ft % 2 else "dve"
            pb.op(en, lambda e: e.tensor_tensor(out=latT[:, ft, :], in0=latT[:, ft, :], in1=rstd[:, :], op=ALU.mult), [r_lat, r_rstd], [r_lat])

    def rope_tables():
        def wrap(o, i, shift):
            m = krR
            pb.op("dve", lambda e: e.tensor_scalar(out=o[:, :], in0=i[:, :], scalar1=float(shift), scalar2=None, op0=ALU.add), [r_tA, r_cs], [r_tB])
            pb.op("dve", lambda e: e.tensor_scalar(out=m[:, :], in0=o[:, :], scalar1=float(math.pi), scalar2=float(-2 * math.pi), op0=ALU.is_gt, op1=ALU.mult), [r_tB], [r_krR])
            pb.op("dve", lambda e: e.tensor_tensor(out=o[:, :], in0=o[:, :], in1=m[:, :], op=ALU.add), [r_tB, r_krR], [r_tB])
            pb.op("dve", lambda e: e.tensor_scalar(out=m[:, :], in0=o[:, :], scalar1=float(-math.pi), scalar2=float(2 * math.pi), op0=ALU.is_lt, op1=ALU.mult), [r_tB], [r_krR])
            pb.op("dve", lambda e: e.tensor_tensor(out=o[:, :], in0=o[:, :], in1=m[:, :], op=ALU.add), [r_tB, r_krR], [r_tB])
        ang = tmpA; kf = tmpB
        pb.op("dve", lambda e: e.tensor_copy(out=ang[:, :], in_=posi[:, :]), [r_pos], [r_tA])
        pb.op("dve", lambda e: e.tensor_scalar(out=ang[:, :], in0=ang[:, :], scalar1=inv_sb[:, 0:1], scalar2=None, op0=ALU.mult), [r_tA, r_pos], [r_tA])
        pb.op("dve", lambda e: e.tensor_scalar(out=kf[:, :], in0=ang[:, :], scalar1=float(1.0 / (2 * math.pi)), scalar2=0.5, op0=ALU.mult, op1=ALU.add), [r_tA], [r_tB])
        pb.op("dve", lambda e: e.tensor_copy(out=posi[:, :], in_=kf[:, :]), [r_tB], [r_pos])
        pb.op("dve", lambda e: e.tensor_copy(out=kf[:, :], in_=posi[:, :]), [r_pos], [r_tB])
        pb.op("dve", lambda e: e.scalar_tensor_tensor(out=ang[:, :], in0=kf[:, :], scalar=float(-2 * math.pi), in1=ang[:, :], op0=ALU.mult, op1=ALU.add), [r_tA, r_tB], [r_tA])
        wrap(kf, ang, 0.0)
        pb.op("act", lambda e: e.activation(out=sin_t[:, :], in_=kf[:, :], func=AF.Sin), [r_tB], [r_cs])
        wrap(kf, ang, math.pi / 2)
        pb.op("act", lambda e: e.activation(out=cos_t[:, :], in_=kf[:, :], func=AF.Sin), [r_tB], [r_cs])
        pb.op("dve", lambda e: e.tensor_scalar(out=sin_t[:, :], in0=sin_t[:, :], scalar1=sgn_sb[:, 0:1], scalar2=None, op0=ALU.mult), [r_cs, r_pos], [r_cs])
    rope_tables()
    if STAGE < 2: return

    def rope_apply(o_ap, a_ap, b_ap, reads, writes):
        pb.op("dve", lambda e: e.tensor_tensor(out=tmpA[:, :], in0=a_ap, in1=cos_t[:, :], op=ALU.mult), reads + [r_cs], [r_tA])
        pb.op("dve", lambda e: e.tensor_tensor(out=tmpB[:, :], in0=b_ap, in1=sin_t[:, :], op=ALU.mult), reads + [r_cs], [r_tB])
        pb.op("dve", lambda e: e.tensor_tensor(out=o_ap, in0=tmpA[:, :], in1=tmpB[:, :], op=ALU.add), [r_tA, r_tB], writes)

    for g in range(4):
        pb.dma("pool", wkr[:, g * 8:(g + 1) * 8, 0:64], w_v[:, g * 8:(g + 1) * 8, 7696:7760], writes=[r_wkr])
        pb.dma("pool", wkr[:, g * 8:(g + 1) * 8, 64:96], w_v[:, g * 8:(g + 1) * 8, 7728:7760], writes=[r_wkr])
        pb.dma("pool", wkr[:, g * 8:(g + 1) * 8, 96:128], w_v[:, g * 8:(g + 1) * 8, 7696:7728], writes=[r_wkr])
    for th in range(2):
        for which, dstT, r_d in ((0, krT, r_kr), (1, krR, r_krR)):
            pi = next_ps()
            fm_group(lambda kt: wkr[:, kt, which * 64:(which + 1) * 64], range(KT), lambda kt: xT[:, kt, th * 512:(th + 1) * 512], 64, pi, [r_wkr, r_xT])
            pb.op("act", lambda e: e.activation(out=dstT[:, th * 512:(th + 1) * 512], in_=psum[pi][0:64, :], func=AF.Copy), [r_ps[pi]], [r_d])
    krb = pb.sb("krb", [64, TPC], BF16); r_krb = Res("krb")
    rope_apply(krb[:, :], krT[:, :], krR[:, :], [r_kr, r_krR], [r_krb])
    for c in range(8):
        pb.dma("sp", sub(c, "krT", 64 * 1024).rearrange("(d t) -> d t", t=1024), krb[:, :], reads=[r_krb])

    if STAGE < 3: return
    latent_segment(6160, 1024, cqT, r_cq, gq_sb)
    if os.environ.get('SUB') in ('a', 'c'): return
    wq = [pb.sb("wq%d" % i, [128, 8, 256], BF16) for i in range(2)]; r_wq = [Res("wq0"), Res("wq1")]
    uq_v = w_uq.rearrange("(ft p) n -> p ft n", p=128)
    qro = pb.sb("qro", [64, TPC], F32); qrr = pb.sb("qrr", [64, TPC], F32); r_qro = Res("qro"); r_qrr = Res("qrr")
    qrb = pb.sb("qrb", [64, TPC], BF16); r_qrb = Res("qrb")
    for h in range(16):
        wi = h % 2
        b0 = h * 192
        pb.dma("pool", wq[wi][:, :, 0:192], uq_v[:, :, b0:b0 + 192], writes=[r_wq[wi]])
        pb.dma("pool", wq[wi][:, :, 192:224], uq_v[:, :, b0 + 160:b0 + 192], writes=[r_wq[wi]])
        pb.dma("pool", wq[wi][:, :, 224:256], uq_v[:, :, b0 + 128:b0 + 160], writes=[r_wq[wi]])
        c = h // 2; hs = h % 2
        dst_h = sub(c, "qT", 2 * 192 * 1024).rearrange("(h d t) -> h d t", h=2, t=1024)
        for th in range(2):
            pi = next_ps()
            fm_group(lambda ft: wq[wi][:, ft, 0:128], range(8), lambda ft: cqT[:, ft, th * 512:(th + 1) * 512], 128, pi, [r_wq[wi], r_cq])
            si = next_stg()
            copy_scaled(evac_engine(), stg[si][:, :], psum[pi][:, :], 1.0, [r_ps[pi]], [r_stg[si]])
            pb.dma("sp", dst_h[hs, 0:128, th * 512:(th + 1) * 512], stg[si][:, :], reads=[r_stg[si]])
            for which, dstT, r_d in ((0, qro, r_qro), (1, qrr, r_qrr)):
                pi = next_ps()
                fm_group(lambda ft: wq[wi][:, ft, 128 + which * 64:192 + which * 64], range(8), lambda ft: cqT[:, ft, th * 512:(th + 1) * 512], 64, pi, [r_wq[wi], r_cq])
                pb.op("act", lambda e: e.activation(out=dstT[:, th * 512:(th + 1) * 512], in_=psum[pi][0:64, :], func=AF.Copy), [r_ps[pi]], [r_d])
        rope_apply(qrb[:, :], qro[:, :], qrr[:, :], [r_qro, r_qrr], [r_qrb])
        pb.dma("sp", dst_h[hs, 128:192, :], qrb[:, :], reads=[r_qrb])

    if STAGE < 4: return
    latent_segment(7184, 512, ckvT, r_ckv, gkv_sb)
    wkv = [pb.sb("wkv%d" % i, [128, 4, 256], BF16) for i in range(2)]; r_wkv = [Res("wkv0"), Res("wkv1")]
    ukv_v = w_ukv.rearrange("(ft p) n -> p ft n", p=128)
    for h in range(16):
        wi = h % 2
        pb.dma("pool", wkv[wi][:, :, :], ukv_v[:, :, h * 256:(h + 1) * 256], writes=[r_wkv[wi]])
        c = h // 2; hs = h % 2
        dst_k = sub(c, "kT", 2 * 128 * 1024).rearrange("(h d t) -> h d t", h=2, t=1024)
        dst_v = sub(c, "v", 1024 * 256).rearrange("(t h d) -> t h d", h=2, d=128)
        for th in range(2):
            pi = next_ps()
            fm_group(lambda ft: wkv[wi][:, ft, 0:128], range(4), lambda ft: ckvT[:, ft, th * 512:(th + 1) * 512], 128, pi, [r_wkv[wi], r_ckv])
            si = next_stg()
            copy_scaled(evac_engine(), stg[si][:, :], psum[pi][:, :], 1.0, [r_ps[pi]], [r_stg[si]])
            pb.dma("sp", dst_k[hs, :, th * 512:(th + 1) * 512], stg[si][:, :], reads=[r_stg[si]])
        for tq in range(2):
            pi = next_ps()
            for j in range(4):
                tt = tq * 4 + j
                for ft in range(4):
                    pb.op("pe", lambda e, ft=ft: e.matmul(psum[pi][:, j * 128:(j + 1) * 128], lhsT=ckvT[:, ft, tt * 128:(tt + 1) * 128], rhs=wkv[wi][:, ft, 128:256], start=(ft == 0), stop=(ft == 3)),
                          reads=[r_wkv[wi], r_ckv], writes=[r_ps[pi]])
            si = next_stg()
            copy_scaled(evac_engine(), stg[si][:, :], psum[pi][:, :], 1.0, [r_ps[pi]], [r_stg[si]])
            for j in range(4):
                tt = tq * 4 + j
                pb.dma("sp", dst_v[tt * 128:(tt + 1) * 128, hs, :], stg[si][:, j * 128:(j + 1) * 128], reads=[r_stg[si]])
    st["nps"] = 8


TOK = 8192
NCH = 128


def p2_consts():
    import ml_dtypes
    s = np.arange(64)
    mask64 = (s[None, :] >= s[:, None]).astype(np.float32)
    k = np.arange(128)
    mask128 = ((k[:, None] // 64) <= (k[None, :] // 64)).astype(np.float32)
    tri = (k[:, None] < k[None, :]).astype(np.float32)
    sel63 = np.zeros((64, 128), np.float32); sel63[63, :] = 1.0
    return {"c_mask64": mask64, "c_mask128": mask128, "c_tri": tri, "c_sel63": sel63, "c_ident": np.eye(128, dtype=np.float32)}


def build_p2():
    nc = bass.Bass("TRN2", target_bir_lowering=False)
    p2in = nc.dram_tensor("p2in", [8, P1N], BF16, kind="ExternalInput").ap()
    p2g = nc.dram_tensor("p2g", [8, 2, 1024], F32, kind="ExternalInput").ap()
    mlb = nc.dram_tensor("mlb", [1, 2], F32, kind="ExternalInput").ap()
    mlg = nc.dram_tensor("mlg", [1, 256], F32, kind="ExternalInput").ap()
    cst = {k: nc.dram_tensor(k, list(v.shape), F32, kind="ExternalInput").ap() for k, v in p2_consts().items()}
    p2out = nc.dram_tensor("p2out", [8, 512, 1024], BF16, kind="ExternalOutput").ap()
    scr = nc.dram_tensor("p2scr", [4, TOK], F32).ap()
    with ExitStack() as es:
        pb = PB(nc, es)
        emit_p2(pb, p2in, p2g, mlb, mlg, cst, p2out, scr)
        pb.barrier()
        print("p2 instructions", pb.n_inst)
    return nc


def emit_p2(pb, p2in, p2g, mlb, mlg, cst, p2out, scr):
    import os
    nc = pb.nc
    outer_es = pb.es
    psum = [pb.ps("ps%d" % i, [128, 512]) for i in range(7)]
    r_ps = [Res("ps%d" % i, excl=True) for i in range(7)]
    psTb = pb.ps("psTb", [128, 1024], BF16); r_psTb = Res("psTb", excl=True)
    ident = pb.sb("ident", [128, 128], BF16); identf = pb.sb("identf", [128, 128], F32); r_id = Res("ident")
    mask128 = pb.sb("mask128", [128, 128], BF16); r_m128 = Res("m128")
    pb.dma("sp", identf[:, :], cst["c_ident"], writes=[r_id])
    pb.op("dve", lambda e: e.tensor_copy(out=ident[:, :], in_=identf[:, :]), [r_id], [r_id])
    tmpf = pb.sb("tmpf", [128, 128], F32); r_tmpf = Res("tmpf")
    pb.dma("sp", tmpf[:, :], cst["c_mask128"], writes=[r_tmpf])
    pb.op("dve", lambda e: e.tensor_copy(out=mask128[:, :], in_=tmpf[:, :]), [r_tmpf], [r_m128])
    ostg = [pb.sb("ostg%d" % i, [128, 512], BF16) for i in range(2)]; r_ostg = [Res("ostg0"), Res("ostg1")]
    cnt = {"ostg": 0}

    def V(fn, r, w): return pb.op("dve", fn, r, w)
    def A(fn, r, w): return pb.op("act", fn, r, w)
    def G(fn, r, w): return pb.op("pool", fn, r, w)
    def T(fn, r, w): return pb.op("pe", fn, r, w)

    def seg(j, name, n):
        return p2in[j, OFF[name]:OFF[name] + n]

    if os.environ.get("P2_SKIP_ML") != "1":
      with ExitStack() as es2:
        pb.es = es2
        qT = pb.sb("qT", [128, TOK], BF16); kT = pb.sb("kT", [128, TOK], BF16); r_qk = Res("qk")
        k_c = pb.sb("k_c", [64, NCH, 128], BF16); r_kc = Res("kc")
        v_aug = pb.sb("v_aug", [64, NCH, 257], BF16); r_v = Res("v")
        og = [pb.sb("og%d" % i, [64, 8, 256], BF16) for i in range(2)]; r_og = [Res("og0"), Res("og1")]
        mu_bc = pb.sb("mu_bc", [64, TOK], F32); r_mubc = Res("mubc")
        g_bc = pb.sb("g_bc", [64, 256], F32); r_gbc = Res("gbc")
        mask64 = pb.sb("mask64", [64, 64], F32); tri = pb.sb("tri", [128, 128], F32); sel63 = pb.sb("sel63", [64, 128], F32); r_c = Res("consts")
        bia = pb.sb("bia", [128, 2], F32); r_bia = Res("bia")
        for j in range(8):
            pb.dma("sp", qT[:, j * 1024:(j + 1) * 1024], seg(j, "mlqT", 131072).rearrange("(d t) -> d t", t=1024), writes=[r_qk])
            pb.dma("sp", kT[:, j * 1024:(j + 1) * 1024], seg(j, "mlkT", 131072).rearrange("(d t) -> d t", t=1024), writes=[r_qk])
            pb.dma("sp", k_c[:, j * 16:(j + 1) * 16, :], seg(j, "mlk", 131072).rearrange("(c p d) -> p c d", p=64, d=128), writes=[r_kc])
            pb.dma("sp", v_aug[:, j * 16:(j + 1) * 16, 0:256], seg(j, "mlv", 262144).rearrange("(c p d) -> p c d", p=64, d=256), writes=[r_v])
        G(lambda e: e.memset(v_aug[:, :, 256:257], 1.0), [], [r_v])
        pb.dma("sp", g_bc[:, :], mlg[0:1, :].partition_broadcast(64), writes=[r_gbc])
        pb.dma("sp", mask64[:, :], cst["c_mask64"], writes=[r_c])
        pb.dma("sp", tri[:, :], cst["c_tri"], writes=[r_c])
        pb.dma("sp", sel63[:, :], cst["c_sel63"], writes=[r_c])
        pb.dma("sp", bia[:, :], mlb[0:1, :].partition_broadcast(128), writes=[r_bia])

        def cm(name): return pb.sb(name, [128, 64], F32)
        ig = cm("ig"); fg = cm("fg"); lf = cm("lf"); Bc = cm("Bc"); Gc = cm("Gc"); mu = cm("mu"); onescm = cm("onescm"); zcm = cm("zcm")
        r_ig, r_fg, r_lf, r_B, r_G, r_mu, r_1 = Res("ig"), Res("fg"), Res("lf"), Res("B"), Res("G"), Res("mu"), Res("ones")
        sm = pb.sb("sm", [128, 8], F32); r_sm = Res("sm")
        rowt = pb.sb("rowt", [1, 256], F32); r_row = Res("row")
        for j in range(8):
            pb.dma("sp", ig[j * 16:(j + 1) * 16, :], p2g[j, 0, :].rearrange("(c i) -> c i", i=64), writes=[r_ig])
            pb.dma("sp", fg[j * 16:(j + 1) * 16, :], p2g[j, 1, :].rearrange("(c i) -> c i", i=64), writes=[r_fg])
        G(lambda e: e.memset(onescm[:, :], 1.0), [], [r_1])
        G(lambda e: e.memset(zcm[:, :], 0.0), [], [r_1])
        V(lambda e: e.tensor_scalar(out=bia[:, :], in0=bia[:, :], scalar1=1.0 / 15.0, scalar2=None, op0=ALU.mult), [r_bia], [r_bia])
        A(lambda e: e.activation(out=ig[:, :], in_=ig[:, :], func=AF.Tanh, scale=1.0 / 15.0, bias=bia[:, 0:1]), [r_ig, r_bia], [r_ig])
        A(lambda e: e.activation(out=fg[:, :], in_=fg[:, :], func=AF.Tanh, scale=1.0 / 15.0, bias=bia[:, 1:2]), [r_fg, r_bia], [r_fg])
        V(lambda e: e.tensor_scalar(out=ig[:, :], in0=ig[:, :], scalar1=15.0, scalar2=None, op0=ALU.mult), [r_ig], [r_ig])
        A(lambda e: e.activation(out=lf[:, :], in_=fg[:, :], func=AF.Exp, scale=-15.0), [r_fg], [r_lf])
        A(lambda e: e.activation(out=lf[:, :], in_=lf[:, :], func=AF.Ln, scale=1.0, bias=1.0), [r_lf], [r_lf])
        V(lambda e: e.tensor_scalar(out=lf[:, :], in0=lf[:, :], scalar1=-1.0, scalar2=None, op0=ALU.mult), [r_lf], [r_lf])
        V(lambda e: e.tensor_tensor_scan(out=Bc[:, :], data0=onescm[:, :], data1=lf[:, :], initial=0.0, op0=ALU.mult, op1=ALU.add), [r_lf, r_1], [r_B])
        T(lambda e: e.matmul(psum[0][:, 0:1], lhsT=tri[:, :], rhs=Bc[:, 63:64], start=True, stop=True), [r_c, r_B], [r_ps[0]])
        V(lambda e: e.tensor_copy(out=sm[:, 0:1], in_=psum[0][:, 0:1]), [r_ps[0]], [r_sm])
        V(lambda e: e.tensor_scalar(out=Bc[:, :], in0=Bc[:, :], scalar1=sm[:, 0:1], scalar2=None, op0=ALU.add), [r_B, r_sm], [r_B])
        V(lambda e: e.tensor_tensor(out=Gc[:, :], in0=ig[:, :], in1=Bc[:, :], op=ALU.subtract), [r_ig, r_B], [r_G])
        V(lambda e: e.tensor_tensor_scan(out=mu[:, :], data0=Gc[:, :], data1=zcm[:, :], initial=0.0, op0=ALU.max, op1=ALU.add), [r_G, r_1], [r_mu])
        T(lambda e: e.matmul(psum[0][0:1, 0:128], lhsT=mu[:, 63:64], rhs=identf[:, :], start=True, stop=True), [r_mu, r_id], [r_ps[0]])
        V(lambda e: e.tensor_copy(out=rowt[0:1, 0:128], in_=psum[0][0:1, 0:128]), [r_ps[0]], [r_row])
        V(lambda e: e.tensor_tensor_scan(out=rowt[0:1, 128:256], data0=rowt[0:1, 0:128], data1=zcm[0:1, 0:64].to_broadcast([1, 128]) if False else rowt[0:1, 0:128], initial=0.0, op0=ALU.max, op1=ALU.max), [r_row], [r_row])
        V(lambda e: e.tensor_copy(out=rowt[0:1, 1:128], in_=rowt[0:1, 128:255]), [r_row], [r_row])
        V(lambda e: e.memset(rowt[0:1, 0:1], 0.0), [r_row], [r_row])
        T(lambda e: e.matmul(psum[0][:, 4:5], lhsT=rowt[0:1, 0:128], rhs=onescm[0:1, 0:1], start=True, stop=True), [r_row, r_1], [r_ps[0]])
        V(lambda e: e.tensor_copy(out=sm[:, 1:2], in_=psum[0][:, 4:5]), [r_ps[0]], [r_sm])
        V(lambda e: e.tensor_scalar(out=mu[:, :], in0=mu[:, :], scalar1=sm[:, 1:2], scalar2=None, op0=ALU.max), [r_mu, r_sm], [r_mu])
        r_scr = Res("scr")
        pb.dma("sp", scr[0, :].rearrange("(c i) -> c i", i=64), mu[:, :], reads=[r_mu], writes=[r_scr])
        pb.dma("sp", mu_bc[:, :], scr[0:1, :].partition_broadcast(64), reads=[r_scr], writes=[r_mubc])
        def col(name): return pb.sb(name, [64, 128], F32)
        G_col = col("G_col"); mu_col = col("mu_col"); B_col = col("B_col"); muend = col("muend"); muprev = col("muprev")
        winter = col("winter"); emt = col("emt"); ws = col("ws")
        wprev = pb.sb("wprev", [128, 128], F32); muend128 = pb.sb("muend128", [128, 128], F32); muprev128 = pb.sb("muprev128", [128, 128], F32)
        r_col = Res("cols")
        for i, (src, dst) in enumerate(((Gc, G_col), (mu, mu_col), (Bc, B_col))):
            T(lambda e: e.matmul(psum[1][0:64, i * 128:(i + 1) * 128], lhsT=src[:, :], rhs=identf[:, :], is_transpose=True, start=True, stop=True), [r_G, r_mu, r_B, r_id], [r_ps[1]])
            V(lambda e: e.tensor_copy(out=dst[:, :], in_=psum[1][0:64, i * 128:(i + 1) * 128]), [r_ps[1]], [r_col])
        T(lambda e: e.matmul(psum[2][:, 0:128], lhsT=sel63[:, :], rhs=mu_col[:, :], start=True, stop=True), [r_c, r_col], [r_ps[2]])
        V(lambda e: e.tensor_copy(out=muend128[:, :], in_=psum[2][:, 0:128]), [r_ps[2]], [r_col])
        V(lambda e: e.memset(muprev128[:, 0:1], 0.0), [], [r_col])
        V(lambda e: e.tensor_copy(out=muprev128[:, 1:128], in_=muend128[:, 0:127]), [r_col], [r_col])
        V(lambda e: e.tensor_tensor(out=winter[:, :], in0=muprev128[0:64, :], in1=mu_col[:, :], op=ALU.subtract), [r_col], [r_col])
        A(lambda e: e.activation(out=winter[:, :], in_=winter[:, :], func=AF.Exp), [r_col], [r_col])
        V(lambda e: e.tensor_tensor(out=emt[:, :], in0=B_col[:, :], in1=mu_col[:, :], op=ALU.add), [r_col], [r_col])
        A(lambda e: e.activation(out=emt[:, :], in_=emt[:, :], func=AF.Exp, scale=-1.0), [r_col], [r_col])
        V(lambda e: e.tensor_tensor(out=ws[:, :], in0=G_col[:, :], in1=muend128[0:64, :], op=ALU.subtract), [r_col], [r_col])
        A(lambda e: e.activation(out=ws[:, :], in_=ws[:, :], func=AF.Exp), [r_col], [r_col])
        V(lambda e: e.tensor_tensor(out=wprev[:, :], in0=muprev128[:, :], in1=muend128[:, :], op=ALU.subtract), [r_col], [r_col])
        A(lambda e: e.activation(out=wprev[:, :], in_=wprev[:, :], func=AF.Exp), [r_col], [r_col])

        CT = pb.sb("CT", [128, 257], F32); CTb = pb.sb("CTb", [128, 257], BF16); r_CT = Res("CT"); r_CTb = Res("CTb")
        V(lambda e: e.memset(CT[:, :], 0.0), [], [r_CT])
        V(lambda e: e.memset(CTb[:, :], 0.0), [], [r_CTb])
        NB = 3
        ET = [pb.sb("ET%d" % i, [64, 64], F32) for i in range(NB)]; r_ET = [Res("ET%d" % i) for i in range(NB)]
        PT = [pb.sb("PT%d" % i, [64, 64], BF16) for i in range(NB)]; r_PT = [Res("PT%d" % i) for i in range(NB)]
        kw = [pb.sb("kw%d" % i, [64, 128], BF16) for i in range(NB)]; r_kw = [Res("kw%d" % i) for i in range(NB)]
        hin = [pb.sb("hin%d" % i, [64, 257], F32) for i in range(NB)]; r_hin = [Res("hin%d" % i) for i in range(NB)]
        hn = [pb.sb("hn%d" % i, [64, 257], F32) for i in range(NB)]; r_hn = [Res("hn%d" % i) for i in range(NB)]
        sc = [pb.sb("sc%d" % i, [64, 4], F32) for i in range(NB)]; r_sc = [Res("sc%d" % i) for i in range(NB)]
        hsq = [pb.sb("hsq%d" % i, [64, 256], F32) for i in range(NB)]; r_hsq = [Res("hsq%d" % i) for i in range(NB)]
        og2 = [pb.sb("ogg%d" % i, [64, 256], F32) for i in range(NB)]; r_og2 = [Res("ogg%d" % i) for i in range(NB)]
        yb = [pb.sb("yb%d" % i, [64, 256], BF16) for i in range(NB)]; r_yb = [Res("yb%d" % i) for i in range(NB)]
        psT = [psTb]
        r_psT = [r_psTb]
        ngrp = NCH // 8
        def stage_a(j):
            b = j % NB
            t0 = j * 64
            sb_ = 2 if j % 2 == 0 else 6
            T(lambda e: e.matmul(psum[sb_][0:64, 0:64], lhsT=kT[:, t0:t0 + 64], rhs=qT[:, t0:t0 + 64], start=True, stop=True), [r_qk], [r_ps[sb_]])
            A(lambda e: e.activation(out=ET[b][:, :], in_=mu_bc[:, t0:t0 + 64], func=AF.Exp, scale=-1.0, bias=G_col[:, j:j + 1]), [r_mubc, r_col], [r_ET[b]])
            G(lambda e: e.tensor_tensor(out=ET[b][:, :], in0=ET[b][:, :], in1=mask64[:, :], op=ALU.mult), [r_ET[b], r_c], [r_ET[b]])
            V(lambda e: e.tensor_tensor(out=PT[b][:, :], in0=psum[sb_][0:64, 0:64], in1=ET[b][:, :], op=ALU.mult), [r_ps[sb_], r_ET[b]], [r_PT[b]])
            G(lambda e: e.tensor_scalar(out=kw[b][:, :], in0=k_c[:, j, :], scalar1=ws[:, j:j + 1], scalar2=None, op0=ALU.mult), [r_kc, r_col], [r_kw[b]])

        def stage_b(j):
            b = j % NB
            t0 = j * 64
            T(lambda e: e.matmul(psum[3][0:64, 0:257], lhsT=PT[b][:, :], rhs=v_aug[:, j, :], start=True, stop=True), [r_PT[b], r_v], [r_ps[3]])
            T(lambda e: e.matmul(psum[4][0:64, 0:257], lhsT=qT[:, t0:t0 + 64], rhs=CTb[:, :], start=True, stop=True), [r_qk, r_CTb], [r_ps[4]])
            V(lambda e: e.tensor_copy(out=hin[b][:, :], in_=psum[3][0:64, 0:257]), [r_ps[3]], [r_hin[b]])
            V(lambda e: e.scalar_tensor_tensor(out=hn[b][:, :], in0=psum[4][0:64, 0:257], scalar=winter[:, j:j + 1], in1=hin[b][:, :], op0=ALU.mult, op1=ALU.add),
              [r_ps[4], r_col, r_hin[b]], [r_hn[b]])
            T(lambda e: e.matmul(psum[5][:, 0:257], lhsT=kw[b][:, :], rhs=v_aug[:, j, :], start=True, stop=True), [r_kw[b], r_v], [r_ps[5]])
            V(lambda e: e.scalar_tensor_tensor(out=CT[:, :], in0=CT[:, :], scalar=wprev[:, j:j + 1], in1=psum[5][:, 0:257], op0=ALU.mult, op1=ALU.add),
              [r_CT, r_col, r_ps[5]], [r_CT])
            G(lambda e: e.tensor_copy(out=CTb[:, :], in_=CT[:, :]), [r_CT], [r_CTb])

        def stage_c1(j):
            b = j % NB
            V(lambda e: e.tensor_scalar(out=sc[b][:, 3:4], in0=hn[b][:, 256:257], scalar1=-1.0, scalar2=None, op0=ALU.mult), [r_hn[b]], [r_sc[b]])
            V(lambda e: e.tensor_tensor(out=sc[b][:, 0:1], in0=sc[b][:, 3:4], in1=hn[b][:, 256:257], op=ALU.max), [r_hn[b], r_sc[b]], [r_sc[b]])
            V(lambda e: e.tensor_tensor(out=sc[b][:, 0:1], in0=sc[b][:, 0:1], in1=emt[:, j:j + 1], op=ALU.max), [r_sc[b], r_col], [r_sc[b]])
            V(lambda e: e.reciprocal(out=sc[b][:, 0:1], in_=sc[b][:, 0:1]), [r_sc[b]], [r_sc[b]])
            V(lambda e: e.tensor_scalar(out=hn[b][:, 0:256], in0=hn[b][:, 0:256], scalar1=sc[b][:, 0:1], scalar2=None, op0=ALU.mult), [r_hn[b], r_sc[b]], [r_hn[b]])
            V(lambda e: e.scalar_tensor_tensor(out=hsq[b][:, :], in0=hn[b][:, 0:256], scalar=1.0, in1=hn[b][:, 0:256], op0=ALU.mult, op1=ALU.mult, accum_out=sc[b][:, 1:2]), [r_hn[b]], [r_hsq[b], r_sc[b]])
            A(lambda e: e.activation(out=sc[b][:, 2:3], in_=sc[b][:, 1:2], func=AF.Ln, scale=1.0 / 256.0, bias=1e-6), [r_sc[b]], [r_sc[b]])
            A(lambda e: e.activation(out=sc[b][:, 2:3], in_=sc[b][:, 2:3], func=AF.Exp, scale=-0.5), [r_sc[b]], [r_sc[b]])
            oi = (j // 8) % 2; ci = j % 8
            G(lambda e: e.tensor_tensor(out=og2[b][:, :], in0=og[oi][:, ci, :], in1=g_bc[:, :], op=ALU.mult), [r_og[oi], r_gbc], [r_og2[b]])

        def stage_c2(j):
            b = j % NB
            ci = j % 8
            V(lambda e: e.scalar_tensor_tensor(out=yb[b][:, :], in0=hn[b][:, 0:256], scalar=sc[b][:, 2:3], in1=og2[b][:, :], op0=ALU.mult, op1=ALU.mult),
              [r_hn[b], r_sc[b], r_og2[b]], [r_yb[b]])
            for half in range(2):
                T(lambda e: e.matmul(psT[0][:, half * 256 + (ci % 4) * 64: half * 256 + (ci % 4) * 64 + 64], lhsT=yb[b][:, half * 128:(half + 1) * 128], rhs=ident[0:64, 0:64], is_transpose=True, start=True, stop=True),
                  [r_yb[b], r_id], [r_psT[0]])
            if ci % 4 == 3:
                oi2 = cnt["ostg"] % 2; cnt["ostg"] += 1
                V(lambda e: e.tensor_copy(out=ostg[oi2][:, :], in_=psT[0][:, 0:512]), [r_psT[0]], [r_ostg[oi2]])
                tb = (j - 3) * 64
                jb2 = tb // 1024; tl = tb % 1024
                for half in range(2):
                    pb.dma("sp", p2out[jb2, half * 128:(half + 1) * 128, tl:tl + 256], ostg[oi2][:, half * 256:(half + 1) * 256], reads=[r_ostg[oi2]])

        for grp in range(ngrp):
            oi = grp % 2
            jb = grp // 2
            cc0 = (grp % 2) * 8
            pb.dma("sp", og[oi][:, :, :], seg(jb, "mlo", 262144).rearrange("(c p d) -> p c d", p=64, d=256)[:, cc0:cc0 + 8, :], writes=[r_og[oi]])
            for ci in range(8):
                j = grp * 8 + ci
                if j == 0:
                    stage_a(0)
                if j + 1 < NCH:
                    stage_a(j + 1)
                stage_b(j)
                if j >= 1:
                    stage_c1(j - 1)
                if j >= 2:
                    stage_c2(j - 2)
        stage_c1(NCH - 1)
        stage_c2(NCH - 2)
        stage_c2(NCH - 1)
        pb.barrier()
      pb.es = outer_es

    if os.environ.get("P2_SKIP_MLA") == "1":
        return
    scale = 192 ** -0.5
    qA = pb.sb("qA", [128, TOK], BF16); qB = pb.sb("qB", [65, TOK], BF16); kA = pb.sb("kA", [128, TOK], BF16); kB = pb.sb("kB", [65, TOK], BF16)
    r_q = Res("q"); r_k = Res("k")
    va = pb.sb("va", [128, 64, 129], BF16); r_va = Res("va")
    sq = [pb.sb("sq%d" % i, [128, 512], BF16) for i in range(2)]; r_sq = [Res("sq0"), Res("sq1")]
    onesb = pb.sb("onesb", [128, 1], BF16); r_ob = Res("onesb")
    nrow = pb.sb("nrow", [1, TOK], BF16); r_nrow = Res("nrow")
    sc1 = pb.sb("sca", [1, 8], F32); r_sc1 = Res("sc1")
    rt = pb.sb("rt", [1, 512], F32); r_rt = Res("rt")
    PTm = [pb.sb("PTm%d" % i, [128, 512], BF16) for i in range(3)]; r_PTm = [Res("PTm%d" % i) for i in range(3)]
    ob = [pb.sb("ob%d" % i, [128, 128], BF16) for i in range(2)]; r_ob2 = [Res("ob0"), Res("ob1")]
    rc = [pb.sb("rc%d" % i, [128, 1], F32) for i in range(2)]; r_rc = [Res("rc0"), Res("rc1")]
    psT2 = psTb; r_psT2 = r_psTb
    G(lambda e: e.memset(onesb[:, :], 1.0), [], [r_ob])
    for hs in range(2):
        for j in range(8):
            src_q = seg(j, "qT", 2 * 192 * 1024).rearrange("(h d t) -> h d t", h=2, t=1024)
            src_k = seg(j, "kT", 2 * 128 * 1024).rearrange("(h d t) -> h d t", h=2, t=1024)
            pb.dma("sp", qA[:, j * 1024:(j + 1) * 1024], src_q[hs, 0:128, :], writes=[r_q])
            pb.dma("sp", qB[0:64, j * 1024:(j + 1) * 1024], src_q[hs, 128:192, :], writes=[r_q])
            pb.dma("sp", kA[:, j * 1024:(j + 1) * 1024], src_k[hs, :, :], writes=[r_k])
            pb.dma("sp", kB[0:64, j * 1024:(j + 1) * 1024], seg(j, "krT", 65536).rearrange("(d t) -> d t", t=1024), writes=[r_k])
            pb.dma("sp", va[:, j * 8:(j + 1) * 8, 0:128], seg(j, "v", 262144).rearrange("(c p h d) -> p c h d", p=128, h=2, d=128)[:, :, hs, :], writes=[r_va])
        G(lambda e: e.memset(va[:, :, 128:129], 1.0), [], [r_va])
        G(lambda e: e.memset(kB[64:65, :], 1.0), [], [r_k])
        V(lambda e: e.memset(sc1[:, 0:1], 0.0), [], [r_sc1])
        idx = 0
        for blk in range(16):
            for (srcA, srcB, r_src) in ((kA, kB, r_k),):
                i0 = idx % 2; idx += 1
                A(lambda e: e.activation(out=sq[i0][:, :], in_=srcA[:, blk * 512:(blk + 1) * 512], func=AF.Square), [r_src], [r_sq[i0]])
                i1 = idx % 2; idx += 1
                V(lambda e: e.tensor_tensor(out=sq[i1][0:64, :], in0=srcB[0:64, blk * 512:(blk + 1) * 512], in1=srcB[0:64, blk * 512:(blk + 1) * 512], op=ALU.mult), [r_src], [r_sq[i1]])
                T(lambda e: e.matmul(psum[0][0:1, :], lhsT=onesb[:, 0:1], rhs=sq[i0][:, :], start=True, stop=False), [r_ob, r_sq[i0]], [r_ps[0]])
                T(lambda e: e.matmul(psum[0][0:1, :], lhsT=onesb[0:64, 0:1], rhs=sq[i1][0:64, :], start=False, stop=True), [r_ob, r_sq[i1]], [r_ps[0]])
                V(lambda e: e.tensor_reduce(out=sc1[:, 1:2], in_=psum[0][0:1, :], axis=AX.X, op=ALU.max), [r_ps[0]], [r_sc1])
                V(lambda e: e.tensor_tensor(out=sc1[:, 0:1], in0=sc1[:, 0:1], in1=sc1[:, 1:2], op=ALU.max), [r_sc1], [r_sc1])
        A(lambda e: e.activation(out=sc1[:, 2:3], in_=sc1[:, 0:1], func=AF.Sqrt), [r_sc1], [r_sc1])
        V(lambda e: e.tensor_scalar(out=sc1[:, 2:3], in0=sc1[:, 2:3], scalar1=-1.0, scalar2=None, op0=ALU.mult), [r_sc1], [r_sc1])
        for blk in range(16):
            i0 = idx % 2; idx += 1
            A(lambda e: e.activation(out=sq[i0][:, :], in_=qA[:, blk * 512:(blk + 1) * 512], func=AF.Square), [r_q], [r_sq[i0]])
            i1 = idx % 2; idx += 1
            V(lambda e: e.tensor_tensor(out=sq[i1][0:64, :], in0=qB[0:64, blk * 512:(blk + 1) * 512], in1=qB[0:64, blk * 512:(blk + 1) * 512], op=ALU.mult), [r_q], [r_sq[i1]])
            T(lambda e: e.matmul(psum[0][0:1, :], lhsT=onesb[:, 0:1], rhs=sq[i0][:, :], start=True, stop=False), [r_ob, r_sq[i0]], [r_ps[0]])
            T(lambda e: e.matmul(psum[0][0:1, :], lhsT=onesb[0:64, 0:1], rhs=sq[i1][0:64, :], start=False, stop=True), [r_ob, r_sq[i1]], [r_ps[0]])
            A(lambda e: e.activation(out=rt[:, :], in_=psum[0][0:1, :], func=AF.Sqrt), [r_ps[0]], [r_rt])
            V(lambda e: e.tensor_scalar(out=nrow[0:1, blk * 512:(blk + 1) * 512], in0=rt[:, :], scalar1=sc1[:, 2:3], scalar2=None, op0=ALU.mult), [r_rt, r_sc1], [r_nrow])
        pb.dma("sp", qB[64:65, :], nrow[0:1, :], reads=[r_nrow], writes=[r_q])
        items = []
        for qb in range(16):
            for kt in range(4 * qb + 4):
                items.append((qb, kt))
        pend = []

        def geom(it, sidx):
            qb, kt = it
            jd = kt - 4 * qb
            i_lo = max(jd, 0)
            return qb, kt, jd, i_lo, qb * 512 + i_lo * 128, 512 - i_lo * 128, 1 + (sidx % 2), sidx % 3

        def emit_qk(it, sidx):
            qb, kt, jd, i_lo, qs, n, sp_, pt = geom(it, sidx)
            T(lambda e: e.matmul(psum[sp_][:, 0:n], lhsT=kA[:, kt * 128:(kt + 1) * 128], rhs=qA[:, qs:qs + n], start=True, stop=False), [r_k, r_q], [r_ps[sp_]])
            T(lambda e: e.matmul(psum[sp_][:, 0:n], lhsT=kB[0:65, kt * 128:(kt + 1) * 128], rhs=qB[0:65, qs:qs + n], start=False, stop=True), [r_k, r_q], [r_ps[sp_]])

        def emit_exp(it, sidx):
            qb, kt, jd, i_lo, qs, n, sp_, pt = geom(it, sidx)
            A(lambda e: e.activation(out=PTm[pt][:, 0:n], in_=psum[sp_][:, 0:n], func=AF.Exp, scale=scale), [r_ps[sp_]], [r_PTm[pt]])
            if jd >= 0:
                G(lambda e: e.tensor_tensor(out=PTm[pt][:, 0:128], in0=PTm[pt][:, 0:128], in1=mask128[:, :], op=ALU.mult), [r_PTm[pt], r_m128], [r_PTm[pt]])

        def flush():
            for (i, bi) in pend:
                T(lambda e: e.matmul(psT2[:, i * 128:(i + 1) * 128], lhsT=ob[bi][:, :], rhs=ident[:, :], is_transpose=True, start=True, stop=True), [r_ob2[bi], r_id], [r_psT2])
            del pend[:]

        def emit_pv(it, sidx):
            qb, kt, jd, i_lo, qs, n, sp_, pt = geom(it, sidx)
            flush()
            for i in range(i_lo, 4):
                last = 4 * qb + i
                T(lambda e: e.matmul(psum[3 + i][:, 0:129], lhsT=PTm[pt][:, (i - i_lo) * 128:(i - i_lo + 1) * 128], rhs=va[:, kt, :], start=(kt == 0), stop=(kt == last)),
                  [r_PTm[pt], r_va], [r_ps[3 + i]])
                if kt == last:
                    bi = i % 2
                    V(lambda e: e.reciprocal(out=rc[bi][:, :], in_=psum[3 + i][:, 128:129]), [r_ps[3 + i]], [r_rc[bi]])
                    V(lambda e: e.tensor_scalar(out=ob[bi][:, :], in0=psum[3 + i][:, 0:128], scalar1=rc[bi][:, 0:1], scalar2=None, op0=ALU.mult), [r_ps[3 + i], r_rc[bi]], [r_ob2[bi]])
                    pend.append((i, bi))
            if kt == 4 * qb + 3:
                flush()
                oi2 = cnt["ostg"] % 2; cnt["ostg"] += 1
                A(lambda e: e.activation(out=ostg[oi2][:, :], in_=psT2[:, 0:512], func=AF.Copy), [r_psT2], [r_ostg[oi2]])
                pb.dma("sp", p2out[qb // 2, 256 + hs * 128:256 + (hs + 1) * 128, (qb % 2) * 512:(qb % 2) * 512 + 512], ostg[oi2][:, :], reads=[r_ostg[oi2]])

        emit_qk(items[0], 0)
        for ii, it in enumerate(items):
            if ii + 1 < len(items):
                emit_qk(items[ii + 1], ii + 1)
            emit_exp(it, ii)
            emit_pv(it, ii)


ALPHA = 4.0 ** 0.25
NE = 32; CAP = 256; FE = 768
NROW = 1 + NE * CAP


def p3_consts():
    k = np.arange(128)
    tri = (k[:, None] < k[None, :]).astype(np.float32)
    iota = np.tile(np.arange(CAP, dtype=np.float32)[None, :], (128, 1))
    ebase = np.tile((np.arange(NE, dtype=np.float32) * CAP + 1)[None, :], (128, 1))
    return {"c_tri": tri, "c_ident": np.eye(128, dtype=np.float32), "c_iota": iota, "c_ebase": ebase}


def build_p3(last):
    nc = bass.Bass("TRN2", target_bir_lowering=False)
    def din(name, shape, dt=F32): return nc.dram_tensor(name, shape, dt, kind="ExternalInput").ap()
    a = dict(
        p3in=din("p3in", [8, 512, TPC], BF16), xres=din("xres", [TPC, D]),
        w_out=din("w_out", [D, D]), ln1_g=din("ln1_g", [1, D]), ln1_b=din("ln1_b", [1, D]),
        mem=din("mem", [256, D]), xg_mem=din("xg_mem", [1, D]), xb_mem=din("xb_mem", [1, D]),
        x_w_q=din("x_w_q", [D, 1024]), x_w_kv=din("x_w_kv", [D, 2048]), x_w_o=din("x_w_o", [1024, D]),
        ln2_g=din("ln2_g", [1, D]), ln2_b=din("ln2_b", [1, D]),
        w_router=din("w_router", [D, NE]), b_router=din("b_router", [1, NE]),
        w_gu=din("w_gu", [NE, D, 2 * FE]), b_gu=din("b_gu", [NE, 2 * FE]), w_down=din("w_down", [NE, FE, D]), b_down=din("b_down", [NE, D]),
        ln3_g=din("ln3_g", [1, D]), ln3_b=din("ln3_b", [1, D]),
    )
    cst = {k: din(k, list(v.shape)) for k, v in p3_consts().items()}
    x_out = nc.dram_tensor("x_out", [TPC, D], F32, kind="ExternalOutput").ap()
    xT_out = nc.dram_tensor("xT_out", [D, TPC], BF16, kind="ExternalOutput").ap()
    scr = dict(pre=nc.dram_tensor("s_pre", [TPC, D], F32).ap(), x1=nc.dram_tensor("s_x1", [TPC, D], F32).ap(), x2=nc.dram_tensor("s_x2", [TPC, D], F32).ap(),
               yall=nc.dram_tensor("s_yall", [NROW, D], BF16, kind="ExternalOutput").ap())
    if os.environ.get("P3DBG"):
        scr["dbg"] = nc.dram_tensor("dbg", [5, 128, 8, NE], F32, kind="ExternalOutput").ap()
        scr["dbgi"] = nc.dram_tensor("dbgi", [128, 8, 8], I32, kind="ExternalOutput").ap()
    with ExitStack() as es:
        pb = PB(nc, es)
        emit_p3(pb, a, cst, x_out, xT_out, scr)
        pb.barrier()
        print("p3 instructions", pb.n_inst)
    return nc


def emit_p3(pb, a, cst, x_out, xT_out, scr):
    nc = pb.nc
    outer_es = pb.es
    STAGE = int(os.environ.get("P3STAGE", "99"))
    psum = [pb.ps("ps%d" % i, [128, 512]) for i in range(7)]
    r_ps = [Res("ps%d" % i, excl=True) for i in range(7)]
    psTb = pb.ps("psTb", [128, 1024], BF16); r_psTb = Res("psTb", excl=True)
    ident = pb.sb("ident", [128, 128], BF16); identf = pb.sb("identf", [128, 128], F32); r_id = Res("ident")
    pb.dma("sp", identf[:, :], cst["c_ident"], writes=[r_id])
    pb.op("dve", lambda e: e.tensor_copy(out=ident[:, :], in_=identf[:, :]), [r_id], [r_id])
    bigA = pb.sb("bigA", [128, D], F32); bigB = pb.sb("bigB", [128, D], F32); bigC = pb.sb("bigC", [128, D], BF16)
    r_A, r_B, r_C = Res("bigA"), Res("bigB"), Res("bigC")
    act_flat = pb.sb("act_flat", [128, 32 * 1024], BF16); r_act = Res("act")
    actT = act_flat[:, :].rearrange("p (a b) -> p a b", b=1024)
    x2b = act_flat[:, :].rearrange("p (a b) -> p a b", b=D)
    st = {"ps": 0, "ev": 0}

    def V(fn, r, w): return pb.op("dve", fn, r, w)
    def A(fn, r, w): return pb.op("act", fn, r, w)
    def G(fn, r, w): return pb.op("pool", fn, r, w)
    def T(fn, r, w): return pb.op("pe", fn, r, w)

    def next_ps(n=6):
        i = st["ps"] % n; st["ps"] += 1
        return i

    def ev_eng():
        st["ev"] += 1
        return "act" if st["ev"] % 2 else "dve"

    def linear_res(w_ap, KT, ktmap, res_ap, pre_ap, wbuf, r_w, rtile, r_rt):
        w_v = w_ap.rearrange("(kt p) n -> p kt n", p=128)
        wi_c = 0
        ri_c = 0
        for cb in range(D // 256):
            wi = wi_c % len(wbuf); wi_c += 1
            ng = max(KT // 8, 1)
            for g in range(ng):
                pb.dma("pool", wbuf[wi][:, g * 8:(g + 1) * 8, :], w_v[:, g * 8:(g + 1) * 8, cb * 256:(cb + 1) * 256], writes=[r_w[wi]])
            for tt in range(8):
                pi = next_ps()
                for kt in range(KT):
                    T(lambda e, kt=kt: e.matmul(psum[pi][:, 0:256], lhsT=actT[:, kt, tt * 128:(tt + 1) * 128], rhs=wbuf[wi][:, ktmap(kt), :], start=(kt == 0), stop=(kt == KT - 1)),
                      [r_act, r_w[wi]], [r_ps[pi]])
                ri = ri_c % len(rtile); ri_c += 1
                pb.dma("sp", rtile[ri][:, :], res_ap[tt * 128:(tt + 1) * 128, cb * 256:(cb + 1) * 256], writes=[r_rt[ri]])
                V(lambda e: e.scalar_tensor_tensor(out=rtile[ri][:, :], in0=rtile[ri][:, :], scalar=float(ALPHA), in1=psum[pi][:, 0:256], op0=ALU.mult, op1=ALU.add),
                  [r_rt[ri], r_ps[pi]], [r_rt[ri]])
                pb.dma("sp", pre_ap[tt * 128:(tt + 1) * 128, cb * 256:(cb + 1) * 256], rtile[ri][:, :], reads=[r_rt[ri]])

    def ln_tile(gbc, bbc, r_gb, stats, mv, r_stat, eps=1e-5, buf=None, r_buf=None, split=False):
        bigA_ = bigA if buf is None else buf
        r_A_ = r_A if r_buf is None else r_buf
        for c in range(8):
            V(lambda e, c=c: e.bn_stats(out=stats[:, c, :], in_=bigA_[:, c * 512:(c + 1) * 512]), [r_A_], [r_stat])
        V(lambda e: e.bn_aggr(out=mv[:, 0:2], in_=stats[:, :, :]), [r_stat], [r_stat])
        A(lambda e: e.activation(out=mv[:, 2:3], in_=mv[:, 1:2], func=AF.Sqrt, bias=float(eps)), [r_stat], [r_stat])
        V(lambda e: e.reciprocal(out=mv[:, 2:3], in_=mv[:, 2:3]), [r_stat], [r_stat])
        V(lambda e: e.tensor_scalar(out=bigA_[:, :], in0=bigA_[:, :], scalar1=mv[:, 0:1], scalar2=mv[:, 2:3], op0=ALU.subtract, op1=ALU.mult), [r_A_, r_stat], [r_A_])
        G(lambda e: e.tensor_tensor(out=bigA_[:, :], in0=bigA_[:, :], in1=gbc[:, :], op=ALU.mult), [r_A_, r_gb], [r_A_])
        if not split:
            ln_tile_b(bbc, r_gb, bigA_, r_A_)

    def ln_tile_b(bbc, r_gb, buf, r_buf):
        V(lambda e: e.tensor_tensor(out=buf[:, :], in0=buf[:, :], in1=bbc[:, :], op=ALU.add), [r_buf, r_gb], [r_buf])

    def transposes_to(dst_fn, writes):
        for g in range(4):
            for k in range(8):
                kt = g * 8 + k
                T(lambda e: e.matmul(psTb[:, k * 128:(k + 1) * 128], lhsT=bigC[:, kt * 128:(kt + 1) * 128], rhs=ident[:, :], is_transpose=True, start=True, stop=True), [r_C, r_id], [r_psTb])
            en = ev_eng()
            if en == "act":
                A(lambda e: e.activation(out=dst_fn(g), in_=psTb[:, :].rearrange("p (k t) -> p k t", t=128), func=AF.Copy), [r_psTb], writes)
            else:
                V(lambda e: e.tensor_copy(out=dst_fn(g), in_=psTb[:, :].rearrange("p (k t) -> p k t", t=128)), [r_psTb], writes)

    def load_gb(gbc, bbc, r_gb, g_ap, b_ap):
        pb.dma("sp", gbc[:, :], g_ap[0:1, :].partition_broadcast(128), writes=[r_gb])
        pb.dma("sp", bbc[:, :], b_ap[0:1, :].partition_broadcast(128), writes=[r_gb])

    for c in range(8):
        pb.dma("sp", actT[:, c * 4:(c + 1) * 4, :], a["p3in"][c].rearrange("(k p) t -> p k t", p=128), writes=[r_act])
    with ExitStack() as es2:
        pb.es = es2
        wbuf = [pb.sb("wbuf%d" % i, [128, 32, 256], BF16) for i in range(3)]; r_w = [Res("w%d" % i) for i in range(3)]
        rtile = [pb.sb("rt%d" % i, [128, 256], F32) for i in range(4)]; r_rt = [Res("rt%d" % i) for i in range(4)]
        def ktmap(kt):
            c = kt // 4; h = (kt % 4) // 2; s = kt % 2
            return h * 16 + c * 2 + s
        linear_res(a["w_out"], 32, ktmap, a["xres"], scr["pre"], wbuf, r_w, rtile, r_rt)
        pb.barrier()
    with ExitStack() as es2:
        pb.es = es2
        gbc = pb.sb("gbc", [128, D], F32); bbc = pb.sb("bbc", [128, D], F32); r_gb = Res("gb")
        stats = pb.sb("stats", [128, 8, 6], F32); mv = pb.sb("mv", [128, 4], F32); r_stat = Res("stat")
        load_gb(gbc, bbc, r_gb, a["ln1_g"], a["ln1_b"])
        stats_b = pb.sb("stats_b", [128, 8, 6], F32); mv_b = pb.sb("mv_b", [128, 4], F32); r_stat_b = Res("stat_b")
        def ln1_s1(tt):
            buf, r_buf = (bigA, r_A) if tt % 2 == 0 else (bigB, r_B)
            st_, mv_, rs_ = (stats, mv, r_stat) if tt % 2 == 0 else (stats_b, mv_b, r_stat_b)
            pb.dma("sp", buf[:, :], scr["pre"][tt * 128:(tt + 1) * 128, :], writes=[r_buf])
            ln_tile(gbc, bbc, r_gb, st_, mv_, rs_, buf=buf, r_buf=r_buf, split=True)

        def ln1_s2(tt):
            buf, r_buf = (bigA, r_A) if tt % 2 == 0 else (bigB, r_B)
            ln_tile_b(bbc, r_gb, buf, r_buf)
            pb.dma("sp", scr["x1"][tt * 128:(tt + 1) * 128, :], buf[:, :], reads=[r_buf])
            A(lambda e: e.activation(out=bigC[:, :], in_=buf[:, :], func=AF.Copy), [r_buf], [r_C])
            transposes_to(lambda g: actT[:, g * 8:(g + 1) * 8, tt * 128:(tt + 1) * 128], [r_act])

        ln1_s1(0)
        for tt in range(8):
            if tt + 1 < 8:
                ln1_s1(tt + 1)
            ln1_s2(tt)
        pb.barrier()
    pb.es = outer_es
    if STAGE < 2:
        for tt in range(8):
            pb.dma("sp", bigA[:, :], scr["x1"][tt * 128:(tt + 1) * 128, :], writes=[r_A])
            pb.dma("sp", x_out[tt * 128:(tt + 1) * 128, :], bigA[:, :], reads=[r_A])
        return

    with ExitStack() as es2:
        pb.es = es2
        wbuf = [pb.sb("xwbuf%d" % i, [128, 32, 256], BF16) for i in range(2)]; r_w = [Res("w0"), Res("w1")]
        rtile = [pb.sb("xrt%d" % i, [128, 256], F32) for i in range(2)]; r_rt = [Res("rt%d" % i) for i in range(2)]
        mem_nT = pb.sb("mem_nT", [128, 32, 256], BF16); r_mn = Res("mem_nT")
        kmT = pb.sb("kmT", [128, 8, 256], BF16); vm = pb.sb("vm", [128, 2, 1024], BF16); r_kv = Res("kvm")
        qxT = pb.sb("qxT", [128, 8, 1024], BF16); r_qx = Res("qxT")
        oxT = pb.sb("oxT", [128, 8, 1024], BF16); r_ox = Res("oxT")
        stats = pb.sb("xstats", [128, 8, 6], F32); mv = pb.sb("xmv", [128, 4], F32); r_stat = Res("stat")
        gbc = bigB; r_gb = r_B
        bbc = pb.sb("xbbc", [128, D], BF16); r_bb = Res("xbbc")
        pb.dma("sp", gbc[:, :], a["xg_mem"][0:1, :].partition_broadcast(128), writes=[r_gb])
        pb.dma("pool", bbc[:, :], a["xb_mem"][0:1, :].partition_broadcast(128), writes=[r_bb])
        for mt in range(2):
            pb.dma("sp", bigA[:, :], a["mem"][mt * 128:(mt + 1) * 128, :], writes=[r_A])
            for c in range(8):
                V(lambda e, c=c: e.bn_stats(out=stats[:, c, :], in_=bigA[:, c * 512:(c + 1) * 512]), [r_A], [r_stat])
            V(lambda e: e.bn_aggr(out=mv[:, 0:2], in_=stats[:, :, :]), [r_stat], [r_stat])
            A(lambda e: e.activation(out=mv[:, 2:3], in_=mv[:, 1:2], func=AF.Sqrt, bias=1e-5), [r_stat], [r_stat])
            V(lambda e: e.reciprocal(out=mv[:, 2:3], in_=mv[:, 2:3]), [r_stat], [r_stat])
            V(lambda e: e.tensor_scalar(out=bigA[:, :], in0=bigA[:, :], scalar1=mv[:, 0:1], scalar2=mv[:, 2:3], op0=ALU.subtract, op1=ALU.mult), [r_A, r_stat], [r_A])
            G(lambda e: e.tensor_tensor(out=bigA[:, :], in0=bigA[:, :], in1=gbc[:, :], op=ALU.mult), [r_A, r_gb], [r_A])
            V(lambda e: e.tensor_tensor(out=bigC[:, :], in0=bigA[:, :], in1=bbc[:, :], op=ALU.add), [r_A, r_bb], [r_C])
            transposes_to(lambda g: mem_nT[:, g * 8:(g + 1) * 8, mt * 128:(mt + 1) * 128], [r_mn])
        wkv_v = a["x_w_kv"].rearrange("(kt p) n -> p kt n", p=128)
        wi_c = 0
        for cb in range(8):
            wi = wi_c % 2; wi_c += 1
            for g in range(4):
                pb.dma("pool", wbuf[wi][:, g * 8:(g + 1) * 8, :], wkv_v[:, g * 8:(g + 1) * 8, cb * 256:(cb + 1) * 256], writes=[r_w[wi]])
            if cb < 4:
                for m in range(2):
                    pi = next_ps()
                    for kt in range(32):
                        T(lambda e, kt=kt: e.matmul(psum[pi][:, 0:256], lhsT=wbuf[wi][:, kt, m * 128:(m + 1) * 128], rhs=mem_nT[:, kt, :], start=(kt == 0), stop=(kt == 31)), [r_w[wi], r_mn], [r_ps[pi]])
                    A(lambda e: e.activation(out=kmT[:, cb * 2 + m, :], in_=psum[pi][:, 0:256], func=AF.Copy), [r_ps[pi]], [r_kv])
            else:
                for mt in range(2):
                    pi = next_ps()
                    for kt in range(32):
                        T(lambda e, kt=kt: e.matmul(psum[pi][:, 0:256], lhsT=mem_nT[:, kt, mt * 128:(mt + 1) * 128], rhs=wbuf[wi][:, kt, :], start=(kt == 0), stop=(kt == 31)), [r_w[wi], r_mn], [r_ps[pi]])
                    V(lambda e: e.tensor_copy(out=vm[:, mt, (cb - 4) * 256:(cb - 3) * 256], in_=psum[pi][:, 0:256]), [r_ps[pi]], [r_kv])
        wq_v = a["x_w_q"].rearrange("(kt p) n -> p kt n", p=128)
        for cb in range(4):
            wi = wi_c % 2; wi_c += 1
            for g in range(4):
                pb.dma("pool", wbuf[wi][:, g * 8:(g + 1) * 8, :], wq_v[:, g * 8:(g + 1) * 8, cb * 256:(cb + 1) * 256], writes=[r_w[wi]])
            for m in range(2):
                for th in range(2):
                    pi = next_ps()
                    for kt in range(32):
                        T(lambda e, kt=kt: e.matmul(psum[pi][:, :], lhsT=wbuf[wi][:, kt, m * 128:(m + 1) * 128], rhs=actT[:, kt, th * 512:(th + 1) * 512], start=(kt == 0), stop=(kt == 31)), [r_w[wi], r_act], [r_ps[pi]])
                    if ev_eng() == "act":
                        A(lambda e: e.activation(out=qxT[:, cb * 2 + m, th * 512:(th + 1) * 512], in_=psum[pi][:, :], func=AF.Copy), [r_ps[pi]], [r_qx])
                    else:
                        V(lambda e: e.tensor_copy(out=qxT[:, cb * 2 + m, th * 512:(th + 1) * 512], in_=psum[pi][:, :]), [r_ps[pi]], [r_qx])
        xs = 256 ** -0.5
        Pm = [pb.sb("Pm%d" % i, [128, 256], BF16) for i in range(2)]; r_Pm = [Res("Pm0"), Res("Pm1")]
        Pe = [pb.sb("Pe%d" % i, [128, 256], F32) for i in range(2)]; r_Pe = [Res("Pe0"), Res("Pe1")]
        PTx = [pb.sb("PTx%d" % i, [128, 2, 128], BF16) for i in range(2)]; r_PTx = [Res("PTx0"), Res("PTx1")]
        sm = [pb.sb("xsm%d" % i, [128, 4], F32) for i in range(2)]; r_sm = [Res("xsm0"), Res("xsm1")]
        it = 0
        for h in range(4):
            for tt in range(8):
                b = it % 2; it += 1
                pi = next_ps()
                for dk in range(2):
                    T(lambda e, dk=dk: e.matmul(psum[pi][:, 0:256], lhsT=qxT[:, 2 * h + dk, tt * 128:(tt + 1) * 128], rhs=kmT[:, 2 * h + dk, :], start=(dk == 0), stop=(dk == 1)), [r_qx, r_kv], [r_ps[pi]])
                V(lambda e: e.tensor_reduce(out=sm[b][:, 0:1], in_=psum[pi][:, 0:256], axis=AX.X, op=ALU.max), [r_ps[pi]], [r_sm[b]])
                V(lambda e: e.tensor_scalar(out=sm[b][:, 0:1], in0=sm[b][:, 0:1], scalar1=-xs, scalar2=None, op0=ALU.mult), [r_sm[b]], [r_sm[b]])
                A(lambda e: e.activation(out=Pe[b][:, :], in_=psum[pi][:, 0:256], func=AF.Exp, scale=xs, bias=sm[b][:, 0:1], accum_out=sm[b][:, 1:2]), [r_ps[pi], r_sm[b]], [r_Pe[b], r_sm[b]])
                V(lambda e: e.reciprocal(out=sm[b][:, 2:3], in_=sm[b][:, 1:2]), [r_sm[b]], [r_sm[b]])
                V(lambda e: e.tensor_scalar(out=Pm[b][:, :], in0=Pe[b][:, :], scalar1=sm[b][:, 2:3], scalar2=None, op0=ALU.mult), [r_Pe[b], r_sm[b]], [r_Pm[b]])
                for mt in range(2):
                    T(lambda e, mt=mt: e.matmul(psTb[:, mt * 128:(mt + 1) * 128], lhsT=Pm[b][:, mt * 128:(mt + 1) * 128], rhs=ident[:, :], is_transpose=True, start=True, stop=True), [r_Pm[b], r_id], [r_psTb])
                V(lambda e: e.tensor_copy(out=PTx[b][:, :, :], in_=psTb[:, 0:256].rearrange("p (k t) -> p k t", t=128)), [r_psTb], [r_PTx[b]])
                pi2 = next_ps()
                for dvh in range(2):
                    for mt in range(2):
                        T(lambda e, mt=mt, dvh=dvh: e.matmul(psum[pi2][:, dvh * 128:(dvh + 1) * 128], lhsT=vm[:, mt, h * 256 + dvh * 128:h * 256 + (dvh + 1) * 128], rhs=PTx[b][:, mt, :], start=(mt == 0), stop=(mt == 1)),
                          [r_kv, r_PTx[b]], [r_ps[pi2]])
                A(lambda e: e.activation(out=oxT[:, 2 * h:2 * h + 2, tt * 128:(tt + 1) * 128], in_=psum[pi2][:, 0:256].rearrange("p (k t) -> p k t", t=128), func=AF.Copy), [r_ps[pi2]], [r_ox])
        V(lambda e: e.tensor_copy(out=actT[:, 0:8, :], in_=oxT[:, :, :]), [r_ox, r_qx], [r_act])
        linear_res(a["x_w_o"], 8, lambda kt: kt, scr["x1"], scr["pre"], wbuf, r_w, rtile, r_rt)
        pb.barrier()
    pb.es = outer_es

    logit = pb.sb("logit", [128, 8, NE], F32); maskt = pb.sb("maskt", [128, 8, NE], F32); gate = pb.sb("gate", [128, 8, NE], F32)
    post = pb.sb("post", [128, 8, NE], F32); maskb = pb.sb("maskb", [128, 8, NE], BF16); top8 = pb.sb("top8", [128, 8, 8], F32)
    r_rt_ = Res("routing")
    with ExitStack() as es2:
        pb.es = es2
        gbc = pb.sb("gbc2", [128, D], F32); bbc = pb.sb("bbc2", [128, D], F32); r_gb = Res("gb")
        stats = pb.sb("stats2", [128, 8, 6], F32); mv = pb.sb("mv2", [128, 4], F32); r_stat = Res("stat")
        load_gb(gbc, bbc, r_gb, a["ln2_g"], a["ln2_b"])
        wr = pb.sb("wr", [128, 32, NE], F32); r_wr = Res("wr")
        pb.dma("sp", wr[:, :, :], a["w_router"].rearrange("(kt p) e -> p kt e", p=128), writes=[r_wr])
        brt = pb.sb("brt", [128, NE], F32)
        pb.dma("sp", brt[:, :], a["b_router"][0:1, :].partition_broadcast(128), writes=[r_wr])
        x2Tf = bigB[:, :].rearrange("p (k t) -> p k t", t=128)
        for tt in range(8):
            pb.dma("sp", bigA[:, :], scr["pre"][tt * 128:(tt + 1) * 128, :], writes=[r_A])
            ln_tile(gbc, bbc, r_gb, stats, mv, r_stat)
            pb.dma("sp", scr["x2"][tt * 128:(tt + 1) * 128, :], bigA[:, :], reads=[r_A])
            A(lambda e: e.activation(out=x2b[:, tt, :], in_=bigA[:, :], func=AF.Copy), [r_A], [r_act])
            for g in range(8):
                pi = next_ps()
                for k in range(4):
                    kt = g * 4 + k
                    T(lambda e: e.matmul(psum[pi][:, k * 128:(k + 1) * 128], lhsT=bigA[:, kt * 128:(kt + 1) * 128], rhs=identf[:, :], is_transpose=True, start=True, stop=True), [r_A, r_id], [r_ps[pi]])
                if ev_eng() == "act":
                    A(lambda e: e.activation(out=x2Tf[:, g * 4:(g + 1) * 4, :], in_=psum[pi][:, :].rearrange("p (k t) -> p k t", t=128), func=AF.Copy), [r_ps[pi]], [r_B])
                else:
                    V(lambda e: e.tensor_copy(out=x2Tf[:, g * 4:(g + 1) * 4, :], in_=psum[pi][:, :].rearrange("p (k t) -> p k t", t=128)), [r_ps[pi]], [r_B])
            pi = next_ps()
            for kt in range(32):
                T(lambda e, kt=kt: e.matmul(psum[pi][:, 0:NE], lhsT=x2Tf[:, kt, :], rhs=wr[:, kt, :], start=(kt == 0), stop=(kt == 31)), [r_B, r_wr], [r_ps[pi]])
            V(lambda e: e.tensor_tensor(out=logit[:, tt, :], in0=psum[pi][:, 0:NE], in1=brt[:, :], op=ALU.add), [r_ps[pi], r_wr], [r_rt_])
            V(lambda e: e.max(out=top8[:, tt, :], in_=logit[:, tt, :]), [r_rt_], [r_rt_])
            V(lambda e: e.tensor_scalar(out=maskt[:, tt, :], in0=logit[:, tt, :], scalar1=top8[:, tt, 3:4], scalar2=None, op0=ALU.is_ge), [r_rt_], [r_rt_])
            V(lambda e: e.tensor_scalar(out=mv[:, 3:4], in0=top8[:, tt, 0:1], scalar1=-1.0, scalar2=None, op0=ALU.mult), [r_rt_, r_stat], [r_stat])
            A(lambda e: e.activation(out=gate[:, tt, :], in_=logit[:, tt, :], func=AF.Exp, bias=mv[:, 3:4]), [r_rt_, r_stat], [r_rt_])
            V(lambda e: e.tensor_tensor(out=gate[:, tt, :], in0=gate[:, tt, :], in1=maskt[:, tt, :], op=ALU.mult), [r_rt_], [r_rt_])
            V(lambda e: e.tensor_reduce(out=mv[:, 3:4], in_=gate[:, tt, :], axis=AX.X, op=ALU.add), [r_rt_, r_stat], [r_stat])
            V(lambda e: e.reciprocal(out=mv[:, 3:4], in_=mv[:, 3:4]), [r_stat], [r_stat])
            V(lambda e: e.tensor_scalar(out=gate[:, tt, :], in0=gate[:, tt, :], scalar1=mv[:, 3:4], scalar2=None, op0=ALU.mult), [r_rt_, r_stat], [r_rt_])
            V(lambda e: e.tensor_copy(out=maskb[:, tt, :], in_=maskt[:, tt, :]), [r_rt_], [r_rt_])
        pb.barrier()
    pb.es = outer_es
    if STAGE < 3:
        for tt in range(8):
            pb.dma("sp", bigA[:, :], scr["x2"][tt * 128:(tt + 1) * 128, :], writes=[r_A])
            pb.dma("sp", x_out[tt * 128:(tt + 1) * 128, :], bigA[:, :], reads=[r_A])
        return

    idx = pb.sb("idx", [128, 8, 8], I32); r_idx = Res("idx")
    gT = pb.sb("gT", [NE, TPC], F32); r_gT = Res("gT")
    with ExitStack() as es2:
        pb.es = es2
        trib = pb.sb("trib", [128, 128], BF16); onesb = pb.sb("onesb", [128, 128], BF16); r_cb = Res("cb")
        trif = bigA[:, 0:128]
        pb.dma("sp", trif, cst["c_tri"], writes=[r_A])
        V(lambda e: e.tensor_copy(out=trib[:, :], in_=trif), [r_A], [r_cb])
        G(lambda e: e.memset(onesb[:, :], 1.0), [], [r_cb])
        iota = pb.sb("iota", [128, CAP], F32); ebase = pb.sb("ebase", [128, NE], F32)
        pb.dma("sp", iota[:, :], cst["c_iota"], writes=[r_cb])
        pb.dma("sp", ebase[:, :], cst["c_ebase"], writes=[r_cb])
        for tt in range(8):
            pi = next_ps()
            T(lambda e: e.matmul(psum[pi][:, 0:NE], lhsT=trib[:, :], rhs=maskb[:, tt, :], start=True, stop=(tt == 0)), [r_cb, r_rt_], [r_ps[pi]])
            for t2 in range(tt):
                T(lambda e, t2=t2: e.matmul(psum[pi][:, 0:NE], lhsT=onesb[:, :], rhs=maskb[:, t2, :], start=False, stop=(t2 == tt - 1)), [r_cb, r_rt_], [r_ps[pi]])
            V(lambda e: e.tensor_copy(out=post[:, tt, :], in_=psum[pi][:, 0:NE]), [r_ps[pi]], [r_rt_])
        valid = pb.sb("valid", [128, 8, NE], F32); ghl = pb.sb("ghl", [128, 8, NE, 2], BF16); gtmp = pb.sb("gtmp", [128, 8, NE], F32)
        V(lambda e: e.tensor_scalar(out=valid[:, :, :], in0=post[:, :, :], scalar1=float(CAP), scalar2=None, op0=ALU.is_lt), [r_rt_], [r_rt_])
        V(lambda e: e.tensor_tensor(out=valid[:, :, :], in0=valid[:, :, :], in1=maskt[:, :, :], op=ALU.mult), [r_rt_], [r_rt_])
        V(lambda e: e.tensor_copy(out=ghl[:, :, :, 0], in_=gate[:, :, :]), [r_rt_], [r_rt_])
        V(lambda e: e.tensor_tensor(out=gtmp[:, :, :], in0=gate[:, :, :], in1=ghl[:, :, :, 0], op=ALU.subtract), [r_rt_], [r_rt_])
        V(lambda e: e.tensor_copy(out=ghl[:, :, :, 1], in_=gtmp[:, :, :]), [r_rt_], [r_rt_])
        bgu_nat = bigB[0:NE, 0:2 * FE]
        pb.dma("sp", bgu_nat, a["b_gu"], writes=[r_B])
        bgu = pb.sb("bgu", [128, 12, NE], F32); r_bgu = Res("bgu")
        for f in range(12):
            pi = next_ps()
            T(lambda e: e.matmul(psum[pi][:, 0:NE], lhsT=bigB[0:NE, f * 128:(f + 1) * 128], rhs=identf[0:NE, 0:NE], is_transpose=True, start=True, stop=True), [r_B, r_id], [r_ps[pi]])
            V(lambda e: e.tensor_copy(out=bgu[:, f, :], in_=psum[pi][:, 0:NE]), [r_ps[pi]], [r_bgu])
        zrow = pb.sb("zrow", [128, 32], BF16); r_z = Res("zrow"); r_yall = Res("yall")
        G(lambda e: e.memset(zrow[:, :], 0.0), [], [r_z])
        pb.dma("sp", scr["yall"][0, :].rearrange("(p f) -> p f", f=32), zrow[:, :], reads=[r_z], writes=[r_yall])

        wgu = [pb.sb("wgu%d" % i, [128, 32, 256], BF16) for i in range(3)]; r_wgu = [Res("wgu%d" % i) for i in range(3)]
        wdn = [pb.sb("wdn%d" % i, [128, 6, 512], BF16) for i in range(3)]; r_wdn = [Res("wdn%d" % i) for i in range(3)]
        sel_all = [pb.sb("sel%d" % i, [128, CAP], BF16) for i in range(16)]; r_sel_all = [Res("sel%d" % i) for i in range(16)]
        xgT = bigB[:, :].bitcast(BF16).rearrange("p (k c) -> p k c", c=CAP); r_xg = r_B
        hT = pb.sb("hT", [128, 6, CAP], BF16); r_hT = Res("hT")
        gact = pb.sb("gact", [128, 6, CAP], F32); r_ga = Res("gact")
        tg = [pb.sb("tg%d" % i, [128, CAP], F32) for i in range(2)]; r_tg = [Res("tg0"), Res("tg1")]
        gsl = pb.sb("gsl", [128, 2, 2], F32); gs1 = pb.sb("gs1", [128, 2], F32); r_gs = Res("gs")
        ystg = [bigC[:, i * 512:(i + 1) * 512] for i in range(4)]; r_ys = [Res("ys%d" % i) for i in range(4)]
        cnt = {"wgu": 0, "wdn": 0, "ys": 0, "tg": 0}
        NEX = int(os.environ.get("P3NEX", str(NE)))
        for ex in range(NEX):
            sel = sel_all[(ex % 2) * 8:(ex % 2) * 8 + 8]; r_sel = r_sel_all[(ex % 2) * 8:(ex % 2) * 8 + 8]
            for tt in range(8):
                V(lambda e, tt=tt: e.tensor_scalar(out=sel[tt][:, :], in0=iota[:, :], scalar1=post[:, tt, ex:ex + 1], scalar2=valid[:, tt, ex:ex + 1], op0=ALU.is_equal, op1=ALU.mult),
                  [r_cb, r_rt_], [r_sel[tt]])
            for kt in range(32):
                pi = next_ps()
                for tt in range(8):
                    T(lambda e, tt=tt: e.matmul(psum[pi][:, 0:CAP], lhsT=x2b[:, tt, kt * 128:(kt + 1) * 128], rhs=sel[tt][:, :], start=(tt == 0), stop=(tt == 7)), [r_act, r_sel[tt]], [r_ps[pi]])
                if ev_eng() == "act":
                    A(lambda e: e.activation(out=xgT[:, kt, :], in_=psum[pi][:, 0:CAP], func=AF.Copy), [r_ps[pi]], [r_xg])
                else:
                    V(lambda e: e.tensor_copy(out=xgT[:, kt, :], in_=psum[pi][:, 0:CAP]), [r_ps[pi]], [r_xg])
            pi = next_ps()
            for stl in range(2):
                for tt in range(8):
                    T(lambda e, tt=tt: e.matmul(psum[pi][:, stl * 2:stl * 2 + 2], lhsT=sel[tt][:, stl * 128:(stl + 1) * 128], rhs=ghl[:, tt, ex, :], start=(tt == 0), stop=(tt == 7)), [r_sel[tt], r_rt_], [r_ps[pi]])
            V(lambda e: e.tensor_copy(out=gsl[:, :, :], in_=psum[pi][:, 0:4].rearrange("p (a b) -> p a b", b=2)), [r_ps[pi]], [r_gs])
            V(lambda e: e.tensor_tensor(out=gs1[:, :], in0=gsl[:, :, 0], in1=gsl[:, :, 1], op=ALU.add), [r_gs], [r_gs])
            wgu_v = a["w_gu"][ex].rearrange("(kt p) n -> p kt n", p=128)
            for blk in range(6):
                wi = cnt["wgu"] % 3; cnt["wgu"] += 1
                for g in range(4):
                    pb.dma("pool", wgu[wi][:, g * 8:(g + 1) * 8, :], wgu_v[:, g * 8:(g + 1) * 8, blk * 256:(blk + 1) * 256], writes=[r_wgu[wi]])
                for m in range(2):
                    f = blk * 2 + m
                    pi = next_ps()
                    for kt in range(32):
                        T(lambda e, kt=kt: e.matmul(psum[pi][:, 0:CAP], lhsT=wgu[wi][:, kt, m * 128:(m + 1) * 128], rhs=xgT[:, kt, :], start=(kt == 0), stop=(kt == 31)), [r_wgu[wi], r_xg], [r_ps[pi]])
                    ti = cnt["tg"] % 2; cnt["tg"] += 1
                    if f < 6:
                        V(lambda e: e.tensor_scalar(out=gact[:, f, :], in0=psum[pi][:, 0:CAP], scalar1=bgu[:, f, ex:ex + 1], scalar2=7.0, op0=ALU.add, op1=ALU.min), [r_ps[pi], r_bgu], [r_ga])
                        A(lambda e: e.activation(out=tg[ti][:, :], in_=gact[:, f, :], func=AF.Sigmoid, scale=1.702), [r_ga], [r_tg[ti]])
                        V(lambda e: e.tensor_tensor(out=gact[:, f, :], in0=gact[:, f, :], in1=tg[ti][:, :], op=ALU.mult), [r_ga, r_tg[ti]], [r_ga])
                    else:
                        V(lambda e: e.tensor_scalar(out=tg[ti][:, :], in0=psum[pi][:, 0:CAP], scalar1=bgu[:, f, ex:ex + 1], scalar2=7.0, op0=ALU.add, op1=ALU.min), [r_ps[pi], r_bgu], [r_tg[ti]])
                        V(lambda e: e.tensor_scalar(out=tg[ti][:, :], in0=tg[ti][:, :], scalar1=-7.0, scalar2=1.0, op0=ALU.max, op1=ALU.add), [r_tg[ti]], [r_tg[ti]])
                        V(lambda e: e.tensor_tensor(out=hT[:, f - 6, :], in0=tg[ti][:, :], in1=gact[:, f - 6, :], op=ALU.mult), [r_tg[ti], r_ga], [r_hT])
            wdn_v = a["w_down"][ex].rearrange("(fk p) n -> p fk n", p=128)
            for cb in range(8):
                wi = cnt["wdn"] % 3; cnt["wdn"] += 1
                pb.dma("pool", wdn[wi][:, :, :], wdn_v[:, :, cb * 512:(cb + 1) * 512], writes=[r_wdn[wi]])
                for stl in range(2):
                    pi = next_ps()
                    for fk in range(6):
                        T(lambda e, fk=fk: e.matmul(psum[pi][:, :], lhsT=hT[:, fk, stl * 128:(stl + 1) * 128], rhs=wdn[wi][:, fk, :], start=(fk == 0), stop=(fk == 5)), [r_hT, r_wdn[wi]], [r_ps[pi]])
                    yi = cnt["ys"] % 4; cnt["ys"] += 1
                    if True:
                        V(lambda e: e.tensor_scalar(out=ystg[yi], in0=psum[pi][:, :], scalar1=gs1[:, stl:stl + 1], scalar2=None, op0=ALU.mult), [r_ps[pi], r_gs], [r_ys[yi]])
                    r0 = 1 + ex * CAP + stl * 128
                    pb.dma("sp", scr["yall"][r0:r0 + 128, cb * 512:(cb + 1) * 512], ystg[yi], reads=[r_ys[yi]])
        pb.barrier()

        key = gtmp
        V(lambda e: e.tensor_copy(out=key[:, :, :], in_=post[:, :, :]), [r_rt_], [r_rt_])
        for tt in range(8):
            V(lambda e, tt=tt: e.tensor_tensor(out=key[:, tt, :], in0=key[:, tt, :], in1=ebase[:, :], op=ALU.add), [r_rt_, r_cb], [r_rt_])
        V(lambda e: e.tensor_tensor(out=key[:, :, :], in0=key[:, :, :], in1=valid[:, :, :], op=ALU.mult), [r_rt_], [r_rt_])
        for tt in range(8):
            V(lambda e, tt=tt: e.max(out=top8[:, tt, :], in_=key[:, tt, :]), [r_rt_], [r_rt_])
        V(lambda e: e.tensor_copy(out=idx[:, :, :], in_=top8[:, :, :]), [r_rt_], [r_idx])
        for tt in range(8):
            pi = next_ps()
            T(lambda e, tt=tt: e.matmul(psum[pi][0:NE, 0:128], lhsT=gate[:, tt, :], rhs=identf[:, :], is_transpose=True, start=True, stop=True), [r_rt_, r_id], [r_ps[pi]])
            V(lambda e, tt=tt: e.tensor_copy(out=gT[:, tt * 128:(tt + 1) * 128], in_=psum[pi][0:NE, 0:128]), [r_ps[pi]], [r_gT])
        pb.barrier()
        if "dbg" in scr:
            for i, t_ in enumerate((post, valid, gate, key, maskt)):
                pb.dma("sp", scr["dbg"][i], t_[:, :, :], reads=[r_rt_])
            pb.dma("sp", scr["dbgi"], idx[:, :, :], reads=[r_idx])
    pb.es = outer_es
    with ExitStack() as es2:
        pb.es = es2
        gbc = pb.sb("gbc3", [128, D], F32); bbc = pb.sb("bbc3", [128, D], F32); r_gb = Res("gb")
        stats = pb.sb("stats3", [128, 8, 6], F32); mv = pb.sb("mv3", [128, 4], F32); r_stat = Res("stat")
        load_gb(gbc, bbc, r_gb, a["ln3_g"], a["ln3_b"])
        bdn = pb.sb("bdn", [NE, D], F32); r_bdn = Res("bdn")
        pb.dma("sp", bdn[:, :], a["b_down"], writes=[r_bdn])
        gat = [pb.sb("gat%d" % i, [128, D], BF16) for i in range(4)]; r_gat = [Res("gat%d" % i) for i in range(4)]
        stats_b3 = pb.sb("stats_b3", [128, 8, 6], F32); mv_b3 = pb.sb("mv_b3", [128, 4], F32); r_stat_b3 = Res("stat_b3")
        xTs = pb.sb("xTs", [128, 32, 128], BF16); r_xTs = Res("xTs")
        def ln3_s1(tt):
            for k in range(4):
                pb.dma("pool", None, None, reads=[r_idx], writes=[r_gat[k]],
                       fn=lambda e, k=k: e.indirect_dma_start(out=gat[k][:, :], out_offset=None, in_=scr["yall"][:, :], in_offset=bass.IndirectOffsetOnAxis(ap=idx[:, tt, k:k + 1], axis=0)))
            buf, r_buf = (bigA, r_A) if tt % 2 == 0 else (bigB, r_B)
            st_, mv_, rs_ = (stats, mv, r_stat) if tt % 2 == 0 else (stats_b3, mv_b3, r_stat_b3)
            pb.dma("sp", buf[:, :], scr["x2"][tt * 128:(tt + 1) * 128, :], writes=[r_buf])
            A(lambda e: e.activation(out=buf[:, :], in_=buf[:, :], func=AF.Copy, scale=float(ALPHA)), [r_buf], [r_buf])
            for k in range(4):
                V(lambda e, k=k: e.tensor_tensor(out=buf[:, :], in0=buf[:, :], in1=gat[k][:, :], op=ALU.add), [r_buf, r_gat[k]], [r_buf])
            for cb in range(8):
                pi = next_ps()
                T(lambda e: e.matmul(psum[pi][:, :], lhsT=gT[:, tt * 128:(tt + 1) * 128], rhs=bdn[:, cb * 512:(cb + 1) * 512], start=True, stop=True), [r_gT, r_bdn], [r_ps[pi]])
                V(lambda e: e.tensor_tensor(out=buf[:, cb * 512:(cb + 1) * 512], in0=buf[:, cb * 512:(cb + 1) * 512], in1=psum[pi][:, :], op=ALU.add), [r_buf, r_ps[pi]], [r_buf])
            if os.environ.get("P3DBG") != "pre3":
                ln_tile(gbc, bbc, r_gb, st_, mv_, rs_, buf=buf, r_buf=r_buf, split=True)

        def ln3_s2(tt):
            buf, r_buf = (bigA, r_A) if tt % 2 == 0 else (bigB, r_B)
            if os.environ.get("P3DBG") != "pre3":
                ln_tile_b(bbc, r_gb, buf, r_buf)
            pb.dma("sp", x_out[tt * 128:(tt + 1) * 128, :], buf[:, :], reads=[r_buf])
            A(lambda e: e.activation(out=bigC[:, :], in_=buf[:, :], func=AF.Copy), [r_buf], [r_C])
            transposes_to(lambda g: xTs[:, g * 8:(g + 1) * 8, :], [r_xTs])
            pb.dma("sp", xT_out.rearrange("(kt p) t -> p kt t", p=128)[:, :, tt * 128:(tt + 1) * 128], xTs[:, :, :], reads=[r_xTs])

        ln3_s1(0)
        for tt in range(8):
            if tt + 1 < 8:
                ln3_s1(tt + 1)
            ln3_s2(tt)
    pb.es = outer_es


def _r1(v):
    return np.ascontiguousarray(np.asarray(v, np.float32)).reshape(1, -1)


def kernel(**inp):
    f32 = np.float32
    g = lambda k: np.asarray(inp[k])
    x = g('x').astype(f32)[0]
    mem = np.ascontiguousarray(g('mem').astype(f32)[0])
    pos = g('positions').astype(np.int32).reshape(1, -1)
    cores = list(range(8))
    c1 = p1_consts(); c2 = p2_consts(); c3 = p3_consts()
    xres = [np.ascontiguousarray(x[c * TPC:(c + 1) * TPC]) for c in cores]
    xT = [np.ascontiguousarray(xres[c].T) for c in cores]
    for l in range(2):
        nc1 = build_p1(x_is_bf16=(l > 0))
        maps = [dict(xT=xT[c], w_in=g('w_in')[l], w_uq=g('mla_w_uq')[l], w_ukv=g('mla_w_ukv')[l], g_q=g('mla_g_q')[l], g_kv=g('mla_g_kv')[l],
                     pos=np.ascontiguousarray(pos[:, c * TPC:(c + 1) * TPC]), **c1) for c in cores]
        r1 = run_bass_kernel_spmd(nc1, maps, core_ids=cores).results
        p1out = np.stack([np.asarray(r["p1out"]) for r in r1])
        p1g = np.stack([np.asarray(r["p1g"]) for r in r1])
        del r1
        nc2 = build_p2()
        maps = [dict(p2in=np.ascontiguousarray(p1out[:, c]), p2g=np.ascontiguousarray(p1g[:, c]),
                     mlb=np.array([[g('ml_b_i')[l, c], g('ml_b_f')[l, c]]], f32), mlg=_r1(g('ml_norm_g')[l, c * 256:(c + 1) * 256]), **c2) for c in cores]
        r2 = run_bass_kernel_spmd(nc2, maps, core_ids=cores).results
        p2out = np.stack([np.asarray(r["p2out"]) for r in r2])
        del r2, p1out
        nc3 = build_p3(l == 1)
        maps = [dict(p3in=np.ascontiguousarray(p2out[:, j]), xres=xres[j], w_out=g('w_out')[l], ln1_g=_r1(g('ln1_g')[l]), ln1_b=_r1(g('ln1_b')[l]),
                     mem=mem, xg_mem=_r1(g('x_g_mem')[l]), xb_mem=_r1(g('x_b_mem')[l]), x_w_q=g('x_w_q')[l], x_w_kv=g('x_w_kv')[l], x_w_o=g('x_w_o')[l],
                     ln2_g=_r1(g('ln2_g')[l]), ln2_b=_r1(g('ln2_b')[l]), w_router=g('w_router')[l], b_router=_r1(g('b_router')[l]),
                     w_gu=g('w_gu')[l], b_gu=g('b_gu')[l], w_down=g('w_down')[l], b_down=g('b_down')[l], ln3_g=_r1(g('ln3_g')[l]), ln3_b=_r1(g('ln3_b')[l]), **c3)
                for j in cores]
        r3 = run_bass_kernel_spmd(nc3, maps, core_ids=cores).results
        xres = [np.asarray(r["x_out"]) for r in r3]
        xT = [np.asarray(r["xT_out"]) for r in r3]
        del r3, p2out
    return np.concatenate(xres, 0)[None].astype(np.float32)
```

```python
import math, os
import os
import numpy as np
from contextlib import ExitStack
import concourse.bass as bass
import concourse.mybir as mybir
from concourse.bass_utils import run_bass_kernel_spmd

F32 = mybir.dt.float32
BF16 = mybir.dt.bfloat16
I32 = mybir.dt.int32
U32 = mybir.dt.uint32
AF = mybir.ActivationFunctionType
ALU = mybir.AluOpType
AX = mybir.AxisListType


class Res:
    __slots__ = ("name", "ws", "rs", "excl", "wdma")

    def __init__(self, name="", excl=False):
        self.name = name
        self.excl = excl
        self.ws = {}
        self.rs = {}
        self.wdma = False


class PB:
    NDMA = 16

    def __init__(self, nc, es):
        self.nc = nc
        self.es = es
        self.eng = {"pe": nc.tensor, "act": nc.scalar, "dve": nc.vector, "pool": nc.gpsimd, "sp": nc.sync}
        self.sem = {k: es.enter_context(nc.semaphore("prog_" + k)) for k in self.eng}
        self.cnt = {k: 0 for k in self.eng}
        self.seen = {k: {} for k in self.eng}
        self.dsem = {}
        self.dval = {}
        self.dnext = {}
        for q in ("sp", "act", "pool"):
            self.dsem[q] = [es.enter_context(nc.semaphore("dma_%s_%d" % (q, i))) for i in range(self.NDMA)]
            self.dval[q] = [0] * self.NDMA
            self.dnext[q] = 0
        self.n_inst = 0
        self.inorder = set(os.environ.get("PB_INORDER", "").split(",")) if "os" in globals() else set()

    def sb(self, name, shape, dt):
        return self.es.enter_context(self.nc.sbuf_tensor(name, shape, dt))

    def ps(self, name, shape, dt=F32):
        return self.es.enter_context(self.nc.psum_tensor(name, shape, dt))

    def _wait(self, en, tok):
        key, sem, val = tok
        if self.seen[en].get(key, 0) >= val:
            return
        self.eng[en].wait_ge(sem, val)
        if getattr(self, 'log', None) is not None: self.log.append((en, 'wait', key, val))
        self.seen[en][key] = val
        self.n_inst += 1

    def _deps(self, en, reads, writes, is_dma=False):
        toks = []
        for r in reads:
            toks.extend(r.ws.values())
        for w in writes:
            if is_dma and w.wdma and not w.rs:
                continue
            toks.extend(w.ws.values())
            toks.extend(w.rs.values())
        for t in toks:
            if t[0] == en and (en == "pe" or en in self.inorder):
                continue
            self._wait(en, t)

    def _commit(self, tok, reads, writes, is_dma=False):
        for r in reads:
            old = r.rs.get(tok[0])
            if old is None or old[2] < tok[2]:
                r.rs[tok[0]] = tok
        for w in writes:
            if is_dma and w.wdma and not w.rs:
                old = w.ws.get(tok[0])
                if old is None or old[2] < tok[2]:
                    w.ws[tok[0]] = tok
            else:
                w.ws = {tok[0]: tok}
                w.rs = {}
                w.wdma = is_dma

    def op(self, en, fn, reads=(), writes=()):
        ex = [r for r in reads if r.excl]
        if ex:
            reads = [r for r in reads if not r.excl]
            writes = list(writes) + ex
        self._deps(en, reads, writes)
        inst = fn(self.eng[en])
        self.cnt[en] += 1
        if getattr(self, 'log', None) is not None: self.log.append((en, 'op', self.cnt[en], [r.name for r in reads], [w.name for w in writes]))
        inst.then_inc(self.sem[en], 1)
        self.n_inst += 1
        self._commit((en, self.sem[en], self.cnt[en]), reads, writes)
        return inst

    def dma(self, q, out, in_, reads=(), writes=(), fn=None, **kw):
        self._deps(q, reads, writes, is_dma=True)
        i = self.dnext[q]
        self.dnext[q] = (i + 1) % self.NDMA
        sem = self.dsem[q][i]
        key = "d_%s_%d" % (q, i)
        if self.dval[q][i] > 0:
            self._wait(q, (key, sem, self.dval[q][i]))
        if fn is not None:
            inst = fn(self.eng[q])
        else:
            inst = self.eng[q].dma_start(out=out, in_=in_, **kw)
        self.dval[q][i] += 16
        inst.then_inc(sem, 16)
        self.n_inst += 1
        self._commit((key, sem, self.dval[q][i]), reads, writes, is_dma=True)
        return inst

    def barrier(self):
        toks = [(k, self.sem[k], self.cnt[k]) for k in self.eng if self.cnt[k] > 0]
        for q in self.dsem:
            for i in range(self.NDMA):
                if self.dval[q][i] > 0:
                    toks.append(("d_%s_%d" % (q, i), self.dsem[q][i], self.dval[q][i]))
        for en in self.eng:
            for t in toks:
                if t[0] == en:
                    continue
                self._wait(en, t)

D = 4096; TPC = 1024

KT = 32
NW = 256
OFF = {}
_o = 0
for _n, _sz in [("mlqT", 128 * 1024), ("mlkT", 128 * 1024), ("mlk", 1024 * 128), ("mlv", 1024 * 256), ("mlo", 1024 * 256),
                ("qT", 2 * 192 * 1024), ("kT", 2 * 128 * 1024), ("krT", 64 * 1024), ("v", 1024 * 256)]:
    OFF[_n] = _o; _o += _sz
P1N = _o
ROPE_INV = (10000.0 ** (-np.arange(32, dtype=np.float32) / 32)).astype(np.float32)


def p1_consts():
    inv = np.concatenate([ROPE_INV, ROPE_INV]).reshape(64, 1).astype(np.float32)
    sgn = np.concatenate([-np.ones(32), np.ones(32)]).reshape(64, 1).astype(np.float32)
    return {"c_inv": inv, "c_sgn": sgn}


def build_p1(x_is_bf16=False):
    nc = bass.Bass("TRN2", target_bir_lowering=False)
    dt_x = BF16 if x_is_bf16 else F32
    xT_d = nc.dram_tensor("xT", [D, TPC], dt_x, kind="ExternalInput").ap()
    w_in = nc.dram_tensor("w_in", [D, 7760], F32, kind="ExternalInput").ap()
    w_uq = nc.dram_tensor("w_uq", [1024, 3072], F32, kind="ExternalInput").ap()
    w_ukv = nc.dram_tensor("w_ukv", [512, 4096], F32, kind="ExternalInput").ap()
    g_q = nc.dram_tensor("g_q", [1024], F32, kind="ExternalInput").ap()
    g_kv = nc.dram_tensor("g_kv", [512], F32, kind="ExternalInput").ap()
    pos_d = nc.dram_tensor("pos", [1, TPC], I32, kind="ExternalInput").ap()
    c_inv = nc.dram_tensor("c_inv", [64, 1], F32, kind="ExternalInput").ap()
    c_sgn = nc.dram_tensor("c_sgn", [64, 1], F32, kind="ExternalInput").ap()
    out = nc.dram_tensor("p1out", [8, P1N], BF16, kind="ExternalOutput").ap()
    outg = nc.dram_tensor("p1g", [8, 2, TPC], F32, kind="ExternalOutput").ap()
    with ExitStack() as es:
        pb = PB(nc, es)
        emit_p1(pb, xT_d, x_is_bf16, w_in, w_uq, w_ukv, g_q, g_kv, pos_d, c_inv, c_sgn, out, outg)
        pb.barrier()
        print("p1 instructions", pb.n_inst)
    return nc


def emit_p1(pb, xT_d, x_is_bf16, w_in, w_uq, w_ukv, g_q, g_kv, pos_d, c_inv, c_sgn, out, outg):
    nc = pb.nc
    xT = pb.sb("xT_sb", [128, KT, TPC], BF16); r_xT = Res("xT")
    wbuf = [pb.sb("wb%d" % i, [128, KT, NW], BF16) for i in range(2)]
    r_w = [Res("w%d" % i) for i in range(2)]
    psum = [pb.ps("ps%d" % i, [128, 512]) for i in range(8)]
    r_ps = [Res("ps%d" % i, excl=True) for i in range(8)]
    stg = [pb.sb("stg%d" % i, [128, 512], BF16) for i in range(4)]
    r_stg = [Res("stg%d" % i) for i in range(4)]
    stgf = [pb.sb("stgf%d" % i, [128, 512], F32) for i in range(2)]
    r_stgf = [Res("stgf%d" % i) for i in range(2)]
    cqT = pb.sb("cqT", [128, 8, TPC], BF16); r_cq = Res("cq")
    ckvT = pb.sb("ckvT", [128, 4, TPC], BF16); r_ckv = Res("ckv")
    gq_sb = pb.sb("gq_sb", [128, 8], F32); gkv_sb = pb.sb("gkv_sb", [128, 4], F32); r_g = Res("g")
    ones = pb.sb("ones", [128, 128], BF16); r_ones = Res("ones")
    rstd = pb.sb("rstd", [128, TPC], F32); r_rstd = Res("rstd")
    krT = pb.sb("krT", [64, TPC], F32); krR = pb.sb("krR", [64, TPC], F32); r_kr = Res("kr"); r_krR = Res("krR")
    cos_t = pb.sb("cos_t", [64, TPC], F32); sin_t = pb.sb("sin_t", [64, TPC], F32); r_cs = Res("cs")
    tmpA = pb.sb("tmpA", [64, TPC], F32); tmpB = pb.sb("tmpB", [64, TPC], F32); r_tA = Res("tA"); r_tB = Res("tB")
    posi = pb.sb("posi", [64, TPC], I32); inv_sb = pb.sb("inv_sb", [64, 1], F32); sgn_sb = pb.sb("sgn_sb", [64, 1], F32); r_pos = Res("pos")
    wkr = pb.sb("wkr", [128, KT, 128], BF16); r_wkr = Res("wkr")
    st = {"w": 0, "ps": 0, "stg": 0, "ev": 0, "nps": 8}

    xT_v = xT_d.rearrange("(kt p) t -> p kt t", p=128)
    xq = "pool" if not x_is_bf16 else "sp"
    for g in range(8):
        pb.dma(xq, xT[:, g * 4:(g + 1) * 4, :], xT_v[:, g * 4:(g + 1) * 4, :], writes=[r_xT])
    w_v = w_in.rearrange("(kt p) n -> p kt n", p=128)
    pb.dma("sp", gq_sb[:, :], g_q.rearrange("(ft p) -> p ft", p=128), writes=[r_g], allow_slow_non_contiguous=True)
    pb.dma("sp", gkv_sb[:, :], g_kv.rearrange("(ft p) -> p ft", p=128), writes=[r_g], allow_slow_non_contiguous=True)
    pb.dma("sp", posi[:, :], pos_d[0:1, :].partition_broadcast(64), writes=[r_pos])
    pb.dma("sp", inv_sb[:, :], c_inv, writes=[r_pos])
    pb.dma("sp", sgn_sb[:, :], c_sgn, writes=[r_pos])
    pb.op("pool", lambda e: e.memset(ones[:, :], 1.0), writes=[r_ones])

    def sub(c, name, n):
        return out[c, OFF[name]:OFF[name] + n]

    def load_w(c0, ncols):
        i = st["w"] % 2; st["w"] += 1
        for g in range(4):
            pb.dma("pool", wbuf[i][:, g * 8:(g + 1) * 8, 0:ncols], w_v[:, g * 8:(g + 1) * 8, c0:c0 + ncols], writes=[r_w[i]])
        return i

    def next_ps():
        i = st["ps"] % st["nps"]; st["ps"] += 1
        return i

    def next_stg():
        i = st["stg"] % 4; st["stg"] += 1
        return i

    def evac_engine():
        st["ev"] += 1
        return "act" if st["ev"] % 2 else "dve"

    def copy_scaled(en, o, i, scale, reads, writes):
        if en == "act":
            pb.op("act", lambda e: e.activation(out=o, in_=i, func=AF.Copy, scale=float(scale)), reads, writes)
        else:
            pb.op("dve", lambda e: e.tensor_scalar(out=o, in0=i, scalar1=float(scale), scalar2=None, op0=ALU.mult), reads, writes)

    def fm_group(lhs_fn, kts, rhs_fn, m, pi, reads):
        n = len(kts)
        for j, kt in enumerate(kts):
            pb.op("pe", lambda e, kt=kt, j=j: e.matmul(psum[pi][0:m, :], lhsT=lhs_fn(kt), rhs=rhs_fn(kt), start=(j == 0), stop=(j == n - 1)),
                  reads=reads, writes=[r_ps[pi]])

    def fm_segment(c0, ncols_total, evac):
        for cb in range(0, ncols_total, NW):
            ncols = min(NW, ncols_total - cb)
            wi = load_w(c0 + cb, ncols)
            for m0 in range(0, ncols, 128):
                m = min(128, ncols - m0)
                for th in range(2):
                    pi = next_ps()
                    fm_group(lambda kt: wbuf[wi][:, kt, m0:m0 + m], range(KT), lambda kt: xT[:, kt, th * 512:(th + 1) * 512], m, pi, [r_w[wi], r_xT])
                    evac(cb + m0, m, th, pi)

    def ev_q(scale, name):
        def f(c, m, th, pi):
            si = next_stg()
            copy_scaled(evac_engine(), stg[si][0:m, :], psum[pi][0:m, :], scale, [r_ps[pi]], [r_stg[si]])
            h = c // 128
            pb.dma("sp", sub(h, name, 128 * 1024).rearrange("(d t) -> d t", t=1024)[:, th * 512:(th + 1) * 512], stg[si][0:m, :], reads=[r_stg[si]])
        return f
    fm_segment(0, 1024, ev_q(1.0, "mlqT"))
    fm_segment(1024, 1024, ev_q(128 ** -0.5, "mlkT"))

    def ev_g(c, m, th, pi):
        si = st["stg"] % 2; st["stg"] += 1
        pb.op("dve", lambda e: e.tensor_copy(out=stgf[si][0:16, :], in_=psum[pi][0:16, :]), [r_ps[pi]], [r_stgf[si]])
        pb.dma("sp", outg[:, 0, th * 512:(th + 1) * 512], stgf[si][0:8, :], reads=[r_stgf[si]])
        pb.dma("sp", outg[:, 1, th * 512:(th + 1) * 512], stgf[si][8:16, :], reads=[r_stgf[si]])
    fm_segment(6144, 16, ev_g)

    def tm_segment(c0, ncols_total, kind, name, hw):
        for cb in range(0, ncols_total, NW):
            wi = load_w(c0 + cb, NW)
            for tt in range(8):
                pi = next_ps()
                for kt in range(KT):
                    pb.op("pe", lambda e, kt=kt: e.matmul(psum[pi][:, 0:NW], lhsT=xT[:, kt, tt * 128:(tt + 1) * 128], rhs=wbuf[wi][:, kt, 0:NW], start=(kt == 0), stop=(kt == KT - 1)),
                          reads=[r_w[wi], r_xT], writes=[r_ps[pi]])
                si = next_stg()
                if kind == "sig":
                    pb.op("act", lambda e: e.activation(out=stg[si][:, 0:NW], in_=psum[pi][:, 0:NW], func=AF.Sigmoid), [r_ps[pi]], [r_stg[si]])
                else:
                    copy_scaled(evac_engine(), stg[si][:, 0:NW], psum[pi][:, 0:NW], kind, [r_ps[pi]], [r_stg[si]])
                nh = NW // hw
                h0 = cb // hw
                for hh in range(max(nh, 1)):
                    if hw >= NW:
                        h = cb // hw; coff = cb % hw; wd = NW
                    else:
                        h = h0 + hh; coff = 0; wd = hw
                    dst = sub(h, name, 1024 * hw).rearrange("(t d) -> t d", d=hw)[tt * 128:(tt + 1) * 128, coff:coff + wd]
                    src = stg[si][:, hh * wd:(hh + 1) * wd] if hw < NW else stg[si][:, 0:NW]
                    pb.dma("sp", dst, src, reads=[r_stg[si]])
    tm_segment(1024, 1024, 128 ** -0.5, "mlk", 128)
    tm_segment(2048, 2048, 1.0, "mlv", 256)
    tm_segment(4096, 2048, "sig", "mlo", 256)

    import os
    STAGE = int(os.environ.get('STAGE', '99'))
    if STAGE < 1: return
    st["nps"] = 6
    st["ps"] = 0

    def latent_segment(c0, nf, latT, r_lat, g_sb):
        nft = nf // 128

        def ev(c, m, th, pi):
            ft = c // 128
            pb.op("dve", lambda e: e.tensor_scalar(out=latT[:, ft, th * 512:(th + 1) * 512], in0=psum[pi][:, :], scalar1=g_sb[:, ft:ft + 1], scalar2=None, op0=ALU.mult),
                  [r_ps[pi], r_g], [r_lat])
            if os.environ.get('SUB') == 'b': return
            si = next_stg()
            pb.op("act", lambda e: e.activation(out=stg[si][:, :], in_=psum[pi][:, :], func=AF.Square), [r_ps[pi]], [r_stg[si]])
            if os.environ.get('SUB2') == 'nomm': return
            pb.op("pe", lambda e: e.matmul(psum[6 + th][:, :], lhsT=ones[:, :], rhs=stg[si][:, :], start=(ft == 0), stop=(ft == nft - 1)),
                  reads=[r_ones, r_stg[si]], writes=[r_ps[6 + th]])
        fm_segment(c0, nf, ev)
        if os.environ.get('SUB') in ('b', 'c'): return
        for th in range(2):
            pb.op("act", lambda e: e.activation(out=rstd[:, th * 512:(th + 1) * 512], in_=psum[6 + th][:, :], func=AF.Sqrt, scale=1.0 / nf, bias=1e-6),
                  [r_ps[6 + th]], [r_rstd])
        pb.op("dve", lambda e: e.reciprocal(out=rstd[:, :], in_=rstd[:, :]), [r_rstd], [r_rstd])
        for ft in range(nft):
            en = "pool" if ft % 2 else "dve"
            pb.op(en, lambda e: e.tensor_tensor(out=latT[:, ft, :], in0=latT[:, ft, :], in1=rstd[:, :], op=ALU.mult), [r_lat, r_rstd], [r_lat])

    def rope_tables():
        def wrap(o, i, shift):
            m = krR
            pb.op("dve", lambda e: e.tensor_scalar(out=o[:, :], in0=i[:, :], scalar1=float(shift), scalar2=None, op0=ALU.add), [r_tA, r_cs], [r_tB])
            pb.op("dve", lambda e: e.tensor_scalar(out=m[:, :], in0=o[:, :], scalar1=float(math.pi), scalar2=float(-2 * math.pi), op0=ALU.is_gt, op1=ALU.mult), [r_tB], [r_krR])
            pb.op("dve", lambda e: e.tensor_tensor(out=o[:, :], in0=o[:, :], in1=m[:, :], op=ALU.add), [r_tB, r_krR], [r_tB])
            pb.op("dve", lambda e: e.tensor_scalar(out=m[:, :], in0=o[:, :], scalar1=float(-math.pi), scalar2=float(2 * math.pi), op0=ALU.is_lt, op1=ALU.mult), [r_tB], [r_krR])
            pb.op("dve", lambda e: e.tensor_tensor(out=o[:, :], in0=o[:, :], in1=m[:, :], op=ALU.add), [r_tB, r_krR], [r_tB])
        ang = tmpA; kf = tmpB
        pb.op("dve", lambda e: e.tensor_copy(out=ang[:, :], in_=posi[:, :]), [r_pos], [r_tA])
        pb.op("dve", lambda e: e.tensor_scalar(out=ang[:, :], in0=ang[:, :], scalar1=inv_sb[:, 0:1], scalar2=None, op0=ALU.mult), [r_tA, r_pos], [r_tA])
        pb.op("dve", lambda e: e.tensor_scalar(out=kf[:, :], in0=ang[:, :], scalar1=float(1.0 / (2 * math.pi)), scalar2=0.5, op0=ALU.mult, op1=ALU.add), [r_tA], [r_tB])
        pb.op("dve", lambda e: e.tensor_copy(out=posi[:, :], in_=kf[:, :]), [r_tB], [r_pos])
        pb.op("dve", lambda e: e.tensor_copy(out=kf[:, :], in_=posi[:, :]), [r_pos], [r_tB])
        pb.op("dve", lambda e: e.scalar_tensor_tensor(out=ang[:, :], in0=kf[:, :], scalar=float(-2 * math.pi), in1=ang[:, :], op0=ALU.mult, op1=ALU.add), [r_tA, r_tB], [r_tA])
        wrap(kf, ang, 0.0)
        pb.op("act", lambda e: e.activation(out=sin_t[:, :], in_=kf[:, :], func=AF.Sin), [r_tB], [r_cs])
        wrap(kf, ang, math.pi / 2)
        pb.op("act", lambda e: e.activation(out=cos_t[:, :], in_=kf[:, :], func=AF.Sin), [r_tB], [r_cs])
        pb.op("dve", lambda e: e.tensor_scalar(out=sin_t[:, :], in0=sin_t[:, :], scalar1=sgn_sb[:, 0:1], scalar2=None, op0=ALU.mult), [r_cs, r_pos], [r_cs])
    rope_tables()
    if STAGE < 2: return

    def rope_apply(o_ap, a_ap, b_ap, reads, writes):
        pb.op("dve", lambda e: e.tensor_tensor(out=tmpA[:, :], in0=a_ap, in1=cos_t[:, :], op=ALU.mult), reads + [r_cs], [r_tA])
        pb.op("dve", lambda e: e.tensor_tensor(out=tmpB[:, :], in0=b_ap, in1=sin_t[:, :], op=ALU.mult), reads + [r_cs], [r_tB])
        pb.op("dve", lambda e: e.tensor_tensor(out=o_ap, in0=tmpA[:, :], in1=tmpB[:, :], op=ALU.add), [r_tA, r_tB], writes)

    for g in range(4):
        pb.dma("pool", wkr[:, g * 8:(g + 1) * 8, 0:64], w_v[:, g * 8:(g + 1) * 8, 7696:7760], writes=[r_wkr])
        pb.dma("pool", wkr[:, g * 8:(g + 1) * 8, 64:96], w_v[:, g * 8:(g + 1) * 8, 7728:7760], writes=[r_wkr])
        pb.dma("pool", wkr[:, g * 8:(g + 1) * 8, 96:128], w_v[:, g * 8:(g + 1) * 8, 7696:7728], writes=[r_wkr])
    for th in range(2):
        for which, dstT, r_d in ((0, krT, r_kr), (1, krR, r_krR)):
            pi = next_ps()
            fm_group(lambda kt: wkr[:, kt, which * 64:(which + 1) * 64], range(KT), lambda kt: xT[:, kt, th * 512:(th + 1) * 512], 64, pi, [r_wkr, r_xT])
            pb.op("act", lambda e: e.activation(out=dstT[:, th * 512:(th + 1) * 512], in_=psum[pi][0:64, :], func=AF.Copy), [r_ps[pi]], [r_d])
    krb = pb.sb("krb", [64, TPC], BF16); r_krb = Res("krb")
    rope_apply(krb[:, :], krT[:, :], krR[:, :], [r_kr, r_krR], [r_krb])
    for c in range(8):
        pb.dma("sp", sub(c, "krT", 64 * 1024).rearrange("(d t) -> d t", t=1024), krb[:, :], reads=[r_krb])

    if STAGE < 3: return
    latent_segment(6160, 1024, cqT, r_cq, gq_sb)
    if os.environ.get('SUB') in ('a', 'c'): return
    wq = [pb.sb("wq%d" % i, [128, 8, 256], BF16) for i in range(2)]; r_wq = [Res("wq0"), Res("wq1")]
    uq_v = w_uq.rearrange("(ft p) n -> p ft n", p=128)
    qro = pb.sb("qro", [64, TPC], F32); qrr = pb.sb("qrr", [64, TPC], F32); r_qro = Res("qro"); r_qrr = Res("qrr")
    qrb = pb.sb("qrb", [64, TPC], BF16); r_qrb = Res("qrb")
    for h in range(16):
        wi = h % 2
        b0 = h * 192
        pb.dma("pool", wq[wi][:, :, 0:192], uq_v[:, :, b0:b0 + 192], writes=[r_wq[wi]])
        pb.dma("pool", wq[wi][:, :, 192:224], uq_v[:, :, b0 + 160:b0 + 192], writes=[r_wq[wi]])
        pb.dma("pool", wq[wi][:, :, 224:256], uq_v[:, :, b0 + 128:b0 + 160], writes=[r_wq[wi]])
        c = h // 2; hs = h % 2
        dst_h = sub(c, "qT", 2 * 192 * 1024).rearrange("(h d t) -> h d t", h=2, t=1024)
        for th in range(2):
            pi = next_ps()
            fm_group(lambda ft: wq[wi][:, ft, 0:128], range(8), lambda ft: cqT[:, ft, th * 512:(th + 1) * 512], 128, pi, [r_wq[wi], r_cq])
            si = next_stg()
            copy_scaled(evac_engine(), stg[si][:, :], psum[pi][:, :], 1.0, [r_ps[pi]], [r_stg[si]])
            pb.dma("sp", dst_h[hs, 0:128, th * 512:(th + 1) * 512], stg[si][:, :], reads=[r_stg[si]])
            for which, dstT, r_d in ((0, qro, r_qro), (1, qrr, r_qrr)):
                pi = next_ps()
                fm_group(lambda ft: wq[wi][:, ft, 128 + which * 64:192 + which * 64], range(8), lambda ft: cqT[:, ft, th * 512:(th + 1) * 512], 64, pi, [r_wq[wi], r_cq])
                pb.op("act", lambda e: e.activation(out=dstT[:, th * 512:(th + 1) * 512], in_=psum[pi][0:64, :], func=AF.Copy), [r_ps[pi]], [r_d])
        rope_apply(qrb[:, :], qro[:, :], qrr[:, :], [r_qro, r_qrr], [r_qrb])
        pb.dma("sp", dst_h[hs, 128:192, :], qrb[:, :], reads=[r_qrb])

    if STAGE < 4: return
    latent_segment(7184, 512, ckvT, r_ckv, gkv_sb)
    wkv = [pb.sb("wkv%d" % i, [128, 4, 256], BF16) for i in range(2)]; r_wkv = [Res("wkv0"), Res("wkv1")]
    ukv_v = w_ukv.rearrange("(ft p) n -> p ft n", p=128)
    for h in range(16):
        wi = h % 2
        pb.dma("pool", wkv[wi][:, :, :], ukv_v[:, :, h * 256:(h + 1) * 256], writes=[r_wkv[wi]])
        c = h // 2; hs = h % 2
        dst_k = sub(c, "kT", 2 * 128 * 1024).rearrange("(h d t) -> h d t", h=2, t=1024)
        dst_v = sub(c, "v", 1024 * 256).rearrange("(t h d) -> t h d", h=2, d=128)
        for th in range(2):
            pi = next_ps()
            fm_group(lambda ft: wkv[wi][:, ft, 0:128], range(4), lambda ft: ckvT[:, ft, th * 512:(th + 1) * 512], 128, pi, [r_wkv[wi], r_ckv])
            si = next_stg()
            copy_scaled(evac_engine(), stg[si][:, :], psum[pi][:, :], 1.0, [r_ps[pi]], [r_stg[si]])
            pb.dma("sp", dst_k[hs, :, th * 512:(th + 1) * 512], stg[si][:, :], reads=[r_stg[si]])
        for tq in range(2):
            pi = next_ps()
            for j in range(4):
                tt = tq * 4 + j
                for ft in range(4):
                    pb.op("pe", lambda e, ft=ft: e.matmul(psum[pi][:, j * 128:(j + 1) * 128], lhsT=ckvT[:, ft, tt * 128:(tt + 1) * 128], rhs=wkv[wi][:, ft, 128:256], start=(ft == 0), stop=(ft == 3)),
                          reads=[r_wkv[wi], r_ckv], writes=[r_ps[pi]])
            si = next_stg()
            copy_scaled(evac_engine(), stg[si][:, :], psum[pi][:, :], 1.0, [r_ps[pi]], [r_stg[si]])
            for j in range(4):
                tt = tq * 4 + j
                pb.dma("sp", dst_v[tt * 128:(tt + 1) * 128, hs, :], stg[si][:, j * 128:(j + 1) * 128], reads=[r_stg[si]])
    st["nps"] = 8


TOK = 8192
NCH = 128


def p2_consts():
    import ml_dtypes
    s = np.arange(64)
    mask64 = (s[None, :] >= s[:, None]).astype(np.float32)
    k = np.arange(128)
    mask128 = ((k[:, None] // 64) <= (k[None, :] // 64)).astype(np.float32)
    tri = (k[:, None] < k[None, :]).astype(np.float32)
    sel63 = np.zeros((64, 128), np.float32); sel63[63, :] = 1.0
    return {"c_mask64": mask64, "c_mask128": mask128, "c_tri": tri, "c_sel63": sel63, "c_ident": np.eye(128, dtype=np.float32)}


def build_p2():
    nc = bass.Bass("TRN2", target_bir_lowering=False)
    p2in = nc.dram_tensor("p2in", [8, P1N], BF16, kind="ExternalInput").ap()
    p2g = nc.dram_tensor("p2g", [8, 2, 1024], F32, kind="ExternalInput").ap()
    mlb = nc.dram_tensor("mlb", [1, 2], F32, kind="ExternalInput").ap()
    mlg = nc.dram_tensor("mlg", [1, 256], F32, kind="ExternalInput").ap()
    cst = {k: nc.dram_tensor(k, list(v.shape), F32, kind="ExternalInput").ap() for k, v in p2_consts().items()}
    p2out = nc.dram_tensor("p2out", [8, 512, 1024], BF16, kind="ExternalOutput").ap()
    scr = nc.dram_tensor("p2scr", [4, TOK], F32).ap()
    with ExitStack() as es:
        pb = PB(nc, es)
        emit_p2(pb, p2in, p2g, mlb, mlg, cst, p2out, scr)
        pb.barrier()
        print("p2 instructions", pb.n_inst)
    return nc


def emit_p2(pb, p2in, p2g, mlb, mlg, cst, p2out, scr):
    import os
    nc = pb.nc
    outer_es = pb.es
    psum = [pb.ps("ps%d" % i, [128, 512]) for i in range(7)]
    r_ps = [Res("ps%d" % i, excl=True) for i in range(7)]
    psTb = pb.ps("psTb", [128, 1024], BF16); r_psTb = Res("psTb", excl=True)
    ident = pb.sb("ident", [128, 128], BF16); identf = pb.sb("identf", [128, 128], F32); r_id = Res("ident")
    mask128 = pb.sb("mask128", [128, 128], BF16); r_m128 = Res("m128")
    pb.dma("sp", identf[:, :], cst["c_ident"], writes=[r_id])
    pb.op("dve", lambda e: e.tensor_copy(out=ident[:, :], in_=identf[:, :]), [r_id], [r_id])
    tmpf = pb.sb("tmpf", [128, 128], F32); r_tmpf = Res("tmpf")
    pb.dma("sp", tmpf[:, :], cst["c_mask128"], writes=[r_tmpf])
    pb.op("dve", lambda e: e.tensor_copy(out=mask128[:, :], in_=tmpf[:, :]), [r_tmpf], [r_m128])
    ostg = [pb.sb("ostg%d" % i, [128, 512], BF16) for i in range(2)]; r_ostg = [Res("ostg0"), Res("ostg1")]
    cnt = {"ostg": 0}

    def V(fn, r, w): return pb.op("dve", fn, r, w)
    def A(fn, r, w): return pb.op("act", fn, r, w)
    def G(fn, r, w): return pb.op("pool", fn, r, w)
    def T(fn, r, w): return pb.op("pe", fn, r, w)

    def seg(j, name, n):
        return p2in[j, OFF[name]:OFF[name] + n]

    if os.environ.get("P2_SKIP_ML") != "1":
      with ExitStack() as es2:
        pb.es = es2
        qT = pb.sb("qT", [128, TOK], BF16); kT = pb.sb("kT", [128, TOK], BF16); r_qk = Res("qk")
        k_c = pb.sb("k_c", [64, NCH, 128], BF16); r_kc = Res("kc")
        v_aug = pb.sb("v_aug", [64, NCH, 257], BF16); r_v = Res("v")
        og = [pb.sb("og%d" % i, [64, 8, 256], BF16) for i in range(2)]; r_og = [Res("og0"), Res("og1")]
        mu_bc = pb.sb("mu_bc", [64, TOK], F32); r_mubc = Res("mubc")
        g_bc = pb.sb("g_bc", [64, 256], F32); r_gbc = Res("gbc")
        mask64 = pb.sb("mask64", [64, 64], F32); tri = pb.sb("tri", [128, 128], F32); sel63 = pb.sb("sel63", [64, 128], F32); r_c = Res("consts")
        bia = pb.sb("bia", [128, 2], F32); r_bia = Res("bia")
        for j in range(8):
            pb.dma("sp", qT[:, j * 1024:(j + 1) * 1024], seg(j, "mlqT", 131072).rearrange("(d t) -> d t", t=1024), writes=[r_qk])
            pb.dma("sp", kT[:, j * 1024:(j + 1) * 1024], seg(j, "mlkT", 131072).rearrange("(d t) -> d t", t=1024), writes=[r_qk])
            pb.dma("sp", k_c[:, j * 16:(j + 1) * 16, :], seg(j, "mlk", 131072).rearrange("(c p d) -> p c d", p=64, d=128), writes=[r_kc])
            pb.dma("sp", v_aug[:, j * 16:(j + 1) * 16, 0:256], seg(j, "mlv", 262144).rearrange("(c p d) -> p c d", p=64, d=256), writes=[r_v])
        G(lambda e: e.memset(v_aug[:, :, 256:257], 1.0), [], [r_v])
        pb.dma("sp", g_bc[:, :], mlg[0:1, :].partition_broadcast(64), writes=[r_gbc])
        pb.dma("sp", mask64[:, :], cst["c_mask64"], writes=[r_c])
        pb.dma("sp", tri[:, :], cst["c_tri"], writes=[r_c])
        pb.dma("sp", sel63[:, :], cst["c_sel63"], writes=[r_c])
        pb.dma("sp", bia[:, :], mlb[0:1, :].partition_broadcast(128), writes=[r_bia])

        def cm(name): return pb.sb(name, [128, 64], F32)
        ig = cm("ig"); fg = cm("fg"); lf = cm("lf"); Bc = cm("Bc"); Gc = cm("Gc"); mu = cm("mu"); onescm = cm("onescm"); zcm = cm("zcm")
        r_ig, r_fg, r_lf, r_B, r_G, r_mu, r_1 = Res("ig"), Res("fg"), Res("lf"), Res("B"), Res("G"), Res("mu"), Res("ones")
        sm = pb.sb("sm", [128, 8], F32); r_sm = Res("sm")
        rowt = pb.sb("rowt", [1, 256], F32); r_row = Res("row")
        for j in range(8):
            pb.dma("sp", ig[j * 16:(j + 1) * 16, :], p2g[j, 0, :].rearrange("(c i) -> c i", i=64), writes=[r_ig])
            pb.dma("sp", fg[j * 16:(j + 1) * 16, :], p2g[j, 1, :].rearrange("(c i) -> c i", i=64), writes=[r_fg])
        G(lambda e: e.memset(onescm[:, :], 1.0), [], [r_1])
        G(lambda e: e.memset(zcm[:, :], 0.0), [], [r_1])
        V(lambda e: e.tensor_scalar(out=bia[:, :], in0=bia[:, :], scalar1=1.0 / 15.0, scalar2=None, op0=ALU.mult), [r_bia], [r_bia])
        A(lambda e: e.activation(out=ig[:, :], in_=ig[:, :], func=AF.Tanh, scale=1.0 / 15.0, bias=bia[:, 0:1]), [r_ig, r_bia], [r_ig])
        A(lambda e: e.activation(out=fg[:, :], in_=fg[:, :], func=AF.Tanh, scale=1.0 / 15.0, bias=bia[:, 1:2]), [r_fg, r_bia], [r_fg])
        V(lambda e: e.tensor_scalar(out=ig[:, :], in0=ig[:, :], scalar1=15.0, scalar2=None, op0=ALU.mult), [r_ig], [r_ig])
        A(lambda e: e.activation(out=lf[:, :], in_=fg[:, :], func=AF.Exp, scale=-15.0), [r_fg], [r_lf])
        A(lambda e: e.activation(out=lf[:, :], in_=lf[:, :], func=AF.Ln, scale=1.0, bias=1.0), [r_lf], [r_lf])
        V(lambda e: e.tensor_scalar(out=lf[:, :], in0=lf[:, :], scalar1=-1.0, scalar2=None, op0=ALU.mult), [r_lf], [r_lf])
        V(lambda e: e.tensor_tensor_scan(out=Bc[:, :], data0=onescm[:, :], data1=lf[:, :], initial=0.0, op0=ALU.mult, op1=ALU.add), [r_lf, r_1], [r_B])
        T(lambda e: e.matmul(psum[0][:, 0:1], lhsT=tri[:, :], rhs=Bc[:, 63:64], start=True, stop=True), [r_c, r_B], [r_ps[0]])
        V(lambda e: e.tensor_copy(out=sm[:, 0:1], in_=psum[0][:, 0:1]), [r_ps[0]], [r_sm])
        V(lambda e: e.tensor_scalar(out=Bc[:, :], in0=Bc[:, :], scalar1=sm[:, 0:1], scalar2=None, op0=ALU.add), [r_B, r_sm], [r_B])
        V(lambda e: e.tensor_tensor(out=Gc[:, :], in0=ig[:, :], in1=Bc[:, :], op=ALU.subtract), [r_ig, r_B], [r_G])
        V(lambda e: e.tensor_tensor_scan(out=mu[:, :], data0=Gc[:, :], data1=zcm[:, :], initial=0.0, op0=ALU.max, op1=ALU.add), [r_G, r_1], [r_mu])
        T(lambda e: e.matmul(psum[0][0:1, 0:128], lhsT=mu[:, 63:64], rhs=identf[:, :], start=True, stop=True), [r_mu, r_id], [r_ps[0]])
        V(lambda e: e.tensor_copy(out=rowt[0:1, 0:128], in_=psum[0][0:1, 0:128]), [r_ps[0]], [r_row])
        V(lambda e: e.tensor_tensor_scan(out=rowt[0:1, 128:256], data0=rowt[0:1, 0:128], data1=zcm[0:1, 0:64].to_broadcast([1, 128]) if False else rowt[0:1, 0:128], initial=0.0, op0=ALU.max, op1=ALU.max), [r_row], [r_row])
        V(lambda e: e.tensor_copy(out=rowt[0:1, 1:128], in_=rowt[0:1, 128:255]), [r_row], [r_row])
        V(lambda e: e.memset(rowt[0:1, 0:1], 0.0), [r_row], [r_row])
        T(lambda e: e.matmul(psum[0][:, 4:5], lhsT=rowt[0:1, 0:128], rhs=onescm[0:1, 0:1], start=True, stop=True), [r_row, r_1], [r_ps[0]])
        V(lambda e: e.tensor_copy(out=sm[:, 1:2], in_=psum[0][:, 4:5]), [r_ps[0]], [r_sm])
        V(lambda e: e.tensor_scalar(out=mu[:, :], in0=mu[:, :], scalar1=sm[:, 1:2], scalar2=None, op0=ALU.max), [r_mu, r_sm], [r_mu])
        r_scr = Res("scr")
        pb.dma("sp", scr[0, :].rearrange("(c i) -> c i", i=64), mu[:, :], reads=[r_mu], writes=[r_scr])
        pb.dma("sp", mu_bc[:, :], scr[0:1, :].partition_broadcast(64), reads=[r_scr], writes=[r_mubc])
        def col(name): return pb.sb(name, [64, 128], F32)
        G_col = col("G_col"); mu_col = col("mu_col"); B_col = col("B_col"); muend = col("muend"); muprev = col("muprev")
        winter = col("winter"); emt = col("emt"); ws = col("ws")
        wprev = pb.sb("wprev", [128, 128], F32); muend128 = pb.sb("muend128", [128, 128], F32); muprev128 = pb.sb("muprev128", [128, 128], F32)
        r_col = Res("cols")
        for i, (src, dst) in enumerate(((Gc, G_col), (mu, mu_col), (Bc, B_col))):
            T(lambda e: e.matmul(psum[1][0:64, i * 128:(i + 1) * 128], lhsT=src[:, :], rhs=identf[:, :], is_transpose=True, start=True, stop=True), [r_G, r_mu, r_B, r_id], [r_ps[1]])
            V(lambda e: e.tensor_copy(out=dst[:, :], in_=psum[1][0:64, i * 128:(i + 1) * 128]), [r_ps[1]], [r_col])
        T(lambda e: e.matmul(psum[2][:, 0:128], lhsT=sel63[:, :], rhs=mu_col[:, :], start=True, stop=True), [r_c, r_col], [r_ps[2]])
        V(lambda e: e.tensor_copy(out=muend128[:, :], in_=psum[2][:, 0:128]), [r_ps[2]], [r_col])
        V(lambda e: e.memset(muprev128[:, 0:1], 0.0), [], [r_col])
        V(lambda e: e.tensor_copy(out=muprev128[:, 1:128], in_=muend128[:, 0:127]), [r_col], [r_col])
        V(lambda e: e.tensor_tensor(out=winter[:, :], in0=muprev128[0:64, :], in1=mu_col[:, :], op=ALU.subtract), [r_col], [r_col])
        A(lambda e: e.activation(out=winter[:, :], in_=winter[:, :], func=AF.Exp), [r_col], [r_col])
        V(lambda e: e.tensor_tensor(out=emt[:, :], in0=B_col[:, :], in1=mu_col[:, :], op=ALU.add), [r_col], [r_col])
        A(lambda e: e.activation(out=emt[:, :], in_=emt[:, :], func=AF.Exp, scale=-1.0), [r_col], [r_col])
        V(lambda e: e.tensor_tensor(out=ws[:, :], in0=G_col[:, :], in1=muend128[0:64, :], op=ALU.subtract), [r_col], [r_col])
        A(lambda e: e.activation(out=ws[:, :], in_=ws[:, :], func=AF.Exp), [r_col], [r_col])
        V(lambda e: e.tensor_tensor(out=wprev[:, :], in0=muprev128[:, :], in1=muend128[:, :], op=ALU.subtract), [r_col], [r_col])
        A(lambda e: e.activation(out=wprev[:, :], in_=wprev[:, :], func=AF.Exp), [r_col], [r_col])

        CT = pb.sb("CT", [128, 257], F32); CTb = pb.sb("CTb", [128, 257], BF16); r_CT = Res("CT"); r_CTb = Res("CTb")
        V(lambda e: e.memset(CT[:, :], 0.0), [], [r_CT])
        V(lambda e: e.memset(CTb[:, :], 0.0), [], [r_CTb])
        NB = 3
        ET = [pb.sb("ET%d" % i, [64, 64], F32) for i in range(NB)]; r_ET = [Res("ET%d" % i) for i in range(NB)]
        PT = [pb.sb("PT%d" % i, [64, 64], BF16) for i in range(NB)]; r_PT = [Res("PT%d" % i) for i in range(NB)]
        kw = [pb.sb("kw%d" % i, [64, 128], BF16) for i in range(NB)]; r_kw = [Res("kw%d" % i) for i in range(NB)]
        hin = [pb.sb("hin%d" % i, [64, 257], F32) for i in range(NB)]; r_hin = [Res("hin%d" % i) for i in range(NB)]
        hn = [pb.sb("hn%d" % i, [64, 257], F32) for i in range(NB)]; r_hn = [Res("hn%d" % i) for i in range(NB)]
        sc = [pb.sb("sc%d" % i, [64, 4], F32) for i in range(NB)]; r_sc = [Res("sc%d" % i) for i in range(NB)]
        hsq = [pb.sb("hsq%d" % i, [64, 256], F32) for i in range(NB)]; r_hsq = [Res("hsq%d" % i) for i in range(NB)]
        og2 = [pb.sb("ogg%d" % i, [64, 256], F32) for i in range(NB)]; r_og2 = [Res("ogg%d" % i) for i in range(NB)]
        yb = [pb.sb("yb%d" % i, [64, 256], BF16) for i in range(NB)]; r_yb = [Res("yb%d" % i) for i in range(NB)]
        psT = [psTb]
        r_psT = [r_psTb]
        ngrp = NCH // 8
        def stage_a(j):
            b = j % NB
            t0 = j * 64
            sb_ = 2 if j % 2 == 0 else 6
            T(lambda e: e.matmul(psum[sb_][0:64, 0:64], lhsT=kT[:, t0:t0 + 64], rhs=qT[:, t0:t0 + 64], start=True, stop=True), [r_qk], [r_ps[sb_]])
            A(lambda e: e.activation(out=ET[b][:, :], in_=mu_bc[:, t0:t0 + 64], func=AF.Exp, scale=-1.0, bias=G_col[:, j:j + 1]), [r_mubc, r_col], [r_ET[b]])
            G(lambda e: e.tensor_tensor(out=ET[b][:, :], in0=ET[b][:, :], in1=mask64[:, :], op=ALU.mult), [r_ET[b], r_c], [r_ET[b]])
            V(lambda e: e.tensor_tensor(out=PT[b][:, :], in0=psum[sb_][0:64, 0:64], in1=ET[b][:, :], op=ALU.mult), [r_ps[sb_], r_ET[b]], [r_PT[b]])
            G(lambda e: e.tensor_scalar(out=kw[b][:, :], in0=k_c[:, j, :], scalar1=ws[:, j:j + 1], scalar2=None, op0=ALU.mult), [r_kc, r_col], [r_kw[b]])

        def stage_b(j):
            b = j % NB
            t0 = j * 64
            T(lambda e: e.matmul(psum[3][0:64, 0:257], lhsT=PT[b][:, :], rhs=v_aug[:, j, :], start=True, stop=True), [r_PT[b], r_v], [r_ps[3]])
            T(lambda e: e.matmul(psum[4][0:64, 0:257], lhsT=qT[:, t0:t0 + 64], rhs=CTb[:, :], start=True, stop=True), [r_qk, r_CTb], [r_ps[4]])
            V(lambda e: e.tensor_copy(out=hin[b][:, :], in_=psum[3][0:64, 0:257]), [r_ps[3]], [r_hin[b]])
            V(lambda e: e.scalar_tensor_tensor(out=hn[b][:, :], in0=psum[4][0:64, 0:257], scalar=winter[:, j:j + 1], in1=hin[b][:, :], op0=ALU.mult, op1=ALU.add),
              [r_ps[4], r_col, r_hin[b]], [r_hn[b]])
            T(lambda e: e.matmul(psum[5][:, 0:257], lhsT=kw[b][:, :], rhs=v_aug[:, j, :], start=True, stop=True), [r_kw[b], r_v], [r_ps[5]])
            V(lambda e: e.scalar_tensor_tensor(out=CT[:, :], in0=CT[:, :], scalar=wprev[:, j:j + 1], in1=psum[5][:, 0:257], op0=ALU.mult, op1=ALU.add),
              [r_CT, r_col, r_ps[5]], [r_CT])
            G(lambda e: e.tensor_copy(out=CTb[:, :], in_=CT[:, :]), [r_CT], [r_CTb])

        def stage_c1(j):
            b = j % NB
            V(lambda e: e.tensor_scalar(out=sc[b][:, 3:4], in0=hn[b][:, 256:257], scalar1=-1.0, scalar2=None, op0=ALU.mult), [r_hn[b]], [r_sc[b]])
            V(lambda e: e.tensor_tensor(out=sc[b][:, 0:1], in0=sc[b][:, 3:4], in1=hn[b][:, 256:257], op=ALU.max), [r_hn[b], r_sc[b]], [r_sc[b]])
            V(lambda e: e.tensor_tensor(out=sc[b][:, 0:1], in0=sc[b][:, 0:1], in1=emt[:, j:j + 1], op=ALU.max), [r_sc[b], r_col], [r_sc[b]])
            V(lambda e: e.reciprocal(out=sc[b][:, 0:1], in_=sc[b][:, 0:1]), [r_sc[b]], [r_sc[b]])
            V(lambda e: e.tensor_scalar(out=hn[b][:, 0:256], in0=hn[b][:, 0:256], scalar1=sc[b][:, 0:1], scalar2=None, op0=ALU.mult), [r_hn[b], r_sc[b]], [r_hn[b]])
            V(lambda e: e.scalar_tensor_tensor(out=hsq[b][:, :], in0=hn[b][:, 0:256], scalar=1.0, in1=hn[b][:, 0:256], op0=ALU.mult, op1=ALU.mult, accum_out=sc[b][:, 1:2]), [r_hn[b]], [r_hsq[b], r_sc[b]])
            A(lambda e: e.activation(out=sc[b][:, 2:3], in_=sc[b][:, 1:2], func=AF.Ln, scale=1.0 / 256.0, bias=1e-6), [r_sc[b]], [r_sc[b]])
            A(lambda e: e.activation(out=sc[b][:, 2:3], in_=sc[b][:, 2:3], func=AF.Exp, scale=-0.5), [r_sc[b]], [r_sc[b]])
            oi = (j // 8) % 2; ci = j % 8
            G(lambda e: e.tensor_tensor(out=og2[b][:, :], in0=og[oi][:, ci, :], in1=g_bc[:, :], op=ALU.mult), [r_og[oi], r_gbc], [r_og2[b]])

        def stage_c2(j):
            b = j % NB
            ci = j % 8
            V(lambda e: e.scalar_tensor_tensor(out=yb[b][:, :], in0=hn[b][:, 0:256], scalar=sc[b][:, 2:3], in1=og2[b][:, :], op0=ALU.mult, op1=ALU.mult),
              [r_hn[b], r_sc[b], r_og2[b]], [r_yb[b]])
            for half in range(2):
                T(lambda e: e.matmul(psT[0][:, half * 256 + (ci % 4) * 64: half * 256 + (ci % 4) * 64 + 64], lhsT=yb[b][:, half * 128:(half + 1) * 128], rhs=ident[0:64, 0:64], is_transpose=True, start=True, stop=True),
                  [r_yb[b], r_id], [r_psT[0]])
            if ci % 4 == 3:
                oi2 = cnt["ostg"] % 2; cnt["ostg"] += 1
                V(lambda e: e.tensor_copy(out=ostg[oi2][:, :], in_=psT[0][:, 0:512]), [r_psT[0]], [r_ostg[oi2]])
                tb = (j - 3) * 64
                jb2 = tb // 1024; tl = tb % 1024
                for half in range(2):
                    pb.dma("sp", p2out[jb2, half * 128:(half + 1) * 128, tl:tl + 256], ostg[oi2][:, half * 256:(half + 1) * 256], reads=[r_ostg[oi2]])

        for grp in range(ngrp):
            oi = grp % 2
            jb = grp // 2
            cc0 = (grp % 2) * 8
            pb.dma("sp", og[oi][:, :, :], seg(jb, "mlo", 262144).rearrange("(c p d) -> p c d", p=64, d=256)[:, cc0:cc0 + 8, :], writes=[r_og[oi]])
            for ci in range(8):
                j = grp * 8 + ci
                if j == 0:
                    stage_a(0)
                if j + 1 < NCH:
                    stage_a(j + 1)
                stage_b(j)
                if j >= 1:
                    stage_c1(j - 1)
                if j >= 2:
                    stage_c2(j - 2)
        stage_c1(NCH - 1)
        stage_c2(NCH - 2)
        stage_c2(NCH - 1)
        pb.barrier()
      pb.es = outer_es

    if os.environ.get("P2_SKIP_MLA") == "1":
        return
    scale = 192 ** -0.5
    qA = pb.sb("qA", [128, TOK], BF16); qB = pb.sb("qB", [65, TOK], BF16); kA = pb.sb("kA", [128, TOK], BF16); kB = pb.sb("kB", [65, TOK], BF16)
    r_q = Res("q"); r_k = Res("k")
    va = pb.sb("va", [128, 64, 129], BF16); r_va = Res("va")
    sq = [pb.sb("sq%d" % i, [128, 512], BF16) for i in range(2)]; r_sq = [Res("sq0"), Res("sq1")]
    onesb = pb.sb("onesb", [128, 1], BF16); r_ob = Res("onesb")
    nrow = pb.sb("nrow", [1, TOK], BF16); r_nrow = Res("nrow")
    sc1 = pb.sb("sca", [1, 8], F32); r_sc1 = Res("sc1")
    rt = pb.sb("rt", [1, 512], F32); r_rt = Res("rt")
    PTm = [pb.sb("PTm%d" % i, [128, 512], BF16) for i in range(3)]; r_PTm = [Res("PTm%d" % i) for i in range(3)]
    ob = [pb.sb("ob%d" % i, [128, 128], BF16) for i in range(2)]; r_ob2 = [Res("ob0"), Res("ob1")]
    rc = [pb.sb("rc%d" % i, [128, 1], F32) for i in range(2)]; r_rc = [Res("rc0"), Res("rc1")]
    psT2 = psTb; r_psT2 = r_psTb
    G(lambda e: e.memset(onesb[:, :], 1.0), [], [r_ob])
    for hs in range(2):
        for j in range(8):
            src_q = seg(j, "qT", 2 * 192 * 1024).rearrange("(h d t) -> h d t", h=2, t=1024)
            src_k = seg(j, "kT", 2 * 128 * 1024).rearrange("(h d t) -> h d t", h=2, t=1024)
            pb.dma("sp", qA[:, j * 1024:(j + 1) * 1024], src_q[hs, 0:128, :], writes=[r_q])
            pb.dma("sp", qB[0:64, j * 1024:(j + 1) * 1024], src_q[hs, 128:192, :], writes=[r_q])
            pb.dma("sp", kA[:, j * 1024:(j + 1) * 1024], src_k[hs, :, :], writes=[r_k])
            pb.dma("sp", kB[0:64, j * 1024:(j + 1) * 1024], seg(j, "krT", 65536).rearrange("(d t) -> d t", t=1024), writes=[r_k])
            pb.dma("sp", va[:, j * 8:(j + 1) * 8, 0:128], seg(j, "v", 262144).rearrange("(c p h d) -> p c h d", p=128, h=2, d=128)[:, :, hs, :], writes=[r_va])
        G(lambda e: e.memset(va[:, :, 128:129], 1.0), [], [r_va])
        G(lambda e: e.memset(kB[64:65, :], 1.0), [], [r_k])
        V(lambda e: e.memset(sc1[:, 0:1], 0.0), [], [r_sc1])
        idx = 0
        for blk in range(16):
            for (srcA, srcB, r_src) in ((kA, kB, r_k),):
                i0 = idx % 2; idx += 1
                A(lambda e: e.activation(out=sq[i0][:, :], in_=srcA[:, blk * 512:(blk + 1) * 512], func=AF.Square), [r_src], [r_sq[i0]])
                i1 = idx % 2; idx += 1
                V(lambda e: e.tensor_tensor(out=sq[i1][0:64, :], in0=srcB[0:64, blk * 512:(blk + 1) * 512], in1=srcB[0:64, blk * 512:(blk + 1) * 512], op=ALU.mult), [r_src], [r_sq[i1]])
                T(lambda e: e.matmul(psum[0][0:1, :], lhsT=onesb[:, 0:1], rhs=sq[i0][:, :], start=True, stop=False), [r_ob, r_sq[i0]], [r_ps[0]])
                T(lambda e: e.matmul(psum[0][0:1, :], lhsT=onesb[0:64, 0:1], rhs=sq[i1][0:64, :], start=False, stop=True), [r_ob, r_sq[i1]], [r_ps[0]])
                V(lambda e: e.tensor_reduce(out=sc1[:, 1:2], in_=psum[0][0:1, :], axis=AX.X, op=ALU.max), [r_ps[0]], [r_sc1])
                V(lambda e: e.tensor_tensor(out=sc1[:, 0:1], in0=sc1[:, 0:1], in1=sc1[:, 1:2], op=ALU.max), [r_sc1], [r_sc1])
        A(lambda e: e.activation(out=sc1[:, 2:3], in_=sc1[:, 0:1], func=AF.Sqrt), [r_sc1], [r_sc1])
        V(lambda e: e.tensor_scalar(out=sc1[:, 2:3], in0=sc1[:, 2:3], scalar1=-1.0, scalar2=None, op0=ALU.mult), [r_sc1], [r_sc1])
        for blk in range(16):
            i0 = idx % 2; idx += 1
            A(lambda e: e.activation(out=sq[i0][:, :], in_=qA[:, blk * 512:(blk + 1) * 512], func=AF.Square), [r_q], [r_sq[i0]])
            i1 = idx % 2; idx += 1
            V(lambda e: e.tensor_tensor(out=sq[i1][0:64, :], in0=qB[0:64, blk * 512:(blk + 1) * 512], in1=qB[0:64, blk * 512:(blk + 1) * 512], op=ALU.mult), [r_q], [r_sq[i1]])
            T(lambda e: e.matmul(psum[0][0:1, :], lhsT=onesb[:, 0:1], rhs=sq[i0][:, :], start=True, stop=False), [r_ob, r_sq[i0]], [r_ps[0]])
            T(lambda e: e.matmul(psum[0][0:1, :], lhsT=onesb[0:64, 0:1], rhs=sq[i1][0:64, :], start=False, stop=True), [r_ob, r_sq[i1]], [r_ps[0]])
            A(lambda e: e.activation(out=rt[:, :], in_=psum[0][0:1, :], func=AF.Sqrt), [r_ps[0]], [r_rt])
            V(lambda e: e.tensor_scalar(out=nrow[0:1, blk * 512:(blk + 1) * 512], in0=rt[:, :], scalar1=sc1[:, 2:3], scalar2=None, op0=ALU.mult), [r_rt, r_sc1], [r_nrow])
        pb.dma("sp", qB[64:65, :], nrow[0:1, :], reads=[r_nrow], writes=[r_q])
        items = []
        for qb in range(16):
            for kt in range(4 * qb + 4):
                items.append((qb, kt))
        pend = []

        def geom(it, sidx):
            qb, kt = it
            jd = kt - 4 * qb
            i_lo = max(jd, 0)
            return qb, kt, jd, i_lo, qb * 512 + i_lo * 128, 512 - i_lo * 128, 1 + (sidx % 2), sidx % 3

        def emit_qk(it, sidx):
            qb, kt, jd, i_lo, qs, n, sp_, pt = geom(it, sidx)
            T(lambda e: e.matmul(psum[sp_][:, 0:n], lhsT=kA[:, kt * 128:(kt + 1) * 128], rhs=qA[:, qs:qs + n], start=True, stop=False), [r_k, r_q], [r_ps[sp_]])
            T(lambda e: e.matmul(psum[sp_][:, 0:n], lhsT=kB[0:65, kt * 128:(kt + 1) * 128], rhs=qB[0:65, qs:qs + n], start=False, stop=True), [r_k, r_q], [r_ps[sp_]])

        def emit_exp(it, sidx):
            qb, kt, jd, i_lo, qs, n, sp_, pt = geom(it, sidx)
            A(lambda e: e.activation(out=PTm[pt][:, 0:n], in_=psum[sp_][:, 0:n], func=AF.Exp, scale=scale), [r_ps[sp_]], [r_PTm[pt]])
            if jd >= 0:
                G(lambda e: e.tensor_tensor(out=PTm[pt][:, 0:128], in0=PTm[pt][:, 0:128], in1=mask128[:, :], op=ALU.mult), [r_PTm[pt], r_m128], [r_PTm[pt]])

        def flush():
            for (i, bi) in pend:
                T(lambda e: e.matmul(psT2[:, i * 128:(i + 1) * 128], lhsT=ob[bi][:, :], rhs=ident[:, :], is_transpose=True, start=True, stop=True), [r_ob2[bi], r_id], [r_psT2])
            del pend[:]

        def emit_pv(it, sidx):
            qb, kt, jd, i_lo, qs, n, sp_, pt = geom(it, sidx)
            flush()
            for i in range(i_lo, 4):
                last = 4 * qb + i
                T(lambda e: e.matmul(psum[3 + i][:, 0:129], lhsT=PTm[pt][:, (i - i_lo) * 128:(i - i_lo + 1) * 128], rhs=va[:, kt, :], start=(kt == 0), stop=(kt == last)),
                  [r_PTm[pt], r_va], [r_ps[3 + i]])
                if kt == last:
                    bi = i % 2
                    V(lambda e: e.reciprocal(out=rc[bi][:, :], in_=psum[3 + i][:, 128:129]), [r_ps[3 + i]], [r_rc[bi]])
                    V(lambda e: e.tensor_scalar(out=ob[bi][:, :], in0=psum[3 + i][:, 0:128], scalar1=rc[bi][:, 0:1], scalar2=None, op0=ALU.mult), [r_ps[3 + i], r_rc[bi]], [r_ob2[bi]])
                    pend.append((i, bi))
            if kt == 4 * qb + 3:
                flush()
                oi2 = cnt["ostg"] % 2; cnt["ostg"] += 1
                A(lambda e: e.activation(out=ostg[oi2][:, :], in_=psT2[:, 0:512], func=AF.Copy), [r_psT2], [r_ostg[oi2]])
                pb.dma("sp", p2out[qb // 2, 256 + hs * 128:256 + (hs + 1) * 128, (qb % 2) * 512:(qb % 2) * 512 + 512], ostg[oi2][:, :], reads=[r_ostg[oi2]])

        emit_qk(items[0], 0)
        for ii, it in enumerate(items):
            if ii + 1 < len(items):
                emit_qk(items[ii + 1], ii + 1)
            emit_exp(it, ii)
            emit_pv(it, ii)


ALPHA = 4.0 ** 0.25
NE = 32; CAP = 256; FE = 768
NROW = 1 + NE * CAP


def p3_consts():
    k = np.arange(128)
    tri = (k[:, None] < k[None, :]).astype(np.float32)
    iota = np.tile(np.arange(CAP, dtype=np.float32)[None, :], (128, 1))
    ebase = np.tile((np.arange(NE, dtype=np.float32) * CAP + 1)[None, :], (128, 1))
    return {"c_tri": tri, "c_ident": np.eye(128, dtype=np.float32), "c_iota": iota, "c_ebase": ebase}


def build_p3(last):
    nc = bass.Bass("TRN2", target_bir_lowering=False)
    def din(name, shape, dt=F32): return nc.dram_tensor(name, shape, dt, kind="ExternalInput").ap()
    a = dict(
        p3in=din("p3in", [8, 512, TPC], BF16), xres=din("xres", [TPC, D]),
        w_out=din("w_out", [D, D]), ln1_g=din("ln1_g", [1, D]), ln1_b=din("ln1_b", [1, D]),
        mem=din("mem", [256, D]), xg_mem=din("xg_mem", [1, D]), xb_mem=din("xb_mem", [1, D]),
        x_w_q=din("x_w_q", [D, 1024]), x_w_kv=din("x_w_kv", [D, 2048]), x_w_o=din("x_w_o", [1024, D]),
        ln2_g=din("ln2_g", [1, D]), ln2_b=din("ln2_b", [1, D]),
        w_router=din("w_router", [D, NE]), b_router=din("b_router", [1, NE]),
        w_gu=din("w_gu", [NE, D, 2 * FE]), b_gu=din("b_gu", [NE, 2 * FE]), w_down=din("w_down", [NE, FE, D]), b_down=din("b_down", [NE, D]),
        ln3_g=din("ln3_g", [1, D]), ln3_b=din("ln3_b", [1, D]),
    )
    cst = {k: din(k, list(v.shape)) for k, v in p3_consts().items()}
    x_out = nc.dram_tensor("x_out", [TPC, D], F32, kind="ExternalOutput").ap()
    xT_out = nc.dram_tensor("xT_out", [D, TPC], BF16, kind="ExternalOutput").ap()
    scr = dict(pre=nc.dram_tensor("s_pre", [TPC, D], F32).ap(), x1=nc.dram_tensor("s_x1", [TPC, D], F32).ap(), x2=nc.dram_tensor("s_x2", [TPC, D], F32).ap(),
               yall=nc.dram_tensor("s_yall", [NROW, D], BF16, kind="ExternalOutput").ap())
    if os.environ.get("P3DBG"):
        scr["dbg"] = nc.dram_tensor("dbg", [5, 128, 8, NE], F32, kind="ExternalOutput").ap()
        scr["dbgi"] = nc.dram_tensor("dbgi", [128, 8, 8], I32, kind="ExternalOutput").ap()
    with ExitStack() as es:
        pb = PB(nc, es)
        emit_p3(pb, a, cst, x_out, xT_out, scr)
        pb.barrier()
        print("p3 instructions", pb.n_inst)
    return nc


def emit_p3(pb, a, cst, x_out, xT_out, scr):
    nc = pb.nc
    outer_es = pb.es
    STAGE = int(os.environ.get("P3STAGE", "99"))
    psum = [pb.ps("ps%d" % i, [128, 512]) for i in range(7)]
    r_ps = [Res("ps%d" % i, excl=True) for i in range(7)]
    psTb = pb.ps("psTb", [128, 1024], BF16); r_psTb = Res("psTb", excl=True)
    ident = pb.sb("ident", [128, 128], BF16); identf = pb.sb("identf", [128, 128], F32); r_id = Res("ident")
    pb.dma("sp", identf[:, :], cst["c_ident"], writes=[r_id])
    pb.op("dve", lambda e: e.tensor_copy(out=ident[:, :], in_=identf[:, :]), [r_id], [r_id])
    bigA = pb.sb("bigA", [128, D], F32); bigB = pb.sb("bigB", [128, D], F32); bigC = pb.sb("bigC", [128, D], BF16)
    r_A, r_B, r_C = Res("bigA"), Res("bigB"), Res("bigC")
    act_flat = pb.sb("act_flat", [128, 32 * 1024], BF16); r_act = Res("act")
    actT = act_flat[:, :].rearrange("p (a b) -> p a b", b=1024)
    x2b = act_flat[:, :].rearrange("p (a b) -> p a b", b=D)
    st = {"ps": 0, "ev": 0}

    def V(fn, r, w): return pb.op("dve", fn, r, w)
    def A(fn, r, w): return pb.op("act", fn, r, w)
    def G(fn, r, w): return pb.op("pool", fn, r, w)
    def T(fn, r, w): return pb.op("pe", fn, r, w)

    def next_ps(n=6):
        i = st["ps"] % n; st["ps"] += 1
        return i

    def ev_eng():
        st["ev"] += 1
        return "act" if st["ev"] % 2 else "dve"

    def linear_res(w_ap, KT, ktmap, res_ap, pre_ap, wbuf, r_w, rtile, r_rt):
        w_v = w_ap.rearrange("(kt p) n -> p kt n", p=128)
        wi_c = 0
        ri_c = 0
        for cb in range(D // 256):
            wi = wi_c % len(wbuf); wi_c += 1
            ng = max(KT // 8, 1)
            for g in range(ng):
                pb.dma("pool", wbuf[wi][:, g * 8:(g + 1) * 8, :], w_v[:, g * 8:(g + 1) * 8, cb * 256:(cb + 1) * 256], writes=[r_w[wi]])
            for tt in range(8):
                pi = next_ps()
                for kt in range(KT):
                    T(lambda e, kt=kt: e.matmul(psum[pi][:, 0:256], lhsT=actT[:, kt, tt * 128:(tt + 1) * 128], rhs=wbuf[wi][:, ktmap(kt), :], start=(kt == 0), stop=(kt == KT - 1)),
                      [r_act, r_w[wi]], [r_ps[pi]])
                ri = ri_c % len(rtile); ri_c += 1
                pb.dma("sp", rtile[ri][:, :], res_ap[tt * 128:(tt + 1) * 128, cb * 256:(cb + 1) * 256], writes=[r_rt[ri]])
                V(lambda e: e.scalar_tensor_tensor(out=rtile[ri][:, :], in0=rtile[ri][:, :], scalar=float(ALPHA), in1=psum[pi][:, 0:256], op0=ALU.mult, op1=ALU.add),
                  [r_rt[ri], r_ps[pi]], [r_rt[ri]])
                pb.dma("sp", pre_ap[tt * 128:(tt + 1) * 128, cb * 256:(cb + 1) * 256], rtile[ri][:, :], reads=[r_rt[ri]])

    def ln_tile(gbc, bbc, r_gb, stats, mv, r_stat, eps=1e-5, buf=None, r_buf=None, split=False):
        bigA_ = bigA if buf is None else buf
        r_A_ = r_A if r_buf is None else r_buf
        for c in range(8):
            V(lambda e, c=c: e.bn_stats(out=stats[:, c, :], in_=bigA_[:, c * 512:(c + 1) * 512]), [r_A_], [r_stat])
        V(lambda e: e.bn_aggr(out=mv[:, 0:2], in_=stats[:, :, :]), [r_stat], [r_stat])
        A(lambda e: e.activation(out=mv[:, 2:3], in_=mv[:, 1:2], func=AF.Sqrt, bias=float(eps)), [r_stat], [r_stat])
        V(lambda e: e.reciprocal(out=mv[:, 2:3], in_=mv[:, 2:3]), [r_stat], [r_stat])
        V(lambda e: e.tensor_scalar(out=bigA_[:, :], in0=bigA_[:, :], scalar1=mv[:, 0:1], scalar2=mv[:, 2:3], op0=ALU.subtract, op1=ALU.mult), [r_A_, r_stat], [r_A_])
        G(lambda e: e.tensor_tensor(out=bigA_[:, :], in0=bigA_[:, :], in1=gbc[:, :], op=ALU.mult), [r_A_, r_gb], [r_A_])
        if not split:
            ln_tile_b(bbc, r_gb, bigA_, r_A_)

    def ln_tile_b(bbc, r_gb, buf, r_buf):
        V(lambda e: e.tensor_tensor(out=buf[:, :], in0=buf[:, :], in1=bbc[:, :], op=ALU.add), [r_buf, r_gb], [r_buf])

    def transposes_to(dst_fn, writes):
        for g in range(4):
            for k in range(8):
                kt = g * 8 + k
                T(lambda e: e.matmul(psTb[:, k * 128:(k + 1) * 128], lhsT=bigC[:, kt * 128:(kt + 1) * 128], rhs=ident[:, :], is_transpose=True, start=True, stop=True), [r_C, r_id], [r_psTb])
            en = ev_eng()
            if en == "act":
                A(lambda e: e.activation(out=dst_fn(g), in_=psTb[:, :].rearrange("p (k t) -> p k t", t=128), func=AF.Copy), [r_psTb], writes)
            else:
                V(lambda e: e.tensor_copy(out=dst_fn(g), in_=psTb[:, :].rearrange("p (k t) -> p k t", t=128)), [r_psTb], writes)

    def load_gb(gbc, bbc, r_gb, g_ap, b_ap):
        pb.dma("sp", gbc[:, :], g_ap[0:1, :].partition_broadcast(128), writes=[r_gb])
        pb.dma("sp", bbc[:, :], b_ap[0:1, :].partition_broadcast(128), writes=[r_gb])

    for c in range(8):
        pb.dma("sp", actT[:, c * 4:(c + 1) * 4, :], a["p3in"][c].rearrange("(k p) t -> p k t", p=128), writes=[r_act])
    with ExitStack() as es2:
        pb.es = es2
        wbuf = [pb.sb("wbuf%d" % i, [128, 32, 256], BF16) for i in range(3)]; r_w = [Res("w%d" % i) for i in range(3)]
        rtile = [pb.sb("rt%d" % i, [128, 256], F32) for i in range(4)]; r_rt = [Res("rt%d" % i) for i in range(4)]
        def ktmap(kt):
            c = kt // 4; h = (kt % 4) // 2; s = kt % 2
            return h * 16 + c * 2 + s
        linear_res(a["w_out"], 32, ktmap, a["xres"], scr["pre"], wbuf, r_w, rtile, r_rt)
        pb.barrier()
    with ExitStack() as es2:
        pb.es = es2
        gbc = pb.sb("gbc", [128, D], F32); bbc = pb.sb("bbc", [128, D], F32); r_gb = Res("gb")
        stats = pb.sb("stats", [128, 8, 6], F32); mv = pb.sb("mv", [128, 4], F32); r_stat = Res("stat")
        load_gb(gbc, bbc, r_gb, a["ln1_g"], a["ln1_b"])
        stats_b = pb.sb("stats_b", [128, 8, 6], F32); mv_b = pb.sb("mv_b", [128, 4], F32); r_stat_b = Res("stat_b")
        def ln1_s1(tt):
            buf, r_buf = (bigA, r_A) if tt % 2 == 0 else (bigB, r_B)
            st_, mv_, rs_ = (stats, mv, r_stat) if tt % 2 == 0 else (stats_b, mv_b, r_stat_b)
            pb.dma("sp", buf[:, :], scr["pre"][tt * 128:(tt + 1) * 128, :], writes=[r_buf])
            ln_tile(gbc, bbc, r_gb, st_, mv_, rs_, buf=buf, r_buf=r_buf, split=True)

        def ln1_s2(tt):
            buf, r_buf = (bigA, r_A) if tt % 2 == 0 else (bigB, r_B)
            ln_tile_b(bbc, r_gb, buf, r_buf)
            pb.dma("sp", scr["x1"][tt * 128:(tt + 1) * 128, :], buf[:, :], reads=[r_buf])
            A(lambda e: e.activation(out=bigC[:, :], in_=buf[:, :], func=AF.Copy), [r_buf], [r_C])
            transposes_to(lambda g: actT[:, g * 8:(g + 1) * 8, tt * 128:(tt + 1) * 128], [r_act])

        ln1_s1(0)
        for tt in range(8):
            if tt + 1 < 8:
                ln1_s1(tt + 1)
            ln1_s2(tt)
        pb.barrier()
    pb.es = outer_es
    if STAGE < 2:
        for tt in range(8):
            pb.dma("sp", bigA[:, :], scr["x1"][tt * 128:(tt + 1) * 128, :], writes=[r_A])
            pb.dma("sp", x_out[tt * 128:(tt + 1) * 128, :], bigA[:, :], reads=[r_A])
        return

    with ExitStack() as es2:
        pb.es = es2
        wbuf = [pb.sb("xwbuf%d" % i, [128, 32, 256], BF16) for i in range(2)]; r_w = [Res("w0"), Res("w1")]
        rtile = [pb.sb("xrt%d" % i, [128, 256], F32) for i in range(2)]; r_rt = [Res("rt%d" % i) for i in range(2)]
        mem_nT = pb.sb("mem_nT", [128, 32, 256], BF16); r_mn = Res("mem_nT")
        kmT = pb.sb("kmT", [128, 8, 256], BF16); vm = pb.sb("vm", [128, 2, 1024], BF16); r_kv = Res("kvm")
        qxT = pb.sb("qxT", [128, 8, 1024], BF16); r_qx = Res("qxT")
        oxT = pb.sb("oxT", [128, 8, 1024], BF16); r_ox = Res("oxT")
        stats = pb.sb("xstats", [128, 8, 6], F32); mv = pb.sb("xmv", [128, 4], F32); r_stat = Res("stat")
        gbc = bigB; r_gb = r_B
        bbc = pb.sb("xbbc", [128, D], BF16); r_bb = Res("xbbc")
        pb.dma("sp", gbc[:, :], a["xg_mem"][0:1, :].partition_broadcast(128), writes=[r_gb])
        pb.dma("pool", bbc[:, :], a["xb_mem"][0:1, :].partition_broadcast(128), writes=[r_bb])
        for mt in range(2):
            pb.dma("sp", bigA[:, :], a["mem"][mt * 128:(mt + 1) * 128, :], writes=[r_A])
            for c in range(8):
                V(lambda e, c=c: e.bn_stats(out=stats[:, c, :], in_=bigA[:, c * 512:(c + 1) * 512]), [r_A], [r_stat])
            V(lambda e: e.bn_aggr(out=mv[:, 0:2], in_=stats[:, :, :]), [r_stat], [r_stat])
            A(lambda e: e.activation(out=mv[:, 2:3], in_=mv[:, 1:2], func=AF.Sqrt, bias=1e-5), [r_stat], [r_stat])
            V(lambda e: e.reciprocal(out=mv[:, 2:3], in_=mv[:, 2:3]), [r_stat], [r_stat])
            V(lambda e: e.tensor_scalar(out=bigA[:, :], in0=bigA[:, :], scalar1=mv[:, 0:1], scalar2=mv[:, 2:3], op0=ALU.subtract, op1=ALU.mult), [r_A, r_stat], [r_A])
            G(lambda e: e.tensor_tensor(out=bigA[:, :], in0=bigA[:, :], in1=gbc[:, :], op=ALU.mult), [r_A, r_gb], [r_A])
            V(lambda e: e.tensor_tensor(out=bigC[:, :], in0=bigA[:, :], in1=bbc[:, :], op=ALU.add), [r_A, r_bb], [r_C])
            transposes_to(lambda g: mem_nT[:, g * 8:(g + 1) * 8, mt * 128:(mt + 1) * 128], [r_mn])
        wkv_v = a["x_w_kv"].rearrange("(kt p) n -> p kt n", p=128)
        wi_c = 0
        for cb in range(8):
            wi = wi_c % 2; wi_c += 1
            for g in range(4):
                pb.dma("pool", wbuf[wi][:, g * 8:(g + 1) * 8, :], wkv_v[:, g * 8:(g + 1) * 8, cb * 256:(cb + 1) * 256], writes=[r_w[wi]])
            if cb < 4:
                for m in range(2):
                    pi = next_ps()
                    for kt in range(32):
                        T(lambda e, kt=kt: e.matmul(psum[pi][:, 0:256], lhsT=wbuf[wi][:, kt, m * 128:(m + 1) * 128], rhs=mem_nT[:, kt, :], start=(kt == 0), stop=(kt == 31)), [r_w[wi], r_mn], [r_ps[pi]])
                    A(lambda e: e.activation(out=kmT[:, cb * 2 + m, :], in_=psum[pi][:, 0:256], func=AF.Copy), [r_ps[pi]], [r_kv])
            else:
                for mt in range(2):
                    pi = next_ps()
                    for kt in range(32):
                        T(lambda e, kt=kt: e.matmul(psum[pi][:, 0:256], lhsT=mem_nT[:, kt, mt * 128:(mt + 1) * 128], rhs=wbuf[wi][:, kt, :], start=(kt == 0), stop=(kt == 31)), [r_w[wi], r_mn], [r_ps[pi]])
                    V(lambda e: e.tensor_copy(out=vm[:, mt, (cb - 4) * 256:(cb - 3) * 256], in_=psum[pi][:, 0:256]), [r_ps[pi]], [r_kv])
        wq_v = a["x_w_q"].rearrange("(kt p) n -> p kt n", p=128)
        for cb in range(4):
            wi = wi_c % 2; wi_c += 1
            for g in range(4):
                pb.dma("pool", wbuf[wi][:, g * 8:(g + 1) * 8, :], wq_v[:, g * 8:(g + 1) * 8, cb * 256:(cb + 1) * 256], writes=[r_w[wi]])
            for m in range(2):
                for th in range(2):
                    pi = next_ps()
                    for kt in range(32):
                        T(lambda e, kt=kt: e.matmul(psum[pi][:, :], lhsT=wbuf[wi][:, kt, m * 128:(m + 1) * 128], rhs=actT[:, kt, th * 512:(th + 1) * 512], start=(kt == 0), stop=(kt == 31)), [r_w[wi], r_act], [r_ps[pi]])
                    if ev_eng() == "act":
                        A(lambda e: e.activation(out=qxT[:, cb * 2 + m, th * 512:(th + 1) * 512], in_=psum[pi][:, :], func=AF.Copy), [r_ps[pi]], [r_qx])
                    else:
                        V(lambda e: e.tensor_copy(out=qxT[:, cb * 2 + m, th * 512:(th + 1) * 512], in_=psum[pi][:, :]), [r_ps[pi]], [r_qx])
        xs = 256 ** -0.5
        Pm = [pb.sb("Pm%d" % i, [128, 256], BF16) for i in range(2)]; r_Pm = [Res("Pm0"), Res("Pm1")]
        Pe = [pb.sb("Pe%d" % i, [128, 256], F32) for i in range(2)]; r_Pe = [Res("Pe0"), Res("Pe1")]
        PTx = [pb.sb("PTx%d" % i, [128, 2, 128], BF16) for i in range(2)]; r_PTx = [Res("PTx0"), Res("PTx1")]
        sm = [pb.sb("xsm%d" % i, [128, 4], F32) for i in range(2)]; r_sm = [Res("xsm0"), Res("xsm1")]
        it = 0
        for h in range(4):
            for tt in range(8):
                b = it % 2; it += 1
                pi = next_ps()
                for dk in range(2):
                    T(lambda e, dk=dk: e.matmul(psum[pi][:, 0:256], lhsT=qxT[:, 2 * h + dk, tt * 128:(tt + 1) * 128], rhs=kmT[:, 2 * h + dk, :], start=(dk == 0), stop=(dk == 1)), [r_qx, r_kv], [r_ps[pi]])
                V(lambda e: e.tensor_reduce(out=sm[b][:, 0:1], in_=psum[pi][:, 0:256], axis=AX.X, op=ALU.max), [r_ps[pi]], [r_sm[b]])
                V(lambda e: e.tensor_scalar(out=sm[b][:, 0:1], in0=sm[b][:, 0:1], scalar1=-xs, scalar2=None, op0=ALU.mult), [r_sm[b]], [r_sm[b]])
                A(lambda e: e.activation(out=Pe[b][:, :], in_=psum[pi][:, 0:256], func=AF.Exp, scale=xs, bias=sm[b][:, 0:1], accum_out=sm[b][:, 1:2]), [r_ps[pi], r_sm[b]], [r_Pe[b], r_sm[b]])
                V(lambda e: e.reciprocal(out=sm[b][:, 2:3], in_=sm[b][:, 1:2]), [r_sm[b]], [r_sm[b]])
                V(lambda e: e.tensor_scalar(out=Pm[b][:, :], in0=Pe[b][:, :], scalar1=sm[b][:, 2:3], scalar2=None, op0=ALU.mult), [r_Pe[b], r_sm[b]], [r_Pm[b]])
                for mt in range(2):
                    T(lambda e, mt=mt: e.matmul(psTb[:, mt * 128:(mt + 1) * 128], lhsT=Pm[b][:, mt * 128:(mt + 1) * 128], rhs=ident[:, :], is_transpose=True, start=True, stop=True), [r_Pm[b], r_id], [r_psTb])
                V(lambda e: e.tensor_copy(out=PTx[b][:, :, :], in_=psTb[:, 0:256].rearrange("p (k t) -> p k t", t=128)), [r_psTb], [r_PTx[b]])
                pi2 = next_ps()
                for dvh in range(2):
                    for mt in range(2):
                        T(lambda e, mt=mt, dvh=dvh: e.matmul(psum[pi2][:, dvh * 128:(dvh + 1) * 128], lhsT=vm[:, mt, h * 256 + dvh * 128:h * 256 + (dvh + 1) * 128], rhs=PTx[b][:, mt, :], start=(mt == 0), stop=(mt == 1)),
                          [r_kv, r_PTx[b]], [r_ps[pi2]])
                A(lambda e: e.activation(out=oxT[:, 2 * h:2 * h + 2, tt * 128:(tt + 1) * 128], in_=psum[pi2][:, 0:256].rearrange("p (k t) -> p k t", t=128), func=AF.Copy), [r_ps[pi2]], [r_ox])
        V(lambda e: e.tensor_copy(out=actT[:, 0:8, :], in_=oxT[:, :, :]), [r_ox, r_qx], [r_act])
        linear_res(a["x_w_o"], 8, lambda kt: kt, scr["x1"], scr["pre"], wbuf, r_w, rtile, r_rt)
        pb.barrier()
    pb.es = outer_es

    logit = pb.sb("logit", [128, 8, NE], F32); maskt = pb.sb("maskt", [128, 8, NE], F32); gate = pb.sb("gate", [128, 8, NE], F32)
    post = pb.sb("post", [128, 8, NE], F32); maskb = pb.sb("maskb", [128, 8, NE], BF16); top8 = pb.sb("top8", [128, 8, 8], F32)
    r_rt_ = Res("routing")
    with ExitStack() as es2:
        pb.es = es2
        gbc = pb.sb("gbc2", [128, D], F32); bbc = pb.sb("bbc2", [128, D], F32); r_gb = Res("gb")
        stats = pb.sb("stats2", [128, 8, 6], F32); mv = pb.sb("mv2", [128, 4], F32); r_stat = Res("stat")
        load_gb(gbc, bbc, r_gb, a["ln2_g"], a["ln2_b"])
        wr = pb.sb("wr", [128, 32, NE], F32); r_wr = Res("wr")
        pb.dma("sp", wr[:, :, :], a["w_router"].rearrange("(kt p) e -> p kt e", p=128), writes=[r_wr])
        brt = pb.sb("brt", [128, NE], F32)
        pb.dma("sp", brt[:, :], a["b_router"][0:1, :].partition_broadcast(128), writes=[r_wr])
        x2Tf = bigB[:, :].rearrange("p (k t) -> p k t", t=128)
        lnD = pb.sb("lnD2", [128, D], F32); r_D = Res("lnD2")
        stats_b2 = pb.sb("stats_b2", [128, 8, 6], F32); mv_b2 = pb.sb("mv_b2", [128, 4], F32); r_stat_b2 = Res("stat_b2")

        def ln2_s1(tt):
            buf, r_buf = (bigA, r_A) if tt % 2 == 0 else (lnD, r_D)
            st_, mv_, rs_ = (stats, mv, r_stat) if tt % 2 == 0 else (stats_b2, mv_b2, r_stat_b2)
            pb.dma("sp", buf[:, :], scr["pre"][tt * 128:(tt + 1) * 128, :], writes=[r_buf])
            ln_tile(gbc, bbc, r_gb, st_, mv_, rs_, buf=buf, r_buf=r_buf, split=True)

        def ln2_s2(tt):
            buf, r_buf = (bigA, r_A) if tt % 2 == 0 else (lnD, r_D)
            st_, mv_, rs_ = (stats, mv, r_stat) if tt % 2 == 0 else (stats_b2, mv_b2, r_stat_b2)
            ln_tile_b(bbc, r_gb, buf, r_buf)
            pb.dma("sp", scr["x2"][tt * 128:(tt + 1) * 128, :], buf[:, :], reads=[r_buf])
            A(lambda e: e.activation(out=x2b[:, tt, :], in_=buf[:, :], func=AF.Copy), [r_buf], [r_act])
            for g in range(8):
                pi = next_ps()
                for k in range(4):
                    kt = g * 4 + k
                    T(lambda e: e.matmul(psum[pi][:, k * 128:(k + 1) * 128], lhsT=buf[:, kt * 128:(kt + 1) * 128], rhs=identf[:, :], is_transpose=True, start=True, stop=True), [r_buf, r_id], [r_ps[pi]])
                if ev_eng() == "act":
                    A(lambda e: e.activation(out=x2Tf[:, g * 4:(g + 1) * 4, :], in_=psum[pi][:, :].rearrange("p (k t) -> p k t", t=128), func=AF.Copy), [r_ps[pi]], [r_B])
                else:
                    V(lambda e: e.tensor_copy(out=x2Tf[:, g * 4:(g + 1) * 4, :], in_=psum[pi][:, :].rearrange("p (k t) -> p k t", t=128)), [r_ps[pi]], [r_B])
            pi = next_ps()
            for kt in range(32):
                T(lambda e, kt=kt: e.matmul(psum[pi][:, 0:NE], lhsT=x2Tf[:, kt, :], rhs=wr[:, kt, :], start=(kt == 0), stop=(kt == 31)), [r_B, r_wr], [r_ps[pi]])
            V(lambda e: e.tensor_tensor(out=logit[:, tt, :], in0=psum[pi][:, 0:NE], in1=brt[:, :], op=ALU.add), [r_ps[pi], r_wr], [r_rt_])
            V(lambda e: e.max(out=top8[:, tt, :], in_=logit[:, tt, :]), [r_rt_], [r_rt_])
            V(lambda e: e.tensor_scalar(out=maskt[:, tt, :], in0=logit[:, tt, :], scalar1=top8[:, tt, 3:4], scalar2=None, op0=ALU.is_ge), [r_rt_], [r_rt_])
            V(lambda e: e.tensor_scalar(out=mv_[:, 3:4], in0=top8[:, tt, 0:1], scalar1=-1.0, scalar2=None, op0=ALU.mult), [r_rt_, rs_], [rs_])
            A(lambda e: e.activation(out=gate[:, tt, :], in_=logit[:, tt, :], func=AF.Exp, bias=mv_[:, 3:4]), [r_rt_, rs_], [r_rt_])
            V(lambda e: e.tensor_tensor(out=gate[:, tt, :], in0=gate[:, tt, :], in1=maskt[:, tt, :], op=ALU.mult), [r_rt_], [r_rt_])
            V(lambda e: e.tensor_reduce(out=mv_[:, 3:4], in_=gate[:, tt, :], axis=AX.X, op=ALU.add), [r_rt_, rs_], [rs_])
            V(lambda e: e.reciprocal(out=mv_[:, 3:4], in_=mv_[:, 3:4]), [rs_], [rs_])
            V(lambda e: e.tensor_scalar(out=gate[:, tt, :], in0=gate[:, tt, :], scalar1=mv_[:, 3:4], scalar2=None, op0=ALU.mult), [r_rt_, rs_], [r_rt_])
            V(lambda e: e.tensor_copy(out=maskb[:, tt, :], in_=maskt[:, tt, :]), [r_rt_], [r_rt_])

        ln2_s1(0)
        for tt in range(8):
            if tt + 1 < 8:
                ln2_s1(tt + 1)
            ln2_s2(tt)
        pb.barrier()
    pb.es = outer_es
    if STAGE < 3:
        for tt in range(8):
            pb.dma("sp", bigA[:, :], scr["x2"][tt * 128:(tt + 1) * 128, :], writes=[r_A])
            pb.dma("sp", x_out[tt * 128:(tt + 1) * 128, :], bigA[:, :], reads=[r_A])
        return

    idx = pb.sb("idx", [128, 8, 8], I32); r_idx = Res("idx")
    gT = pb.sb("gT", [NE, TPC], F32); r_gT = Res("gT")
    with ExitStack() as es2:
        pb.es = es2
        trib = pb.sb("trib", [128, 128], BF16); onesb = pb.sb("onesb", [128, 128], BF16); r_cb = Res("cb")
        trif = bigA[:, 0:128]
        pb.dma("sp", trif, cst["c_tri"], writes=[r_A])
        V(lambda e: e.tensor_copy(out=trib[:, :], in_=trif), [r_A], [r_cb])
        G(lambda e: e.memset(onesb[:, :], 1.0), [], [r_cb])
        iota = pb.sb("iota", [128, CAP], F32); ebase = pb.sb("ebase", [128, NE], F32)
        pb.dma("sp", iota[:, :], cst["c_iota"], writes=[r_cb])
        pb.dma("sp", ebase[:, :], cst["c_ebase"], writes=[r_cb])
        for tt in range(8):
            pi = next_ps()
            T(lambda e: e.matmul(psum[pi][:, 0:NE], lhsT=trib[:, :], rhs=maskb[:, tt, :], start=True, stop=(tt == 0)), [r_cb, r_rt_], [r_ps[pi]])
            for t2 in range(tt):
                T(lambda e, t2=t2: e.matmul(psum[pi][:, 0:NE], lhsT=onesb[:, :], rhs=maskb[:, t2, :], start=False, stop=(t2 == tt - 1)), [r_cb, r_rt_], [r_ps[pi]])
            V(lambda e: e.tensor_copy(out=post[:, tt, :], in_=psum[pi][:, 0:NE]), [r_ps[pi]], [r_rt_])
        valid = pb.sb("valid", [128, 8, NE], F32); ghl = pb.sb("ghl", [128, 8, NE, 2], BF16); gtmp = pb.sb("gtmp", [128, 8, NE], F32)
        V(lambda e: e.tensor_scalar(out=valid[:, :, :], in0=post[:, :, :], scalar1=float(CAP), scalar2=None, op0=ALU.is_lt), [r_rt_], [r_rt_])
        V(lambda e: e.tensor_tensor(out=valid[:, :, :], in0=valid[:, :, :], in1=maskt[:, :, :], op=ALU.mult), [r_rt_], [r_rt_])
        V(lambda e: e.tensor_copy(out=ghl[:, :, :, 0], in_=gate[:, :, :]), [r_rt_], [r_rt_])
        V(lambda e: e.tensor_tensor(out=gtmp[:, :, :], in0=gate[:, :, :], in1=ghl[:, :, :, 0], op=ALU.subtract), [r_rt_], [r_rt_])
        V(lambda e: e.tensor_copy(out=ghl[:, :, :, 1], in_=gtmp[:, :, :]), [r_rt_], [r_rt_])
        bgu_nat = bigB[0:NE, 0:2 * FE]
        pb.dma("sp", bgu_nat, a["b_gu"], writes=[r_B])
        bgu = pb.sb("bgu", [128, 12, NE], F32); r_bgu = Res("bgu")
        for f in range(12):
            pi = next_ps()
            T(lambda e: e.matmul(psum[pi][:, 0:NE], lhsT=bigB[0:NE, f * 128:(f + 1) * 128], rhs=identf[0:NE, 0:NE], is_transpose=True, start=True, stop=True), [r_B, r_id], [r_ps[pi]])
            V(lambda e: e.tensor_copy(out=bgu[:, f, :], in_=psum[pi][:, 0:NE]), [r_ps[pi]], [r_bgu])
        zrow = pb.sb("zrow", [128, 32], BF16); r_z = Res("zrow"); r_yall = Res("yall")
        G(lambda e: e.memset(zrow[:, :], 0.0), [], [r_z])
        pb.dma("sp", scr["yall"][0, :].rearrange("(p f) -> p f", f=32), zrow[:, :], reads=[r_z], writes=[r_yall])

        wgu = [pb.sb("wgu%d" % i, [128, 32, 256], BF16) for i in range(3)]; r_wgu = [Res("wgu%d" % i) for i in range(3)]
        wdn = [pb.sb("wdn%d" % i, [128, 6, 512], BF16) for i in range(3)]; r_wdn = [Res("wdn%d" % i) for i in range(3)]
        sel_all = [pb.sb("sel%d" % i, [128, CAP], BF16) for i in range(16)]; r_sel_all = [Res("sel%d" % i) for i in range(16)]
        xgT = bigB[:, :].bitcast(BF16).rearrange("p (k c) -> p k c", c=CAP); r_xg = r_B
        hT = pb.sb("hT", [128, 6, CAP], BF16); r_hT = Res("hT")
        gact = pb.sb("gact", [128, 6, CAP], F32); r_ga = Res("gact")
        tg = [pb.sb("tg%d" % i, [128, CAP], F32) for i in range(2)]; r_tg = [Res("tg0"), Res("tg1")]
        gsl = pb.sb("gsl", [128, 2, 2], F32); gs1 = pb.sb("gs1", [128, 2], F32); r_gs = Res("gs")
        ystg = [bigC[:, i * 512:(i + 1) * 512] for i in range(4)]; r_ys = [Res("ys%d" % i) for i in range(4)]
        cnt = {"wgu": 0, "wdn": 0, "ys": 0, "tg": 0}
        NEX = int(os.environ.get("P3NEX", str(NE)))
        for ex in range(NEX):
            sel = sel_all[(ex % 2) * 8:(ex % 2) * 8 + 8]; r_sel = r_sel_all[(ex % 2) * 8:(ex % 2) * 8 + 8]
            for tt in range(8):
                V(lambda e, tt=tt: e.tensor_scalar(out=sel[tt][:, :], in0=iota[:, :], scalar1=post[:, tt, ex:ex + 1], scalar2=valid[:, tt, ex:ex + 1], op0=ALU.is_equal, op1=ALU.mult),
                  [r_cb, r_rt_], [r_sel[tt]])
            for kt in range(32):
                pi = next_ps()
                for tt in range(8):
                    T(lambda e, tt=tt: e.matmul(psum[pi][:, 0:CAP], lhsT=x2b[:, tt, kt * 128:(kt + 1) * 128], rhs=sel[tt][:, :], start=(tt == 0), stop=(tt == 7)), [r_act, r_sel[tt]], [r_ps[pi]])
                if ev_eng() == "act":
                    A(lambda e: e.activation(out=xgT[:, kt, :], in_=psum[pi][:, 0:CAP], func=AF.Copy), [r_ps[pi]], [r_xg])
                else:
                    V(lambda e: e.tensor_copy(out=xgT[:, kt, :], in_=psum[pi][:, 0:CAP]), [r_ps[pi]], [r_xg])
            pi = next_ps()
            for stl in range(2):
                for tt in range(8):
                    T(lambda e, tt=tt: e.matmul(psum[pi][:, stl * 2:stl * 2 + 2], lhsT=sel[tt][:, stl * 128:(stl + 1) * 128], rhs=ghl[:, tt, ex, :], start=(tt == 0), stop=(tt == 7)), [r_sel[tt], r_rt_], [r_ps[pi]])
            V(lambda e: e.tensor_copy(out=gsl[:, :, :], in_=psum[pi][:, 0:4].rearrange("p (a b) -> p a b", b=2)), [r_ps[pi]], [r_gs])
            V(lambda e: e.tensor_tensor(out=gs1[:, :], in0=gsl[:, :, 0], in1=gsl[:, :, 1], op=ALU.add), [r_gs], [r_gs])
            wgu_v = a["w_gu"][ex].rearrange("(kt p) n -> p kt n", p=128)
            for blk in range(6):
                wi = cnt["wgu"] % 3; cnt["wgu"] += 1
                for g in range(4):
                    pb.dma("pool", wgu[wi][:, g * 8:(g + 1) * 8, :], wgu_v[:, g * 8:(g + 1) * 8, blk * 256:(blk + 1) * 256], writes=[r_wgu[wi]])
                for m in range(2):
                    f = blk * 2 + m
                    pi = next_ps()
                    for kt in range(32):
                        T(lambda e, kt=kt: e.matmul(psum[pi][:, 0:CAP], lhsT=wgu[wi][:, kt, m * 128:(m + 1) * 128], rhs=xgT[:, kt, :], start=(kt == 0), stop=(kt == 31)), [r_wgu[wi], r_xg], [r_ps[pi]])
                    ti = cnt["tg"] % 2; cnt["tg"] += 1
                    if f < 6:
                        V(lambda e: e.tensor_scalar(out=gact[:, f, :], in0=psum[pi][:, 0:CAP], scalar1=bgu[:, f, ex:ex + 1], scalar2=7.0, op0=ALU.add, op1=ALU.min), [r_ps[pi], r_bgu], [r_ga])
                        A(lambda e: e.activation(out=tg[ti][:, :], in_=gact[:, f, :], func=AF.Sigmoid, scale=1.702), [r_ga], [r_tg[ti]])
                        V(lambda e: e.tensor_tensor(out=gact[:, f, :], in0=gact[:, f, :], in1=tg[ti][:, :], op=ALU.mult), [r_ga, r_tg[ti]], [r_ga])
                    else:
                        V(lambda e: e.tensor_scalar(out=tg[ti][:, :], in0=psum[pi][:, 0:CAP], scalar1=bgu[:, f, ex:ex + 1], scalar2=7.0, op0=ALU.add, op1=ALU.min), [r_ps[pi], r_bgu], [r_tg[ti]])
                        V(lambda e: e.tensor_scalar(out=tg[ti][:, :], in0=tg[ti][:, :], scalar1=-7.0, scalar2=1.0, op0=ALU.max, op1=ALU.add), [r_tg[ti]], [r_tg[ti]])
                        V(lambda e: e.tensor_tensor(out=hT[:, f - 6, :], in0=tg[ti][:, :], in1=gact[:, f - 6, :], op=ALU.mult), [r_tg[ti], r_ga], [r_hT])
            wdn_v = a["w_down"][ex].rearrange("(fk p) n -> p fk n", p=128)
            for cb in range(8):
                wi = cnt["wdn"] % 3; cnt["wdn"] += 1
                pb.dma("pool", wdn[wi][:, :, :], wdn_v[:, :, cb * 512:(cb + 1) * 512], writes=[r_wdn[wi]])
                for stl in range(2):
                    pi = next_ps()
                    for fk in range(6):
                        T(lambda e, fk=fk: e.matmul(psum[pi][:, :], lhsT=hT[:, fk, stl * 128:(stl + 1) * 128], rhs=wdn[wi][:, fk, :], start=(fk == 0), stop=(fk == 5)), [r_hT, r_wdn[wi]], [r_ps[pi]])
                    yi = cnt["ys"] % 4; cnt["ys"] += 1
                    if True:
                        V(lambda e: e.tensor_scalar(out=ystg[yi], in0=psum[pi][:, :], scalar1=gs1[:, stl:stl + 1], scalar2=None, op0=ALU.mult), [r_ps[pi], r_gs], [r_ys[yi]])
                    r0 = 1 + ex * CAP + stl * 128
                    pb.dma("sp", scr["yall"][r0:r0 + 128, cb * 512:(cb + 1) * 512], ystg[yi], reads=[r_ys[yi]])
        pb.barrier()

        key = gtmp
        V(lambda e: e.tensor_copy(out=key[:, :, :], in_=post[:, :, :]), [r_rt_], [r_rt_])
        for tt in range(8):
            V(lambda e, tt=tt: e.tensor_tensor(out=key[:, tt, :], in0=key[:, tt, :], in1=ebase[:, :], op=ALU.add), [r_rt_, r_cb], [r_rt_])
        V(lambda e: e.tensor_tensor(out=key[:, :, :], in0=key[:, :, :], in1=valid[:, :, :], op=ALU.mult), [r_rt_], [r_rt_])
        for tt in range(8):
            V(lambda e, tt=tt: e.max(out=top8[:, tt, :], in_=key[:, tt, :]), [r_rt_], [r_rt_])
        V(lambda e: e.tensor_copy(out=idx[:, :, :], in_=top8[:, :, :]), [r_rt_], [r_idx])
        for tt in range(8):
            pi = next_ps()
            T(lambda e, tt=tt: e.matmul(psum[pi][0:NE, 0:128], lhsT=gate[:, tt, :], rhs=identf[:, :], is_transpose=True, start=True, stop=True), [r_rt_, r_id], [r_ps[pi]])
            V(lambda e, tt=tt: e.tensor_copy(out=gT[:, tt * 128:(tt + 1) * 128], in_=psum[pi][0:NE, 0:128]), [r_ps[pi]], [r_gT])
        pb.barrier()
        if "dbg" in scr:
            for i, t_ in enumerate((post, valid, gate, key, maskt)):
                pb.dma("sp", scr["dbg"][i], t_[:, :, :], reads=[r_rt_])
            pb.dma("sp", scr["dbgi"], idx[:, :, :], reads=[r_idx])
    pb.es = outer_es
    with ExitStack() as es2:
        pb.es = es2
        gbc = pb.sb("gbc3", [128, D], F32); bbc = pb.sb("bbc3", [128, D], F32); r_gb = Res("gb")
        stats = pb.sb("stats3", [128, 8, 6], F32); mv = pb.sb("mv3", [128, 4], F32); r_stat = Res("stat")
        load_gb(gbc, bbc, r_gb, a["ln3_g"], a["ln3_b"])
        bdn = pb.sb("bdn", [NE, D], F32); r_bdn = Res("bdn")
        pb.dma("sp", bdn[:, :], a["b_down"], writes=[r_bdn])
        gat = [pb.sb("gat%d" % i, [128, D], BF16) for i in range(4)]; r_gat = [Res("gat%d" % i) for i in range(4)]
        stats_b3 = pb.sb("stats_b3", [128, 8, 6], F32); mv_b3 = pb.sb("mv_b3", [128, 4], F32); r_stat_b3 = Res("stat_b3")
        xTs = pb.sb("xTs", [128, 32, 128], BF16); r_xTs = Res("xTs")
        def ln3_s1(tt):
            for k in range(4):
                pb.dma("pool", None, None, reads=[r_idx], writes=[r_gat[k]],
                       fn=lambda e, k=k: e.indirect_dma_start(out=gat[k][:, :], out_offset=None, in_=scr["yall"][:, :], in_offset=bass.IndirectOffsetOnAxis(ap=idx[:, tt, k:k + 1], axis=0)))
            buf, r_buf = (bigA, r_A) if tt % 2 == 0 else (bigB, r_B)
            st_, mv_, rs_ = (stats, mv, r_stat) if tt % 2 == 0 else (stats_b3, mv_b3, r_stat_b3)
            pb.dma("sp", buf[:, :], scr["x2"][tt * 128:(tt + 1) * 128, :], writes=[r_buf])
            A(lambda e: e.activation(out=buf[:, :], in_=buf[:, :], func=AF.Copy, scale=float(ALPHA)), [r_buf], [r_buf])
            for k in range(4):
                V(lambda e, k=k: e.tensor_tensor(out=buf[:, :], in0=buf[:, :], in1=gat[k][:, :], op=ALU.add), [r_buf, r_gat[k]], [r_buf])
            for cb in range(8):
                pi = next_ps()
                T(lambda e: e.matmul(psum[pi][:, :], lhsT=gT[:, tt * 128:(tt + 1) * 128], rhs=bdn[:, cb * 512:(cb + 1) * 512], start=True, stop=True), [r_gT, r_bdn], [r_ps[pi]])
                V(lambda e: e.tensor_tensor(out=buf[:, cb * 512:(cb + 1) * 512], in0=buf[:, cb * 512:(cb + 1) * 512], in1=psum[pi][:, :], op=ALU.add), [r_buf, r_ps[pi]], [r_buf])
            if os.environ.get("P3DBG") != "pre3":
                ln_tile(gbc, bbc, r_gb, st_, mv_, rs_, buf=buf, r_buf=r_buf, split=True)

        def ln3_s2(tt):
            buf, r_buf = (bigA, r_A) if tt % 2 == 0 else (bigB, r_B)
            if os.environ.get("P3DBG") != "pre3":
                ln_tile_b(bbc, r_gb, buf, r_buf)
            pb.dma("sp", x_out[tt * 128:(tt + 1) * 128, :], buf[:, :], reads=[r_buf])
            A(lambda e: e.activation(out=bigC[:, :], in_=buf[:, :], func=AF.Copy), [r_buf], [r_C])
            transposes_to(lambda g: xTs[:, g * 8:(g + 1) * 8, :], [r_xTs])
            pb.dma("sp", xT_out.rearrange("(kt p) t -> p kt t", p=128)[:, :, tt * 128:(tt + 1) * 128], xTs[:, :, :], reads=[r_xTs])

        ln3_s1(0)
        for tt in range(8):
            if tt + 1 < 8:
                ln3_s1(tt + 1)
            ln3_s2(tt)
    pb.es = outer_es


def _r1(v):
    return np.ascontiguousarray(np.asarray(v, np.float32)).reshape(1, -1)


def kernel(**inp):
    f32 = np.float32
    g = lambda k: np.asarray(inp[k])
    x = g('x').astype(f32)[0]
    mem = np.ascontiguousarray(g('mem').astype(f32)[0])
    pos = g('positions').astype(np.int32).reshape(1, -1)
    cores = list(range(8))
    c1 = p1_consts(); c2 = p2_consts(); c3 = p3_consts()
    xres = [np.ascontiguousarray(x[c * TPC:(c + 1) * TPC]) for c in cores]
    xT = [np.ascontiguousarray(xres[c].T) for c in cores]
    for l in range(2):
        nc1 = build_p1(x_is_bf16=(l > 0))
        maps = [dict(xT=xT[c], w_in=g('w_in')[l], w_uq=g('mla_w_uq')[l], w_ukv=g('mla_w_ukv')[l], g_q=g('mla_g_q')[l], g_kv=g('mla_g_kv')[l],
                     pos=np.ascontiguousarray(pos[:, c * TPC:(c + 1) * TPC]), **c1) for c in cores]
        r1 = run_bass_kernel_spmd(nc1, maps, core_ids=cores).results
        p1out = np.stack([np.asarray(r["p1out"]) for r in r1])
        p1g = np.stack([np.asarray(r["p1g"]) for r in r1])
        del r1
        nc2 = build_p2()
        maps = [dict(p2in=np.ascontiguousarray(p1out[:, c]), p2g=np.ascontiguousarray(p1g[:, c]),
                     mlb=np.array([[g('ml_b_i')[l, c], g('ml_b_f')[l, c]]], f32), mlg=_r1(g('ml_norm_g')[l, c * 256:(c + 1) * 256]), **c2) for c in cores]
        r2 = run_bass_kernel_spmd(nc2, maps, core_ids=cores).results
        p2out = np.stack([np.asarray(r["p2out"]) for r in r2])
        del r2, p1out
        nc3 = build_p3(l == 1)
        maps = [dict(p3in=np.ascontiguousarray(p2out[:, j]), xres=xres[j], w_out=g('w_out')[l], ln1_g=_r1(g('ln1_g')[l]), ln1_b=_r1(g('ln1_b')[l]),
                     mem=mem, xg_mem=_r1(g('x_g_mem')[l]), xb_mem=_r1(g('x_b_mem')[l]), x_w_q=g('x_w_q')[l], x_w_kv=g('x_w_kv')[l], x_w_o=g('x_w_o')[l],
                     ln2_g=_r1(g('ln2_g')[l]), ln2_b=_r1(g('ln2_b')[l]), w_router=g('w_router')[l], b_router=_r1(g('b_router')[l]),
                     w_gu=g('w_gu')[l], b_gu=g('b_gu')[l], w_down=g('w_down')[l], b_down=g('b_down')[l], ln3_g=_r1(g('ln3_g')[l]), ln3_b=_r1(g('ln3_b')[l]), **c3)
                for j in cores]
        r3 = run_bass_kernel_spmd(nc3, maps, core_ids=cores).results
        xres = [np.asarray(r["x_out"]) for r in r3]
        xT = [np.asarray(r["xT_out"]) for r in r3]
        del r3, p2out
    return np.concatenate(xres, 0)[None].astype(np.float32)
```
